# Optimizing a Trainium2 kernel written in Bass

```python
import jax, jax.numpy as jnp
from jax import lax
import numpy as np

D_MODEL = 1024
BATCH = 4
SEQ = 8192
DEPTH = 2

EPS = 1e-6
HEAD_DIM = 64
ATTN_Q_HEADS = 8
ATTN_KV_HEADS = 2
ATTN_GROUP = ATTN_Q_HEADS // ATTN_KV_HEADS
ATTN_WIDTH = ATTN_Q_HEADS * HEAD_DIM
KV_WIDTH = ATTN_KV_HEADS * HEAD_DIM
WINDOW = 128
ATTN_BLOCK = WINDOW
ROPE_THETA = 10000.0
SSM_HEADS = 8
SSM_HEAD_DIM = 64
SSM_WIDTH = SSM_HEADS * SSM_HEAD_DIM
SSM_GROUPS = 2
SSM_HEADS_PER_GROUP = SSM_HEADS // SSM_GROUPS
SSM_STATE = 128
CONV_WIDTH = 4
CONV_CH = SSM_WIDTH + 2 * SSM_GROUPS * SSM_STATE
CHUNK = 128
MIX_WIDTH = ATTN_WIDTH + SSM_WIDTH
IN_WIDTH = ATTN_WIDTH + 2 * KV_WIDTH + SSM_WIDTH + CONV_CH + SSM_HEADS
IN_SPLITS = [ATTN_WIDTH,
             ATTN_WIDTH + KV_WIDTH,
             ATTN_WIDTH + 2 * KV_WIDTH,
             ATTN_WIDTH + 2 * KV_WIDTH + SSM_WIDTH,
             ATTN_WIDTH + 2 * KV_WIDTH + SSM_WIDTH + CONV_CH]
FFN_DIM = 2816
N_EXPERTS = 8
TOP_K = 2
EXPERT_DIM = 3584
MOE_BLOCK = 256
N_DENSE = (DEPTH + 1) // 2
N_MOE = DEPTH // 2

kernel_name = "hymba_swa_sink_ssd_moe_adaln"


def rms_norm(x, g):
    xf = x.astype(jnp.float32)
    y = xf * lax.rsqrt(jnp.mean(xf * xf, axis=-1, keepdims=True) + EPS)
    return (y * g.astype(jnp.float32)).astype(x.dtype)


def modulate(h, shift, scale):
    return h * (1 + scale[:, None, :]) + shift[:, None, :]


def apply_rope(t, cos, sin):
    tf = t.astype(jnp.float32)
    t1, t2 = jnp.split(tf, 2, axis=-1)
    return jnp.concatenate([t1 * cos - t2 * sin, t2 * cos + t1 * sin], axis=-1).astype(t.dtype)


def sliding_window_attention(q, k, v, sinks):
    b, s = q.shape[:2]
    nb = s // ATTN_BLOCK
    qb = q.reshape(b, nb, ATTN_BLOCK, ATTN_KV_HEADS, ATTN_GROUP, HEAD_DIM)
    kb = k.reshape(b, nb, ATTN_BLOCK, ATTN_KV_HEADS, HEAD_DIM)
    vb = v.reshape(b, nb, ATTN_BLOCK, ATTN_KV_HEADS, HEAD_DIM)

    def with_prev(t):
        prev = jnp.pad(t[:, :-1], ((0, 0), (1, 0), (0, 0), (0, 0), (0, 0)))
        return jnp.concatenate([prev, t], axis=2)

    kk, vv = with_prev(kb), with_prev(vb)
    scores = jnp.einsum('bnqhgd,bnkhd->bhgnqk', qb, kk).astype(jnp.float32) * (HEAD_DIM ** -0.5)
    qi = jnp.arange(ATTN_BLOCK)[:, None]
    kj = jnp.arange(2 * ATTN_BLOCK)[None, :]
    delta = ATTN_BLOCK + qi - kj
    band = (delta >= 0) & (delta < WINDOW)
    key_valid = (jnp.arange(nb)[:, None] * ATTN_BLOCK - ATTN_BLOCK + kj) >= 0
    mask = band[None] & key_valid[:, None, :]
    scores = jnp.where(mask, scores, -jnp.inf)
    sink = sinks.astype(jnp.float32).reshape(ATTN_KV_HEADS, ATTN_GROUP)[None, :, :, None, None, None]
    m = jnp.maximum(jnp.max(scores, axis=-1, keepdims=True), sink)
    p = jnp.exp(scores - m)
    denom = jnp.sum(p, axis=-1, keepdims=True) + jnp.exp(sink - m)
    out = jnp.einsum('bhgnqk,bnkhd->bnqhgd', p / denom, vv.astype(jnp.float32))
    return out.reshape(b, s, ATTN_WIDTH).astype(q.dtype)


def causal_depthwise_conv(u, w, bias):
    out = lax.conv_general_dilated(
        u, w[:, None, :].astype(u.dtype), window_strides=(1,),
        padding=[(CONV_WIDTH - 1, 0)], dimension_numbers=('NWC', 'WIO', 'NWC'),
        feature_group_count=CONV_CH)
    return out + bias.astype(u.dtype)


def ssd_chunked(x, dt, a, b_mat, c_mat):
    bsz, s = x.shape[:2]
    nc = s // CHUNK
    g, r, p_, n = SSM_GROUPS, SSM_HEADS_PER_GROUP, SSM_HEAD_DIM, SSM_STATE
    xd = (x.astype(jnp.float32) * dt[..., None]).reshape(bsz, nc, CHUNK, g, r, p_)
    a_cs = jnp.cumsum((dt * a).reshape(bsz, nc, CHUNK, g, r), axis=2)
    bc = b_mat.astype(jnp.float32).reshape(bsz, nc, CHUNK, g, n)
    cc = c_mat.astype(jnp.float32).reshape(bsz, nc, CHUNK, g, n)
    seg = a_cs[:, :, :, None] - a_cs[:, :, None, :]
    causal = jnp.tril(jnp.ones((CHUNK, CHUNK), dtype=bool))[:, :, None, None]
    decay = jnp.exp(jnp.where(causal, seg, -jnp.inf))
    cb = jnp.einsum('bclgn,bcsgn->bclsg', cc, bc)
    y_diag = jnp.einsum('bclsgr,bcsgrp->bclgrp', cb[..., None] * decay, xd)
    decay_to_end = jnp.exp(a_cs[:, :, -1:] - a_cs)
    chunk_states = jnp.einsum('bclgn,bclgrp->bcgrpn', bc, xd * decay_to_end[..., None])
    chunk_decay = jnp.exp(a_cs[:, :, -1])

    def step(h, inp):
        st, dec = inp
        return dec[..., None, None] * h + st, h

    h0 = jnp.zeros((bsz, g, r, p_, n), jnp.float32)
    _, h_prev = lax.scan(step, h0, (jnp.moveaxis(chunk_states, 1, 0), jnp.moveaxis(chunk_decay, 1, 0)))
    h_prev = jnp.moveaxis(h_prev, 0, 1)
    y_off = jnp.einsum('bclgn,bcgrpn->bclgrp', cc, h_prev) * jnp.exp(a_cs)[..., None]
    return (y_diag + y_off).reshape(bsz, s, SSM_HEADS, SSM_HEAD_DIM)


def swiglu(h, w_gate, w_up, w_down):
    return (jax.nn.silu(h @ w_gate) * (h @ w_up)) @ w_down


def moe_swiglu(h, router_w, w_gate, w_up, w_down):
    b, s, d = h.shape
    t = b * s
    hf = h.reshape(t, d)
    logits = (hf @ router_w).astype(jnp.float32)
    top_val, top_idx = lax.top_k(logits, TOP_K)
    gates = jax.nn.softmax(top_val, axis=-1)
    tk = t * TOP_K
    e_flat = top_idx.reshape(tk).astype(jnp.int32)
    g_flat = gates.reshape(tk)
    e_s, order = lax.sort((e_flat, jnp.arange(tk, dtype=jnp.int32)), num_keys=1, is_stable=True)
    tok_s = order // TOP_K
    g_s = g_flat[order]
    counts = jnp.bincount(e_flat, length=N_EXPERTS).astype(jnp.int32)
    padded = (counts + MOE_BLOCK - 1) // MOE_BLOCK * MOE_BLOCK
    pad_end = jnp.cumsum(padded)
    pad_start = pad_end - padded
    start = jnp.cumsum(counts) - counts
    dest = pad_start[e_s] + jnp.arange(tk, dtype=jnp.int32) - start[e_s]
    n_rows = (tk + MOE_BLOCK - 1) // MOE_BLOCK * MOE_BLOCK + N_EXPERTS * MOE_BLOCK
    n_blocks = n_rows // MOE_BLOCK
    row_tok = jnp.full((n_rows,), t, jnp.int32).at[dest].set(tok_s)
    row_gate = jnp.zeros((n_rows,), jnp.float32).at[dest].set(g_s)
    block_expert = jnp.minimum(
        jnp.searchsorted(pad_end, jnp.arange(n_blocks, dtype=jnp.int32) * MOE_BLOCK, side='right'),
        N_EXPERTS - 1)
    h_pad = jnp.concatenate([hf, jnp.zeros((1, d), hf.dtype)], axis=0)
    xb = h_pad[row_tok].reshape(n_blocks, MOE_BLOCK, d)

    def expert_block(args):
        xblk, e = args
        return swiglu(xblk, w_gate[e], w_up[e], w_down[e])

    yb = lax.map(expert_block, (xb, block_expert)).reshape(n_rows, d)
    out = jnp.zeros((t + 1, d), h.dtype).at[row_tok].add((yb * row_gate[:, None]).astype(h.dtype))
    return out[:t].reshape(b, s, d)


def setup_inputs(seed: int = 0) -> dict:
    key = jax.random.key(seed)
    ks = jax.random.split(key, 32)
    f32 = jnp.float32
    nrm = lambda k, shape, scale: jax.random.normal(k, shape, f32) * scale
    x = jax.random.normal(ks[0], (BATCH, SEQ, D_MODEL), f32)
    c = jax.random.normal(ks[1], (BATCH, D_MODEL), f32)
    offsets = jax.random.randint(ks[2], (BATCH, 1), 0, 4096, dtype=jnp.int32)
    positions = offsets + jnp.arange(SEQ, dtype=jnp.int32)[None, :]
    dt0 = jnp.exp(jax.random.uniform(ks[3], (DEPTH, SSM_HEADS), f32) * (np.log(0.1) - np.log(0.001)) + np.log(0.001))
    return {
        "x": x,
        "c": c,
        "positions": positions,
        "ada_w": nrm(ks[4], (DEPTH, D_MODEL, 6 * D_MODEL), 0.5 * D_MODEL ** -0.5),
        "ada_b": nrm(ks[5], (DEPTH, 6 * D_MODEL), 0.02),
        "norm_mix_g": 1.0 + nrm(ks[6], (DEPTH, D_MODEL), 0.02),
        "norm_ffn_g": 1.0 + nrm(ks[7], (DEPTH, D_MODEL), 0.02),
        "w_in": nrm(ks[8], (DEPTH, D_MODEL, IN_WIDTH), D_MODEL ** -0.5),
        "w_out": nrm(ks[9], (DEPTH, MIX_WIDTH, D_MODEL), MIX_WIDTH ** -0.5),
        "attn_sinks": nrm(ks[10], (DEPTH, ATTN_Q_HEADS), 0.5),
        "conv_w": nrm(ks[11], (DEPTH, CONV_WIDTH, CONV_CH), CONV_WIDTH ** -0.5),
        "conv_b": nrm(ks[12], (DEPTH, CONV_CH), 0.02),
        "dt_bias": dt0 + jnp.log(-jnp.expm1(-dt0)),
        "a_log": jnp.log(jax.random.uniform(ks[13], (DEPTH, SSM_HEADS), f32, 1.0, 16.0)),
        "d_skip": 1.0 + nrm(ks[14], (DEPTH, SSM_HEADS), 0.1),
        "ssm_norm_g": 1.0 + nrm(ks[15], (DEPTH, SSM_WIDTH), 0.02),
        "ffn_w_gate": nrm(ks[16], (N_DENSE, D_MODEL, FFN_DIM), D_MODEL ** -0.5),
        "ffn_w_up": nrm(ks[17], (N_DENSE, D_MODEL, FFN_DIM), D_MODEL ** -0.5),
        "ffn_w_down": nrm(ks[18], (N_DENSE, FFN_DIM, D_MODEL), FFN_DIM ** -0.5),
        "router_w": nrm(ks[19], (N_MOE, D_MODEL, N_EXPERTS), D_MODEL ** -0.5),
        "moe_w_gate": nrm(ks[20], (N_MOE, N_EXPERTS, D_MODEL, EXPERT_DIM), D_MODEL ** -0.5),
        "moe_w_up": nrm(ks[21], (N_MOE, N_EXPERTS, D_MODEL, EXPERT_DIM), D_MODEL ** -0.5),
        "moe_w_down": nrm(ks[22], (N_MOE, N_EXPERTS, EXPERT_DIM, D_MODEL), EXPERT_DIM ** -0.5),
        "final_norm_g": 1.0 + nrm(ks[23], (D_MODEL,), 0.02),
    }


def reference(x, c, positions, ada_w, ada_b, norm_mix_g, norm_ffn_g, w_in, w_out,
              attn_sinks, conv_w, conv_b, dt_bias, a_log, d_skip, ssm_norm_g,
              ffn_w_gate, ffn_w_up, ffn_w_down, router_w, moe_w_gate, moe_w_up,
              moe_w_down, final_norm_g):
    f32 = jnp.float32
    b, s, _ = x.shape
    inv_freq = ROPE_THETA ** (-jnp.arange(0, HEAD_DIM, 2, dtype=f32) / HEAD_DIM)
    ang = positions.astype(f32)[..., None] * inv_freq
    cos = jnp.cos(ang)[:, :, None, :]
    sin = jnp.sin(ang)[:, :, None, :]
    c_act = jax.nn.silu(c)
    for l in range(DEPTH):
        mod = c_act @ ada_w[l] + ada_b[l]
        sh_m, sc_m, g_m, sh_f, sc_f, g_f = jnp.split(mod, 6, axis=-1)
        h = modulate(rms_norm(x, norm_mix_g[l]), sh_m, sc_m)
        proj = h @ w_in[l]
        q, k, v, z, xbc, dt_raw = jnp.split(proj, IN_SPLITS, axis=-1)
        q = apply_rope(q.reshape(b, s, ATTN_Q_HEADS, HEAD_DIM), cos, sin)
        k = apply_rope(k.reshape(b, s, ATTN_KV_HEADS, HEAD_DIM), cos, sin)
        v = v.reshape(b, s, ATTN_KV_HEADS, HEAD_DIM)
        attn_out = sliding_window_attention(q, k, v, attn_sinks[l])
        xbc = jax.nn.silu(causal_depthwise_conv(xbc, conv_w[l], conv_b[l]))
        xs, bm, cm = jnp.split(xbc, [SSM_WIDTH, SSM_WIDTH + SSM_GROUPS * SSM_STATE], axis=-1)
        xs = xs.reshape(b, s, SSM_HEADS, SSM_HEAD_DIM)
        dt = jax.nn.softplus(dt_raw.astype(f32) + dt_bias[l].astype(f32))
        a = -jnp.exp(a_log[l].astype(f32))
        y = ssd_chunked(xs, dt, a, bm.reshape(b, s, SSM_GROUPS, SSM_STATE),
                        cm.reshape(b, s, SSM_GROUPS, SSM_STATE))
        y = y + d_skip[l].astype(f32)[:, None] * xs.astype(f32)
        y = y.reshape(b, s, SSM_WIDTH) * jax.nn.silu(z.astype(f32))
        yg = y.reshape(b, s, SSM_GROUPS, SSM_WIDTH // SSM_GROUPS)
        yg = yg * lax.rsqrt(jnp.mean(yg * yg, axis=-1, keepdims=True) + EPS)
        ssm_out = (yg.reshape(b, s, SSM_WIDTH) * ssm_norm_g[l].astype(f32)).astype(x.dtype)
        mixed = jnp.concatenate([attn_out, ssm_out], axis=-1) @ w_out[l]
        x = x + g_m[:, None, :] * mixed
        h = modulate(rms_norm(x, norm_ffn_g[l]), sh_f, sc_f)
        if l % 2 == 0:
            f = swiglu(h, ffn_w_gate[l // 2], ffn_w_up[l // 2], ffn_w_down[l // 2])
        else:
            f = moe_swiglu(h, router_w[l // 2], moe_w_gate[l // 2], moe_w_up[l // 2], moe_w_down[l // 2])
        x = x + g_f[:, None, :] * f
    return rms_norm(x, final_norm_g)
```

```python
import math
from contextlib import ExitStack

import numpy as np
import ml_dtypes

import concourse.bass as bass
import concourse.mybir as mybir
from concourse.bass_utils import run_bass_kernel_spmd

F32 = mybir.dt.float32
BF16 = mybir.dt.bfloat16
I32 = mybir.dt.int32
AF = mybir.ActivationFunctionType
ALU = mybir.AluOpType
PI = math.pi

D = 1024
NQH = 8
INW = 2952
C_Q, C_QS, C_K, C_KS, C_X, C_V, C_Z, C_DT = 0, 512, 1024, 1152, 1280, 2304, 2432, 2944
FFN = 2816
EXD = 3584
NEXP = 8
EPS = 1e-6
NEG = -30000.0
P_GM, P_GF, P_CW, P_CB, P_SK, P_DTB, P_ALOG, P_DSK, P_SSMG, P_RW, P_TOT = 0, 8, 16, 48, 56, 64, 72, 80, 88, 600, 664
B_ID, B_U, B_ONES, B_MCUR, B_MPREV, B_TOT = 0, 128, 256, 384, 896, 1408


class Res:
    __slots__ = ("name", "w", "r", "hold")

    def __init__(self, name):
        self.name = name
        self.w = None
        self.r = {}
        self.hold = None


class Sched:
    ENG = ("pe", "act", "dve", "pool", "sp")

    def __init__(self, nc, es):
        self.nc = nc
        self.es = es
        self.prog = {e: [] for e in self.ENG}
        self.sem = {}
        self.cnt = {}
        for e in ("pe", "act", "dve", "pool"):
            self.sem[e] = es.enter_context(nc.semaphore("c_" + e))
            self.cnt[e] = 0
        self.waited = {e: {} for e in self.ENG}
        self.nres = 0

    def res(self, name=None):
        self.nres += 1
        return Res(name or ("r%d" % self.nres))

    def _waits(self, eng, reads, writes):
        deps = []
        for r in reads:
            if r.w is not None:
                deps.append((r.w, True))
        for w in writes:
            if w.w is not None:
                deps.append((w.w, False))
            for k, (v, e) in w.r.items():
                deps.append(((k, v, e), False))
        waits = {}
        for (k, v, e), raw in deps:
            if e == eng:
                if eng == "pe":
                    continue
                if not raw:
                    continue
                if v < self.cnt[eng] - SAME_ENG_DIST:
                    continue
            if self.waited[eng].get(k, 0) >= v:
                continue
            if waits.get(k, 0) < v:
                waits[k] = v
        for k, v in waits.items():
            self.waited[eng][k] = v
        return list(waits.items())

    def _commit(self, ticket, reads, writes):
        k, v, e = ticket
        for r in reads:
            old = r.r.get(k)
            if old is None or old[0] < v:
                r.r[k] = (v, e)
        for w in writes:
            w.w = ticket
            w.r = {}

    def op(self, eng, fn, r=(), w=()):
        if eng != "pe":
            for x in w:
                if x.hold is not None and x.hold[0] > 0:
                    x.hold[0] -= 1
        waits = self._waits(eng, r, w)
        self.cnt[eng] += 1
        self.prog[eng].append((waits, fn, (eng, 1)))
        self._commit((eng, self.cnt[eng], eng), r, w)

    def dma(self, q, sem, out, in_, r=(), w=()):
        if sem not in self.sem:
            self.sem[sem] = self.es.enter_context(self.nc.semaphore("d_" + sem))
            self.cnt[sem] = 0
        waits = self._waits(q, r, w)
        self.cnt[sem] += 16
        self.prog[q].append((waits, (lambda e: e.dma_start(out=out, in_=in_)), (sem, 16)))
        self._commit((sem, self.cnt[sem], "dma"), r, w)

    def idma(self, sem, out, out_off, in_, in_off, r=(), w=()):
        if sem not in self.sem:
            self.sem[sem] = self.es.enter_context(self.nc.semaphore("d_" + sem))
            self.cnt[sem] = 0
        waits = self._waits("pool", r, w)
        hist = self.__dict__.setdefault("idma_hist", [])
        if len(hist) >= IDMA_DEPTH:
            k0, v0 = hist[-IDMA_DEPTH]
            if self.waited["pool"].get(k0, 0) < v0:
                self.waited["pool"][k0] = v0
                waits = [(k, v) for (k, v) in waits if k != k0] + [(k0, max(v0, dict(waits).get(k0, 0)))]
        self.cnt[sem] += 16
        hist.append((sem, self.cnt[sem]))
        self.prog["pool"].append((waits, (lambda e: e.indirect_dma_start(out=out, out_offset=out_off, in_=in_, in_offset=in_off)), (sem, 16)))
        self._commit((sem, self.cnt[sem], "dma"), r, w)

    def barrier(self):
        for eng in self.ENG:
            waits = []
            for k, v in self.cnt.items():
                if v == 0 or k == eng:
                    continue
                if self.waited[eng].get(k, 0) >= v:
                    continue
                self.waited[eng][k] = v
                waits.append((k, v))
            self.prog[eng].append((waits, None, None))

    def wait_all(self, eng, reads):
        waits = self._waits(eng, reads, ())
        self.prog[eng].append((waits, None, None))

    def emit(self, block):
        S = self

        def run(name, e):
            for waits, fn, inc in S.prog[name]:
                for k, v in waits:
                    e.wait_ge(S.sem[k], v)
                if fn is None:
                    continue
                ins = fn(e)
                if inc is not None:
                    ins.then_inc(S.sem[inc[0]], inc[1])

        @block.sync
        def _(e):
            run("sp", e)

        @block.tensor
        def _(e):
            run("pe", e)

        @block.scalar
        def _(e):
            run("act", e)

        @block.vector
        def _(e):
            run("dve", e)

        @block.gpsimd
        def _(e):
            run("pool", e)


def TT(out, in0, in1, op):
    return lambda e: e.tensor_tensor(out=out, in0=in0, in1=in1, op=op)


def TS(out, in0, s1, s2=None, op0=ALU.mult, op1=None):
    if op1 is None:
        return lambda e: e.tensor_scalar(out=out, in0=in0, scalar1=s1, scalar2=None, op0=op0)
    return lambda e: e.tensor_scalar(out=out, in0=in0, scalar1=s1, scalar2=s2, op0=op0, op1=op1)


def STT(out, in0, scalar, in1, op0, op1):
    return lambda e: e.scalar_tensor_tensor(out=out, in0=in0, scalar=scalar, in1=in1, op0=op0, op1=op1)


def ACTF(out, in_, func, bias=None, scale=None, accum_out=None):
    kw = {}
    if bias is not None:
        kw["bias"] = bias
    if scale is not None:
        kw["scale"] = scale
    if accum_out is not None:
        kw["accum_out"] = accum_out
    return lambda e: e.activation(out=out, in_=in_, func=func, **kw)


def ACTS(lst):
    def f(e):
        ins = None
        for (out, in_, func, kw) in lst:
            ins = e.activation(out=out, in_=in_, func=func, **kw)
        return ins
    return f


def CP(out, in_):
    return lambda e: e.tensor_copy(out=out, in_=in_)


def MEMSET(ap, v):
    return lambda e: e.memset(ap, v)


def MMS(lst):
    def f(e):
        ins = None
        for (out, lhsT, rhs, st, sp) in lst:
            ins = e.matmul(out, lhsT=lhsT, rhs=rhs, start=st, stop=sp)
        return ins
    return f


def TRS(lst):
    def f(e):
        ins = None
        for (out, in_, ident) in lst:
            ins = e.transpose(out=out, in_=in_, identity=ident)
        return ins
    return f


SLOTR = 512
DBG_M1 = False
SAME_ENG_DIST = 1000000
ALLOC = {}
IDMA_DEPTH = 6


def build(L, stop_after=None, split=True, sparse=True):
    NB = L // 128
    LM = L // 2 if split else L
    assert (not sparse) or (2 * LM) % SLOTR == 0
    TTOK = min(1024, LM)
    NBT = TTOK // 128
    nc = bass.Bass("TRN2", target_bir_lowering=False)

    def din(name, shape, dt=F32):
        return nc.dram_tensor(name, list(shape), dt, kind="ExternalInput").ap()

    x_in = din("x", [L, D])
    pos_in = din("pos", [1, L], I32)
    cT_in = din("cT", [128, 8])
    sel_in = din("sel", [128, 2])
    cstf_in = din("cstf", [128, 130])
    cstb_in = din("cstb", [128, B_TOT], BF16)
    fng_in = din("fng", [128, D])
    lp_in = [din("lp%d" % l, [128, P_TOT]) for l in range(2)]
    adaw_in = [din("adaw%d" % l, [D, 6 * D]) for l in range(2)]
    adab_in = [din("adab%d" % l, [1, 6 * D]) for l in range(2)]
    win_in = [din("win%d" % l, [D, INW]) for l in range(2)]
    wout_in = [din("wout%d" % l, [D, D]) for l in range(2)]
    fg_in = din("ffg", [D, FFN])
    fu_in = din("ffu", [D, FFN])
    fd_in = din("ffd", [FFN, D])
    mg_in = mu_in = md_in = None
    if not sparse:
        mg_in = din("mog", [NEXP, D, EXD])
        mu_in = din("mou", [NEXP, D, EXD])
        md_in = din("mod", [NEXP, EXD, D])
    NFM = EXD // 128
    NSL = (2 * LM) // SLOTR + NEXP if sparse else 1
    NROWS = NSL * SLOTR
    if sparse:
        mgl_in = din("mogl", [NFM * 1024, 1024])
        mul_in = din("moul", [NFM * 1024, 1024])
        mdl_in = din("modl", [NFM * 1024, 1024])
        cst2_in = din("cst2", [128, NSL + 1 + NFM])
        hs_d = nc.dram_tensor("hs_d", [NROWS, D], BF16, kind="Internal").ap()
        ys_d = nc.dram_tensor("ys_d", [NROWS, D], F32, kind="Internal").ap()
    out_d = nc.dram_tensor("out", [LM, D], F32, kind="ExternalOutput").ap()
    xa_d = nc.dram_tensor("xa", [L, D], F32, kind="Internal").ap()
    xb_d = nc.dram_tensor("xb", [L, D], F32, kind="Internal").ap()
    cos_d = nc.dram_tensor("cosd", [128, L], F32, kind="Internal").ap()
    sin_d = nc.dram_tensor("sind", [128, L], F32, kind="Internal").ap()

    with ExitStack() as es:
        S = Sched(nc, es)

        uniq = [0]

        def sbt(es_, name, shape, dt=F32):
            uniq[0] += 1
            nb = int(np.prod(shape[1:])) * (2 if dt == BF16 else 4)
            ALLOC[id(es_)] = ALLOC.get(id(es_), 0) + ((nb + 31) // 32) * 32
            return es_.enter_context(nc.sbuf_tensor("s%d_%s" % (uniq[0], name), list(shape), dt))

        banks = []
        for i in range(8):
            t = es.enter_context(nc.psum_tensor("ps%d" % i, [128, 512], F32))
            banks.append((t, S.res("ps%d" % i)))
        pscur = [0]
        for (_t, _r) in banks:
            _r.hold = [0]

        def PS_try(n=1):
            for d in range(8):
                t, r = banks[(pscur[0] + d) % 8]
                if r.hold[0] == 0:
                    pscur[0] = pscur[0] + d + 1
                    r.hold[0] = n
                    return t, r
            return None

        def PS(n=1):
            got = PS_try(n)
            assert got is not None, "no free PSUM bank"
            return got

        def PSG(n=1):
            while True:
                got = PS_try(n)
                if got is not None:
                    return got
                yield None

        cstf = sbt(es, "cstf", [128, 130]); R_c = S.res("const")
        cstb = sbt(es, "cstb", [128, B_TOT], BF16)
        lp = [sbt(es, "lp%d" % l, [128, P_TOT]) for l in range(2)]
        cT = sbt(es, "cT", [128, 8])
        modc = [sbt(es, "modc%d" % l, [128, 48]) for l in range(2)]
        gsm = [sbt(es, "gsm%d" % l, [128, 8]) for l in range(2)]
        gsf = [sbt(es, "gsf%d" % l, [128, 8]) for l in range(2)]
        gmrow = [sbt(es, "gmrow%d" % l, [128, D]) for l in range(2)]
        gfrow = [sbt(es, "gfrow%d" % l, [128, D]) for l in range(2)]
        onesr = sbt(es, "onesr", [1, 128])
        one1 = sbt(es, "one1", [1, 1])
        epsD = sbt(es, "epsD", [128, 1])
        epsG = sbt(es, "epsG", [128, 1])
        R_mod = S.res("mod")

        ident_f = cstf[:, 0:128]
        invf = cstf[:, 128:129]
        sgn = cstf[:, 129:130]
        ident_b = cstb[:, B_ID:B_ID + 128]
        U_b = cstb[:, B_U:B_U + 128]
        ones_b = cstb[:, B_ONES:B_ONES + 128]
        mcur_b = cstb[:, B_MCUR:B_MCUR + 512]
        mprev_b = cstb[:, B_MPREV:B_MPREV + 512]

        S.dma("sp", "cst", cstf[:], cstf_in, w=[R_c])
        S.dma("sp", "cst", cstb[:], cstb_in, w=[R_c])
        S.dma("sp", "cst", cT[:], cT_in, w=[R_c])
        selt = sbt(es, "selt", [128, 2])
        S.dma("sp", "cst", selt[:], sel_in, w=[R_c])
        for l in range(2):
            S.dma("sp", "cst", lp[l][:], lp_in[l], w=[R_c])
        R_c2 = S.res("const2")
        S.op("pool", MEMSET(onesr[:], 1.0), w=[R_c2])
        S.op("pool", MEMSET(one1[:], 1.0), w=[R_c2])
        S.op("pool", MEMSET(epsD[:], EPS), w=[R_c2])
        S.op("pool", MEMSET(epsG[:], EPS), w=[R_c2])

        with ExitStack() as pe_:
            cact = sbt(pe_, "cact", [128, 8]); R_cact = S.res()
            ctmp = sbt(pe_, "ctmp", [128, 8])
            modrow = sbt(pe_, "modrow", [1, 6 * D]); R_mr = S.res()
            adab = sbt(pe_, "adab", [1, 6 * D]); R_ab = S.res()
            stg = [sbt(pe_, "adstg%d" % i, [128, 8, 512]) for i in range(2)]
            R_stg = [S.res() for _ in range(2)]
            S.op("act", ACTF(ctmp[:], cT[:], AF.Exp, scale=-1.0), r=[R_c], w=[R_cact])
            S.op("dve", TS(ctmp[:], ctmp[:], 1.0, None, ALU.add), r=[R_cact], w=[R_cact])
            S.op("dve", lambda e: e.reciprocal(out=ctmp[:], in_=ctmp[:]), r=[R_cact], w=[R_cact])
            S.op("dve", TT(cact[:], ctmp[:], cT[:], ALU.mult), r=[R_cact, R_c], w=[R_cact])
            for l in range(2):
                S.dma("sp", "adab", adab[:], adab_in[l], w=[R_ab])
                for cc in range(12):
                    k = cc % 2
                    S.dma("sp", "adstg%d" % k, stg[k][:],
                          adaw_in[l][:, cc * 512:(cc + 1) * 512].rearrange("(c p) n -> p c n", p=128), w=[R_stg[k]])
                    pt, pr = PS()
                    S.op("pe", MMS([(pt[0:1, :], cact[:, dch:dch + 1], stg[k][:, dch, :], dch == 0, dch == 7)
                                    for dch in range(8)]), r=[R_cact, R_stg[k]], w=[pr])
                    S.op("dve", TT(modrow[0:1, cc * 512:(cc + 1) * 512], pt[0:1, :], adab[0:1, cc * 512:(cc + 1) * 512], ALU.add),
                         r=[R_ab], w=[pr, R_mr])
                pt, pr = PS()
                S.op("pe", MMS([(pt[:, j:j + 1], modrow[0:1, j * 128:(j + 1) * 128], one1[0:1, 0:1], True, True)
                                for j in range(48)]), r=[R_mr, R_c2], w=[pr])
                S.op("dve", CP(modc[l][:], pt[:, 0:48]), w=[pr, R_mod])
                for (dst, off) in ((gmrow[l], 2 * D), (gfrow[l], 5 * D)):
                    for hf in range(2):
                        pt, pr = PS()
                        S.op("pe", MMS([(pt[:, :], onesr[0:1, :], modrow[0:1, off + hf * 512: off + (hf + 1) * 512], True, True)]),
                             r=[R_mr, R_c2], w=[pr])
                        S.op("act", ACTF(dst[:, hf * 512:(hf + 1) * 512], pt[:, :], AF.Copy), w=[pr, R_mod])
                S.op("dve", STT(gsm[l][:], modc[l][:, 8:16], 1.0, lp[l][:, P_GM:P_GM + 8], ALU.add, ALU.mult), r=[R_c, R_mod], w=[R_mod])
                S.op("dve", STT(gsf[l][:], modc[l][:, 32:40], 1.0, lp[l][:, P_GF:P_GF + 8], ALU.add, ALU.mult), r=[R_c, R_mod], w=[R_mod])

        S.barrier()
        R_tab = [S.res("ropetab0"), S.res("ropetab1")]
        with ExitStack() as pe_:
            CH = min(1024, L)
            posi = sbt(pe_, "posi", [128, CH], I32); R_pi = S.res()
            ang = sbt(pe_, "ang", [128, CH]); R_ang = S.res()
            t0 = sbt(pe_, "rt0", [128, CH]); R_t0 = S.res()
            ti = sbt(pe_, "rti", [128, CH], I32); R_ti = S.res()
            t1 = sbt(pe_, "rt1", [128, CH]); R_t1 = S.res()
            t2 = sbt(pe_, "rt2", [128, CH]); R_t2 = S.res()
            tabs = [sbt(pe_, "rtab%d" % i, [128, CH]) for i in range(2)]
            R_tabs = [S.res() for _ in range(2)]
            for c0 in range(0, L, CH):
                S.dma("sp", "posi", posi[:], pos_in[0:1, c0:c0 + CH].partition_broadcast(128).rearrange("p o n -> p (o n)"), w=[R_pi])
                S.op("dve", CP(t0[:], posi[:]), r=[R_pi], w=[R_t0])
                S.op("dve", TS(ang[:], t0[:], invf), r=[R_t0, R_c], w=[R_ang])
                for which in range(2):
                    if which == 0:
                        S.op("dve", TS(t1[:], ang[:], PI / 2, None, ALU.add), r=[R_ang], w=[R_t1])
                        src = t1
                        R_src = R_t1
                    else:
                        src = ang
                        R_src = R_ang
                    S.op("dve", TS(t0[:], src[:], 1.0 / (2 * PI), 0.5, ALU.mult, ALU.add), r=[R_src], w=[R_t0])
                    S.op("dve", CP(ti[:], t0[:]), r=[R_t0], w=[R_ti])
                    S.op("dve", CP(t0[:], ti[:]), r=[R_ti], w=[R_t0])
                    S.op("dve", STT(t2[:], t0[:], -6.28125, src[:], ALU.mult, ALU.add), r=[R_t0, R_src], w=[R_t2])
                    S.op("dve", STT(t2[:], t0[:], -0.0019353071795864769, t2[:], ALU.mult, ALU.add), r=[R_t0, R_t2], w=[R_t2])
                    S.op("dve", TS(t0[:], t2[:], -PI, 2 * PI, ALU.is_lt, ALU.mult), r=[R_t2], w=[R_t0])
                    S.op("dve", TT(t2[:], t0[:], t2[:], ALU.add), r=[R_t0, R_t2], w=[R_t2])
                    S.op("dve", TS(t2[:], t2[:], PI, -PI, ALU.min, ALU.max), r=[R_t2], w=[R_t2])
                    if which == 0:
                        S.op("act", ACTF(tabs[0][:], t2[:], AF.Sin), r=[R_t2], w=[R_tabs[0]])
                        S.dma("sp", "tabst0", cos_d[:, c0:c0 + CH], tabs[0][:], r=[R_tabs[0]], w=[R_tab[0]])
                    else:
                        S.op("act", ACTF(t1[:], t2[:], AF.Sin), r=[R_t2], w=[R_t1])
                        S.op("dve", TS(tabs[1][:], t1[:], sgn), r=[R_t1, R_c], w=[R_tabs[1]])
                        S.dma("sp", "tabst1", sin_d[:, c0:c0 + CH], tabs[1][:], r=[R_tabs[1]], w=[R_tab[1]])

        S.barrier()
        def mixer(l, xsrc, R_src, xdst, R_dst):
            P = lp[l]
            with ExitStack() as me:
                winb = sbt(me, "winb", [128, 8, INW], BF16); R_win = S.res()
                woutb = sbt(me, "woutb", [128, 8, D], BF16); R_wout = S.res()
                negA = sbt(me, "negA", [128, 8]); esink = sbt(me, "esink", [128, 8])
                Dexp = sbt(me, "Dexp", [128, 8, 64]); R_lc = S.res()
                hst = sbt(me, "hst", [128, 512]); R_hst = S.res()
                hstb = sbt(me, "hstb", [128, 512], BF16); R_hstb = S.res()
                XD = 6
                kT = [sbt(me, "kT%d" % i, [128, 128], BF16) for i in range(4)]; R_kT = [S.res() for _ in range(4)]
                vaug = [sbt(me, "vaug%d" % i, [128, 2, 65], BF16) for i in range(4)]; R_va = [S.res() for _ in range(4)]
                xpre = [sbt(me, "xpre%d" % i, [128, 8, 132], BF16) for i in range(2)]; R_xp = [S.res() for _ in range(2)]
                dgw = sbt(me, "dgw", [128, 32, 128], BF16)
                xin = [sbt(me, "xin%d" % i, [128, D]) for i in range(XD)]; R_xin = [S.res() for _ in range(XD)]
                cs = [sbt(me, "cs%d" % i, [128, 2, 128]) for i in range(2)]; R_cs = [S.res() for _ in range(2)]
                junk = sbt(me, "junk", [128, D], BF16); R_junk = S.res()
                ss = sbt(me, "ss", [128, 1]); R_ss = S.res()
                lnv = sbt(me, "lnv", [128, 1]); rstd = sbt(me, "rstd", [128, 1]); R_rstd = S.res()
                xn = sbt(me, "xn", [128, D]); R_xn = S.res()
                hT = sbt(me, "hT", [128, 8, 128], BF16); R_hT = S.res()
                rt1 = sbt(me, "rt1", [128, 4, 128]); R_rt1 = S.res()
                rt2 = sbt(me, "rt2", [128, 4, 128]); R_rt2 = S.res()
                qT = [sbt(me, "qT%d" % i, [128, 4, 128], BF16) for i in range(3)]; R_qT = [S.res() for _ in range(3)]
                xdt = sbt(me, "xdt", [128, 8]); R_xdt = S.res()
                sp1 = sbt(me, "sp1", [128, 8]); R_sp1 = S.res()
                dt = [sbt(me, "dt%d" % i, [128, 8]) for i in range(3)]; R_dt = [S.res() for _ in range(3)]
                dtA = [sbt(me, "dtA%d" % i, [128, 8], BF16) for i in range(3)]; R_dtA = [S.res() for _ in range(3)]
                xbcT = [sbt(me, "xbcT%d" % i, [128, 8, 128], BF16) for i in range(3)]; R_xbcT = [S.res() for _ in range(3)]
                pTp = [sbt(me, "pTp%d" % g, [128, 512], BF16) for g in range(2)]; R_pTp = [S.res() for _ in range(2)]
                pTc = [sbt(me, "pTc%d" % g, [128, 512], BF16) for g in range(2)]; R_pTc = [S.res() for _ in range(2)]
                den = sbt(me, "den", [128, 8]); R_den = S.res()
                attn = [sbt(me, "attn%d" % i, [128, 8, 64], BF16) for i in range(3)]; R_attn = [S.res() for _ in range(3)]
                xd = [sbt(me, "xd%d" % i, [128, 8, 64], BF16) for i in range(2)]; R_xd = [S.res() for _ in range(2)]
                xdd = sbt(me, "xdd", [128, 8, 64], BF16); R_xdd = S.res()
                xst = [sbt(me, "xst%d" % i, [128, 512], BF16) for i in range(2)]; R_xst = [S.res() for _ in range(2)]
                Btok = [sbt(me, "Btok%d" % i, [128, 2, 128], BF16) for i in range(2)]; R_Bt = [S.res() for _ in range(2)]
                acs = sbt(me, "acs", [128, 16]); R_acs = S.res()
                nacs = [sbt(me, "nacs%d" % i, [128, 8]) for i in range(2)]; ea = [sbt(me, "ea%d" % i, [128, 8]) for i in range(2)]; dte = [sbt(me, "dte%d" % i, [128, 8]) for i in range(2)]
                cdec = [sbt(me, "cdec%d" % i, [128, 8]) for i in range(2)]; dif = [sbt(me, "dif%d" % i, [128, 8]) for i in range(2)]; R_sm = [S.res() for _ in range(2)]
                udta = sbt(me, "udta", [128, 8, 128], BF16); R_udta = S.res()
                dec = sbt(me, "dec", [128, 8, 128], BF16); R_dec = S.res()
                cbs = sbt(me, "cbs", [128, 2, 128], BF16); R_cbs = S.res()
                mt = [sbt(me, "mt%d" % i, [128, 8, 128], BF16) for i in range(2)]; R_mt = [S.res() for _ in range(2)]
                htmp = sbt(me, "htmp", [128, 512]); R_htmp = S.res()
                y1 = sbt(me, "y1", [128, 512]); R_y1 = S.res()
                y2 = sbt(me, "y2", [128, 512]); R_y2 = S.res()
                sz = [sbt(me, "sz%d" % i, [128, 512]) for i in range(4)]; R_sz = [S.res() for _ in range(4)]
                ssg = sbt(me, "ssg", [128, 2]); rg = sbt(me, "rg", [128, 2]); lng = sbt(me, "lng", [128, 2]); R_ssg = S.res()
                ssm = [sbt(me, "ssm%d" % i, [128, 512], BF16) for i in range(2)]; R_ssm = [S.res() for _ in range(2)]
                mixT = sbt(me, "mixT", [128, 8, 128], BF16); R_mixT = S.res()
                xo = sbt(me, "xo", [128, D]); R_xo = S.res()
                wstg = [sbt(me, "wstg%d" % i, [128, D]) for i in range(2)]; R_wstg = [S.res() for _ in range(2)]

                cast_eng = ["pool", "act", "dve"]
                wc = 0
                for dch in range(8):
                    for pc in range(3):
                        k = wc % 2
                        eng = cast_eng[wc % 3]
                        wc += 1
                        c0, c1 = pc * 984, (pc + 1) * 984
                        S.dma("sp", "wstg%d" % k, wstg[k][:, 0:984], win_in[l][dch * 128:(dch + 1) * 128, c0:c1], w=[R_wstg[k]])
                        if eng == "act":
                            S.op("act", ACTF(winb[:, dch, c0:c1], wstg[k][:, 0:984], AF.Copy), r=[R_wstg[k]], w=[R_win])
                        else:
                            S.op(eng, CP(winb[:, dch, c0:c1], wstg[k][:, 0:984]), r=[R_wstg[k]], w=[R_win])
                for dch in range(8):
                    k = dch % 2
                    S.dma("sp", "wstg%d" % k, wstg[k][:, 0:D], wout_in[l][dch * 128:(dch + 1) * 128, :], w=[R_wstg[k]])
                    eng = cast_eng[dch % 3]
                    if eng == "act":
                        S.op("act", ACTF(woutb[:, dch, :], wstg[k][:, 0:D], AF.Copy), r=[R_wstg[k]], w=[R_wout])
                    else:
                        S.op(eng, CP(woutb[:, dch, :], wstg[k][:, 0:D]), r=[R_wstg[k]], w=[R_wout])
                S.op("act", ACTF(negA[:], P[:, P_ALOG:P_ALOG + 8], AF.Exp), r=[R_c], w=[R_lc])
                S.op("dve", TS(negA[:], negA[:], -1.0), r=[R_lc], w=[R_lc])
                S.op("act", ACTF(esink[:], P[:, P_SK:P_SK + 8], AF.Exp), r=[R_c], w=[R_lc])
                S.op("pool", CP(Dexp[:], P[:, P_DSK:P_DSK + 8].unsqueeze(2).broadcast_to([128, 8, 64])), r=[R_c], w=[R_lc])
                S.op("dve", TT(dgw[:], ident_b.unsqueeze(1).broadcast_to([128, 32, 128]),
                               P[:, P_CW:P_CW + 32].unsqueeze(2).broadcast_to([128, 32, 128]), ALU.mult), r=[R_c], w=[R_lc])
                S.op("pool", MEMSET(hst[:], 0.0), w=[R_hst])
                S.op("pool", MEMSET(hstb[:], 0.0), w=[R_hstb])
                for i in range(4):
                    S.op("pool", MEMSET(vaug[i][:], 1.0), w=[R_va[i]])
                    S.op("pool", MEMSET(kT[i][:], 0.0), w=[R_kT[i]])
                for i in range(2):
                    S.op("pool", MEMSET(xpre[i][:], 0.0), w=[R_xp[i]])

                def load(i):
                    par = i % 2
                    S.dma("sp", "xin%d" % (i % XD), xin[i % XD][:], xsrc[i * 128:(i + 1) * 128, :], r=R_src, w=[R_xin[i % XD]])
                    S.dma("sp", "cs%d" % par, cs[par][:, 0, :], cos_d[:, i * 128:(i + 1) * 128], r=R_tab, w=[R_cs[par]])
                    S.dma("sp", "cs%d" % par, cs[par][:, 1, :], sin_d[:, i * 128:(i + 1) * 128], r=R_tab, w=[R_cs[par]])

                def stageA(i):
                    par = i % 2
                    if i + 1 < NB:
                        load(i + 1)
                    X = xin[i % XD]
                    yield S.op("act", ACTF(junk[:], X[:], AF.Square, accum_out=ss[:]), r=[R_xin[i % XD]], w=[R_junk, R_ss])
                    yield S.op("act", ACTF(lnv[:], ss[:], AF.Ln, bias=epsD[:], scale=1.0 / D), r=[R_ss, R_c2], w=[R_rstd])
                    yield S.op("act", ACTF(rstd[:], lnv[:], AF.Exp, scale=-0.5), r=[R_rstd], w=[R_rstd])
                    yield S.op("act", ACTF(xn[:], X[:], AF.Identity, scale=rstd[:]), r=[R_xin[i % XD], R_rstd], w=[R_xn])
                    pa, ra = yield from PSG()
                    pb, rb = yield from PSG()
                    yield S.op("pe", TRS([(pa[:, j * 128:(j + 1) * 128], xn[:, j * 128:(j + 1) * 128], ident_f) for j in range(4)]),
                         r=[R_xn, R_c], w=[ra])
                    yield S.op("pe", TRS([(pb[:, j * 128:(j + 1) * 128], xn[:, (4 + j) * 128:(5 + j) * 128], ident_f) for j in range(4)]),
                         r=[R_xn, R_c], w=[rb])
                    yield S.op("act", ACTS([(hT[:, j, :], pa[:, j * 128:(j + 1) * 128], AF.Identity,
                                       dict(scale=gsm[l][:, j:j + 1], bias=modc[l][:, j:j + 1])) for j in range(4)]),
                         r=[R_mod], w=[ra, R_hT])
                    yield S.op("act", ACTS([(hT[:, 4 + j, :], pb[:, j * 128:(j + 1) * 128], AF.Identity,
                                       dict(scale=gsm[l][:, 4 + j:5 + j], bias=modc[l][:, 4 + j:5 + j])) for j in range(4)]),
                         r=[R_mod], w=[rb, R_hT])

                    def fm_group(ptile, cols):
                        lst = []
                        for j, c0 in enumerate(cols):
                            for dch in range(8):
                                lst.append((ptile[:, j * 128:(j + 1) * 128], winb[:, dch, c0:c0 + 128], hT[:, dch, :], dch == 0, dch == 7))
                        return MMS(lst)
                    pq, rq = yield from PSG()
                    yield S.op("pe", fm_group(pq, [C_Q + 128 * j for j in range(4)]), r=[R_win, R_hT], w=[rq])
                    pqs, rqs = yield from PSG()
                    yield S.op("pe", fm_group(pqs, [C_QS + 128 * j for j in range(4)]), r=[R_win, R_hT], w=[rqs])
                    pk, rk = yield from PSG(2)
                    yield S.op("pe", fm_group(pk, [C_K, C_KS]), r=[R_win, R_hT], w=[rk])
                    cosb = cs[par][:, 0:1, :].broadcast_to([128, 4, 128])
                    sinb = cs[par][:, 1:2, :].broadcast_to([128, 4, 128])
                    pq3 = pq[:, :].rearrange("p (c t) -> p c t", c=4)
                    pqs3 = pqs[:, :].rearrange("p (c t) -> p c t", c=4)
                    yield S.op("dve", TT(rt1[:], pq3, cosb, ALU.mult), r=[R_cs[par]], w=[rq, R_rt1])
                    yield S.op("dve", TT(rt2[:], pqs3, sinb, ALU.mult), r=[R_cs[par]], w=[rqs, R_rt2])
                    yield S.op("dve", TT(qT[i % 3][:], rt1[:], rt2[:], ALU.add), r=[R_rt1, R_rt2], w=[R_qT[i % 3]])
                    yield S.op("dve", TT(rt1[:, 0, :], pk[:, 0:128], cs[par][:, 0, :], ALU.mult), r=[R_cs[par]], w=[rk, R_rt1])
                    yield S.op("dve", TT(rt2[:, 0, :], pk[:, 128:256], cs[par][:, 1, :], ALU.mult), r=[R_cs[par]], w=[rk, R_rt2])
                    yield S.op("dve", TT(kT[i % 4][:], rt1[:, 0, :], rt2[:, 0, :], ALU.add), r=[R_rt1, R_rt2], w=[R_kT[i % 4]])
                    pxa, rxa = yield from PSG()
                    yield S.op("pe", fm_group(pxa, [C_X + 128 * j for j in range(4)]), r=[R_win, R_hT], w=[rxa])
                    pxb, rxb = yield from PSG()
                    yield S.op("pe", fm_group(pxb, [C_X + 128 * (4 + j) for j in range(4)]), r=[R_win, R_hT], w=[rxb])
                    pv, rv = yield from PSG(2)
                    yield S.op("pe", MMS([(pv[:, 0:128], hT[:, dch, :], winb[:, dch, C_V:C_V + 128], dch == 0, dch == 7) for dch in range(8)]
                                   + [(pv[:, 128:136], hT[:, dch, :], winb[:, dch, C_DT:C_DT + 8], dch == 0, dch == 7) for dch in range(8)]),
                         r=[R_win, R_hT], w=[rv])
                    pz, rz = yield from PSG()
                    yield S.op("pe", MMS([(pz[:, :], hT[:, dch, :], winb[:, dch, C_Z:C_Z + 512], dch == 0, dch == 7) for dch in range(8)]),
                         r=[R_win, R_hT], w=[rz])
                    yield S.op("act", ACTF(vaug[i % 4][:, :, 0:64], pv[:, 0:128].rearrange("p (g d) -> p g d", g=2), AF.Copy), w=[rv, R_va[i % 4]])
                    yield S.op("dve", TT(xdt[:], pv[:, 128:136], P[:, P_DTB:P_DTB + 8], ALU.add), r=[R_c], w=[rv, R_xdt])
                    yield S.op("act", ACTF(sp1[:], xdt[:], AF.Abs), r=[R_xdt], w=[R_sp1])
                    yield S.op("act", ACTF(sp1[:], sp1[:], AF.Exp, scale=-1.0), r=[R_sp1], w=[R_sp1])
                    yield S.op("act", ACTF(sp1[:], sp1[:], AF.Ln, bias=1.0), r=[R_sp1], w=[R_sp1])
                    yield S.op("dve", STT(dt[i % 3][:], xdt[:], 0.0, sp1[:], ALU.max, ALU.add), r=[R_xdt, R_sp1], w=[R_dt[i % 3]])
                    yield S.op("dve", TT(dtA[i % 3][:], dt[i % 3][:], negA[:], ALU.mult), r=[R_dt[i % 3], R_lc], w=[R_dtA[i % 3]])
                    yield S.op("act", ACTF(xpre[par][:, 0:4, 3:131], pxa[:, :].rearrange("p (c t) -> p c t", c=4), AF.Copy), w=[rxa, R_xp[par]])
                    yield S.op("act", ACTF(xpre[par][:, 4:8, 3:131], pxb[:, :].rearrange("p (c t) -> p c t", c=4), AF.Copy), w=[rxb, R_xp[par]])
                    yield S.op("act", ACTF(sz[i % 4][:], pz[:, :], AF.Silu), w=[rz, R_sz[i % 4]])

                def stageA2(i):
                    par = i % 2
                    for hf in range(2):
                        pcv, rcv = yield from PSG()
                        yield S.op("pe", MMS([(pcv[:, j * 128:(j + 1) * 128], dgw[:, (hf * 4 + j) * 4 + k_, :], xpre[par][:, hf * 4 + j, k_:k_ + 128],
                                               k_ == 0, k_ == 3) for j in range(4) for k_ in range(4)]), r=[R_xp[par], R_lc], w=[rcv])
                        if hf == 1:
                            yield S.op("pool", CP(xpre[1 - par][:, :, 0:3], xpre[par][:, :, 128:131]), r=[R_xp[par]], w=[R_xp[1 - par]])
                        yield S.op("act", ACTS([(xbcT[i % 3][:, hf * 4 + j, :], pcv[:, j * 128:(j + 1) * 128], AF.Silu,
                                                 dict(bias=P[:, P_CB + hf * 4 + j:P_CB + hf * 4 + j + 1])) for j in range(4)]),
                             r=[R_c], w=[rcv, R_xbcT[i % 3]])

                def stageB(i):
                    par = i % 2
                    q2 = qT[i % 3][:].rearrange("p c t -> p (c t)")
                    for g in range(2):
                        gs_ = slice(g * 64, (g + 1) * 64)
                        if i > 0:
                            pp, rp = yield from PSG()
                            yield S.op("pe", MMS([(pp[:, :], kT[(i - 1) % 4][gs_, :], q2[gs_, :], True, False),
                                            (pp[:, :], ident_b, mprev_b, False, True)]),
                                 r=[R_kT[(i - 1) % 4], R_qT[i % 3], R_c], w=[rp])
                            yield S.op("act", ACTF(pTp[g][:], pp[:, :], AF.Exp, scale=0.125), w=[rp, R_pTp[g]])
                        pc, rc = yield from PSG()
                        yield S.op("pe", MMS([(pc[:, :], kT[i % 4][gs_, :], q2[gs_, :], True, False),
                                        (pc[:, :], ident_b, mcur_b, False, True)]),
                             r=[R_kT[i % 4], R_qT[i % 3], R_c], w=[rc])
                        yield S.op("act", ACTF(pTc[g][:], pc[:, :], AF.Exp, scale=0.125), w=[rc, R_pTc[g]])
                    for g in range(2):
                        po, ro = yield from PSG(2)
                        lst = []
                        for j in range(4):
                            o_ = po[:, j * 65:(j + 1) * 65]
                            if i > 0:
                                lst.append((o_, pTp[g][:, j * 128:(j + 1) * 128], vaug[(i - 1) % 4][:, g, :], True, False))
                                lst.append((o_, pTc[g][:, j * 128:(j + 1) * 128], vaug[i % 4][:, g, :], False, True))
                            else:
                                lst.append((o_, pTc[g][:, j * 128:(j + 1) * 128], vaug[i % 4][:, g, :], True, True))
                        yield S.op("pe", MMS(lst), r=[R_pTp[g], R_pTc[g], R_va[i % 4], R_va[(i - 1) % 4]], w=[ro])
                        po3 = po[:, 0:260].rearrange("p (h d) -> p h d", h=4)
                        yield S.op("dve", TT(den[:, g * 4:(g + 1) * 4], po3[:, :, 64], esink[:, g * 4:(g + 1) * 4], ALU.add), r=[R_lc], w=[ro, R_den])
                        yield S.op("dve", lambda e, g=g: e.reciprocal(out=den[:, g * 4:(g + 1) * 4], in_=den[:, g * 4:(g + 1) * 4]), r=[R_den], w=[R_den])
                        yield S.op("dve", TT(attn[i % 3][:, g * 4:(g + 1) * 4, :], po3[:, :, 0:64],
                                       den[:, g * 4:(g + 1) * 4].unsqueeze(2).broadcast_to([128, 4, 64]), ALU.mult),
                             r=[R_den], w=[ro, R_attn[i % 3]])

                def stageB2(i):
                    par = i % 2
                    ptx, rtx = yield from PSG(3)
                    ptxb = ptx[:, :].bitcast(BF16)
                    yield S.op("pe", TRS([(ptxb[:, j * 128:(j + 1) * 128], xbcT[i % 3][:, j, :], ident_b) for j in range(6)]), r=[R_xbcT[i % 3], R_c], w=[rtx])
                    yield S.op("dve", TT(xd[par][:], ptxb[:, 0:512].rearrange("p (h d) -> p h d", h=8),
                                   dt[i % 3][:].unsqueeze(2).broadcast_to([128, 8, 64]), ALU.mult), r=[R_dt[i % 3]], w=[rtx, R_xd[par]])
                    yield S.op("act", ACTF(xst[par][:], ptxb[:, 0:512], AF.Copy), w=[rtx, R_xst[par]])
                    yield S.op("act", ACTF(Btok[par][:], ptxb[:, 512:768].rearrange("p (g n) -> p g n", g=2), AF.Copy), w=[rtx, R_Bt[par]])
                    pac, rac = yield from PSG()
                    yield S.op("pe", MMS([(pac[:, 0:8], U_b, dtA[i % 3][:], True, True), (pac[:, 8:16], ones_b, dtA[i % 3][:], True, True)]),
                         r=[R_dtA[i % 3], R_c], w=[rac])
                    yield S.op("act", ACTF(acs[:], pac[:, 0:16], AF.Copy), w=[rac, R_acs])
                    yield S.op("dve", TT(dif[par][:], acs[:, 8:16], acs[:, 0:8], ALU.subtract), r=[R_acs], w=[R_sm[par]])
                    yield S.op("dve", TS(nacs[par][:], acs[:, 0:8], -1.0), r=[R_acs], w=[R_sm[par]])
                    yield S.op("act", ACTF(dte[par][:], dif[par][:], AF.Exp), r=[R_sm[par]], w=[R_sm[par]])
                    yield S.op("act", ACTF(cdec[par][:], acs[:, 8:16], AF.Exp), r=[R_acs], w=[R_sm[par]])
                    yield S.op("act", ACTF(ea[par][:], acs[:, 0:8], AF.Exp), r=[R_acs], w=[R_sm[par]])
                    yield S.op("pool", TT(udta[:], U_b.unsqueeze(1).broadcast_to([128, 8, 128]),
                                    dtA[i % 3][:].unsqueeze(2).broadcast_to([128, 8, 128]), ALU.mult), r=[R_dtA[i % 3], R_c], w=[R_udta])
                    u2 = udta[:].rearrange("p h t -> p (h t)")
                    for hf in range(2):
                        pd, rd = yield from PSG()
                        yield S.op("pe", MMS([(pd[:, :], ones_b, u2[:, hf * 512:(hf + 1) * 512], True, False),
                                        (pd[:, :], ident_b, mcur_b, False, True)]), r=[R_udta, R_c], w=[rd])
                        yield S.op("act", ACTS([(dec[:, hf * 4 + j, :], pd[:, j * 128:(j + 1) * 128], AF.Exp,
                                           dict(bias=nacs[par][:, hf * 4 + j:hf * 4 + j + 1])) for j in range(4)]), r=[R_sm[par]], w=[rd, R_dec])
                    pcb, rcb = yield from PSG()
                    yield S.op("pe", MMS([(pcb[:, g * 128:(g + 1) * 128], xbcT[i % 3][:, 4 + g, :], xbcT[i % 3][:, 6 + g, :], True, True) for g in range(2)]),
                         r=[R_xbcT[i % 3]], w=[rcb])
                    yield S.op("act", ACTF(cbs[:], pcb[:, 0:256].rearrange("p (g t) -> p g t", g=2), AF.Copy), w=[rcb, R_cbs])
                    yield S.op("dve", TT(mt[par][:].rearrange("p (g r) t -> p g r t", g=2), dec[:].rearrange("p (g r) t -> p g r t", g=2),
                                    cbs[:].unsqueeze(2).broadcast_to([128, 2, 4, 128]), ALU.mult), r=[R_dec, R_cbs], w=[R_mt[par]])
                def stageB2b(i):
                    par = i % 2
                    py, ry = yield from PSG()
                    yield S.op("pe", MMS([(py[:, h * 64:(h + 1) * 64], mt[par][:, h, :], xd[par][:, h, :], True, True) for h in range(8)]),
                         r=[R_mt[par], R_xd[par]], w=[ry])
                    pyo, ryo = yield from PSG()
                    yield S.op("pe", MMS([(pyo[:, g * 256:(g + 1) * 256], xbcT[i % 3][:, 6 + g, :], hstb[:, g * 256:(g + 1) * 256], True, True) for g in range(2)]),
                         r=[R_xbcT[i % 3], R_hstb], w=[ryo])
                    yield S.op("pool", TT(xdd[:], xd[par][:], dte[par][:].unsqueeze(2).broadcast_to([128, 8, 64]), ALU.mult), r=[R_xd[par], R_sm[par]], w=[R_xdd])
                    x2 = xdd[:].rearrange("p h d -> p (h d)")
                    pst, rst = yield from PSG()
                    yield S.op("pe", MMS([(pst[:, g * 256:(g + 1) * 256], Btok[par][:, g, :], x2[:, g * 256:(g + 1) * 256], True, True) for g in range(2)]),
                         r=[R_Bt[par], R_xdd], w=[rst])
                    yield S.op("dve", TT(htmp[:].rearrange("p (h d) -> p h d", h=8), hst[:].rearrange("p (h d) -> p h d", h=8),
                                    cdec[par][:].unsqueeze(2).broadcast_to([128, 8, 64]), ALU.mult), r=[R_hst, R_sm[par]], w=[R_htmp])
                    yield S.op("dve", TT(hst[:], htmp[:], pst[:, :], ALU.add), r=[R_htmp], w=[rst, R_hst])
                    yield S.op("dve", TT(y1[:].rearrange("p (h d) -> p h d", h=8), pyo[:, :].rearrange("p (h d) -> p h d", h=8),
                                   ea[par][:].unsqueeze(2).broadcast_to([128, 8, 64]), ALU.mult), r=[R_sm[par]], w=[ryo, R_y1])
                    yield S.op("act", ACTF(hstb[:], hst[:], AF.Copy), r=[R_hst], w=[R_hstb])
                    yield S.op("dve", TT(y1[:], y1[:], py[:, :], ALU.add), r=[R_y1], w=[ry, R_y1])
                    yield S.op("pool", TT(y2[:], xst[par][:], Dexp[:].rearrange("p h d -> p (h d)"), ALU.mult), r=[R_xst[par], R_lc], w=[R_y2])
                    yield S.op("dve", TT(y2[:], y2[:], y1[:], ALU.add), r=[R_y1, R_y2], w=[R_y2])
                    yield S.op("dve", TT(y1[:], y2[:], sz[i % 4][:], ALU.mult), r=[R_y2, R_sz[i % 4]], w=[R_y1])
                    yield S.op("act", ACTS([(junk[:, g * 256:(g + 1) * 256], y1[:, g * 256:(g + 1) * 256], AF.Square,
                                       dict(accum_out=ssg[:, g:g + 1])) for g in range(2)]), r=[R_y1], w=[R_junk, R_ssg])
                    yield S.op("act", ACTF(lng[:], ssg[:], AF.Ln, bias=epsG[:], scale=1.0 / 256), r=[R_ssg, R_c2], w=[R_ssg])
                    yield S.op("act", ACTF(rg[:], lng[:], AF.Exp, scale=-0.5), r=[R_ssg], w=[R_ssg])
                    yield S.op("dve", TT(y2[:].rearrange("p (g c) -> p g c", g=2), y1[:].rearrange("p (g c) -> p g c", g=2),
                                   rg[:].unsqueeze(2).broadcast_to([128, 2, 256]), ALU.mult), r=[R_y1, R_ssg], w=[R_y2])
                    yield S.op("dve", TT(ssm[par][:], y2[:], P[:, P_SSMG:P_SSMG + 512], ALU.mult), r=[R_y2, R_c], w=[R_ssm[par]])

                def stageC(i):
                    par = i % 2
                    X = xin[i % XD]
                    pm, rm = yield from PSG()
                    pmb = pm[:, :].bitcast(BF16)
                    a2 = attn[i % 3][:].rearrange("p h d -> p (h d)")
                    yield S.op("pe", TRS([(pmb[:, j * 128:(j + 1) * 128], a2[:, j * 128:(j + 1) * 128], ident_b) for j in range(4)]
                                   + [(pmb[:, (4 + j) * 128:(5 + j) * 128], ssm[par][:, j * 128:(j + 1) * 128], ident_b) for j in range(4)]),
                         r=[R_attn[i % 3], R_ssm[par], R_c], w=[rm])
                    yield S.op("act", ACTF(mixT[:].rearrange("p c t -> p (c t)"), pmb[:, :], AF.Copy), w=[rm, R_mixT])
                    for hf in range(2):
                        pop, rop = yield from PSG()
                        yield S.op("pe", MMS([(pop[:, :], mixT[:, fch, :], woutb[:, fch, hf * 512:(hf + 1) * 512], fch == 0, fch == 7) for fch in range(8)]),
                             r=[R_mixT, R_wout], w=[rop])
                        yield S.op("dve", TT(xo[:, hf * 512:(hf + 1) * 512], pop[:, :], gmrow[l][:, hf * 512:(hf + 1) * 512], ALU.mult),
                             r=[R_mod], w=[rop, R_xo])
                    yield S.op("dve", TT(X[:], X[:], xo[:], ALU.add), r=[R_xo, R_xin[i % XD]], w=[R_xin[i % XD]])
                    yield S.dma("sp", "xst%d" % (i % XD), xdst[i * 128:(i + 1) * 128, :], X[:], r=[R_xin[i % XD]], w=[R_dst[par]])
                load(0)
                for t in range(NB + 4):
                    gens = []
                    if t < NB:
                        gens.append((stageA(t), 1))
                    if 0 <= t - 1 < NB:
                        gens.append((stageA2(t - 1), 1))
                    if 0 <= t - 2 < NB:
                        gens.append((stageB(t - 2), 1))
                        gens.append((stageB2(t - 2), 1))
                    if 0 <= t - 3 < NB:
                        gens.append((stageB2b(t - 3), 1))
                    if 0 <= t - 4 < NB:
                        gens.append((stageC(t - 4), 1))
                    while gens:
                        for it_ in list(gens):
                            g_, w_ = it_
                            try:
                                for _ in range(w_):
                                    next(g_)
                            except StopIteration:
                                gens.remove(it_)

        def chanmix(l, xsrc, R_src, xdst, R_dst, moe, final, ntok, blend):
            P = lp[l]
            NT = ntok // TTOK
            NF = (EXD if moe else FFN) // 128
            NE = NEXP if moe else 1
            GF = 2
            with ExitStack() as me:
                xt = sbt(me, "xt", [128, NBT, D]); R_xt = S.res()
                hTt = sbt(me, "hTt", [128, 8, TTOK], BF16); R_hTt = S.res()
                xn = [sbt(me, "cxn%d" % i, [128, D]) for i in range(2)]; R_xn = [S.res() for _ in range(2)]
                junk = sbt(me, "cjunk", [128, D], BF16); R_junk = S.res()
                ss = sbt(me, "css", [128, 1]); lnv = sbt(me, "clnv", [128, 1]); rstd = sbt(me, "crstd", [128, 1]); R_ss = S.res()
                ss3 = [sbt(me, "css3_%d" % i, [128, 1]) for i in range(3)]; lnv3 = [sbt(me, "clnv3_%d" % i, [128, 1]) for i in range(3)]
                rstd3 = [sbt(me, "crstd3_%d" % i, [128, 1]) for i in range(3)]; R_ss3 = [S.res() for _ in range(3)]
                xn3 = [sbt(me, "cxn3_%d" % i, [128, D]) for i in range(3)]; R_xn3 = [S.res() for _ in range(3)]
                actT = [sbt(me, "actT%d" % i, [128, GF, TTOK], BF16) for i in range(2)]; R_actT = [S.res() for _ in range(2)]
                sg = [sbt(me, "sg%d" % i, [128, 512], BF16) for i in range(2)]; R_sg = [S.res() for _ in range(2)]
                NSLOT = 3 * GF
                gstg = [sbt(me, "gstg%d" % i, [128, 8, 128]) for i in range(3)]; R_gstg = [S.res() for _ in range(3)]
                ustg = [sbt(me, "ustg%d" % i, [128, 8, 128]) for i in range(3)]; R_ustg = [S.res() for _ in range(3)]
                dstg = [sbt(me, "dstg%d" % i, [128, D]) for i in range(3)]; R_dstg = [S.res() for _ in range(3)]
                wgb = [sbt(me, "wgb%d" % i, [128, 8, 128], BF16) for i in range(NSLOT)]; R_wgb = [S.res() for _ in range(NSLOT)]
                wub = [sbt(me, "wub%d" % i, [128, 8, 128], BF16) for i in range(NSLOT)]; R_wub = [S.res() for _ in range(NSLOT)]
                wdb = [sbt(me, "wdb%d" % i, [128, D], BF16) for i in range(NSLOT)]; R_wdb = [S.res() for _ in range(NSLOT)]
                if moe:
                    hTf = sbt(me, "hTf", [128, 8, 128]); R_hTf = S.res()
                    lg = sbt(me, "lg", [128, 8]); top8 = sbt(me, "top8", [128, 8]); R_lg = S.res()
                    gt = sbt(me, "gt", [128, 4]); R_gt = S.res()
                    m1 = sbt(me, "m1", [128, 8]); m2 = sbt(me, "m2", [128, 8]); R_m = S.res()
                    wgt = sbt(me, "wgt", [128, NBT, 8]); R_wgt = S.res()
                if blend:
                    xB = [sbt(me, "xB%d" % i, [128, D]) for i in range(2)]; R_xB = [S.res() for _ in range(2)]
                if final:
                    fng = sbt(me, "fng", [128, D]); R_fng = S.res()
                    S.dma("sp", "fng", fng[:], fng_in, w=[R_fng])
                    ob = [sbt(me, "ob%d" % i, [128, D]) for i in range(2)]; R_ob = [S.res() for _ in range(2)]
                slot = [0]
                stgc = [0]
                castc = [0]
                cast_eng = ["pool", "act", "pool", "dve"]
                for t in range(NT):
                    def cblk(b):
                        k = b % 3
                        row0 = t * TTOK + b * 128
                        tc = slice(b * 128, (b + 1) * 128)
                        S.dma("sp", "cxt%d" % b, xt[:, b, :], xsrc[row0:row0 + 128, :], r=R_src, w=[R_xt])
                        yield None
                        yield S.op("act", ACTF(junk[:], xt[:, b, :], AF.Square, accum_out=ss3[k][:]), r=[R_xt], w=[R_junk, R_ss3[k]])
                        yield S.op("act", ACTF(lnv3[k][:], ss3[k][:], AF.Ln, bias=epsD[:], scale=1.0 / D), r=[R_ss3[k], R_c2], w=[R_ss3[k]])
                        yield S.op("act", ACTF(rstd3[k][:], lnv3[k][:], AF.Exp, scale=-0.5), r=[R_ss3[k]], w=[R_ss3[k]])
                        yield S.op("act", ACTF(xn3[k][:], xt[:, b, :], AF.Identity, scale=rstd3[k][:]), r=[R_xt, R_ss3[k]], w=[R_xn3[k]])
                        pa, ra = yield from PSG()
                        pb, rb = yield from PSG()
                        yield S.op("pe", TRS([(pa[:, j * 128:(j + 1) * 128], xn3[k][:, j * 128:(j + 1) * 128], ident_f) for j in range(4)]),
                             r=[R_xn3[k], R_c], w=[ra])
                        yield S.op("pe", TRS([(pb[:, j * 128:(j + 1) * 128], xn3[k][:, (4 + j) * 128:(5 + j) * 128], ident_f) for j in range(4)]),
                             r=[R_xn3[k], R_c], w=[rb])
                        yield S.op("act", ACTS([(hTt[:, j, tc], pa[:, j * 128:(j + 1) * 128], AF.Identity,
                                           dict(scale=gsf[l][:, j:j + 1], bias=modc[l][:, 24 + j:25 + j])) for j in range(4)]),
                             r=[R_mod], w=[ra, R_hTt])
                        yield S.op("act", ACTS([(hTt[:, 4 + j, tc], pb[:, j * 128:(j + 1) * 128], AF.Identity,
                                           dict(scale=gsf[l][:, 4 + j:5 + j], bias=modc[l][:, 28 + j:29 + j])) for j in range(4)]),
                             r=[R_mod], w=[rb, R_hTt])
                    fast = (not moe) and (not blend)
                    if fast:
                        act_g = []
                        nxt = 0
                        while nxt < NBT or act_g:
                            while len(act_g) < 3 and nxt < NBT:
                                act_g.append(cblk(nxt))
                                nxt += 1
                            for g_ in list(act_g):
                                try:
                                    next(g_)
                                except StopIteration:
                                    act_g.remove(g_)
                    for b in (range(0) if fast else range(NBT)):
                        row0 = t * TTOK + b * 128
                        k = b % 2
                        S.dma("sp", "cxt%d" % b, xt[:, b, :], xsrc[row0:row0 + 128, :], r=R_src, w=[R_xt])
                        if blend:
                            S.dma("sp", "cxB%d" % k, xB[k][:], xsrc[ntok + row0:ntok + row0 + 128, :], r=R_src, w=[R_xB[k]])
                            S.op("dve", TS(xB[k][:], xB[k][:], selt[:, 1:2]), r=[R_xB[k], R_c], w=[R_xB[k]])
                            S.op("dve", STT(xt[:, b, :], xt[:, b, :], selt[:, 0:1], xB[k][:], ALU.mult, ALU.add), r=[R_xB[k], R_c, R_xt], w=[R_xt])
                        S.op("act", ACTF(junk[:], xt[:, b, :], AF.Square, accum_out=ss[:]), r=[R_xt], w=[R_junk, R_ss])
                        S.op("act", ACTF(lnv[:], ss[:], AF.Ln, bias=epsD[:], scale=1.0 / D), r=[R_ss, R_c2], w=[R_ss])
                        S.op("act", ACTF(rstd[:], lnv[:], AF.Exp, scale=-0.5), r=[R_ss], w=[R_ss])
                        S.op("act", ACTF(xn[k][:], xt[:, b, :], AF.Identity, scale=rstd[:]), r=[R_xt, R_ss], w=[R_xn[k]])
                        pa, ra = PS()
                        pb, rb = PS()
                        S.op("pe", TRS([(pa[:, j * 128:(j + 1) * 128], xn[k][:, j * 128:(j + 1) * 128], ident_f) for j in range(4)]),
                             r=[R_xn[k], R_c], w=[ra])
                        S.op("pe", TRS([(pb[:, j * 128:(j + 1) * 128], xn[k][:, (4 + j) * 128:(5 + j) * 128], ident_f) for j in range(4)]),
                             r=[R_xn[k], R_c], w=[rb])
                        tc = slice(b * 128, (b + 1) * 128)
                        if not moe:
                            S.op("act", ACTS([(hTt[:, j, tc], pa[:, j * 128:(j + 1) * 128], AF.Identity,
                                               dict(scale=gsf[l][:, j:j + 1], bias=modc[l][:, 24 + j:25 + j])) for j in range(4)]),
                                 r=[R_mod], w=[ra, R_hTt])
                            S.op("act", ACTS([(hTt[:, 4 + j, tc], pb[:, j * 128:(j + 1) * 128], AF.Identity,
                                               dict(scale=gsf[l][:, 4 + j:5 + j], bias=modc[l][:, 28 + j:29 + j])) for j in range(4)]),
                                 r=[R_mod], w=[rb, R_hTt])
                        else:
                            S.op("act", ACTS([(hTf[:, j, :], pa[:, j * 128:(j + 1) * 128], AF.Identity,
                                               dict(scale=gsf[l][:, j:j + 1], bias=modc[l][:, 24 + j:25 + j])) for j in range(4)]),
                                 r=[R_mod], w=[ra, R_hTf])
                            S.op("act", ACTS([(hTf[:, 4 + j, :], pb[:, j * 128:(j + 1) * 128], AF.Identity,
                                               dict(scale=gsf[l][:, 4 + j:5 + j], bias=modc[l][:, 28 + j:29 + j])) for j in range(4)]),
                                 r=[R_mod], w=[rb, R_hTf])
                            S.op("pool", CP(hTt[:, :, tc], hTf[:]), r=[R_hTf], w=[R_hTt])
                            pr_, rr_ = PS()
                            S.op("pe", MMS([(pr_[:, 0:8], hTf[:, dch, :], P[:, P_RW + dch * 8:P_RW + dch * 8 + 8], dch == 0, dch == 7) for dch in range(8)]),
                                 r=[R_hTf, R_c], w=[rr_])
                            S.op("dve", CP(lg[:], pr_[:, 0:8]), w=[rr_, R_lg])
                            S.op("dve", lambda e: e.max(out=top8[:], in_=lg[:]), r=[R_lg], w=[R_lg])
                            S.op("dve", TT(gt[:, 0:1], top8[:, 1:2], top8[:, 0:1], ALU.subtract), r=[R_lg], w=[R_gt])
                            S.op("act", ACTF(gt[:, 1:2], gt[:, 0:1], AF.Exp), r=[R_gt], w=[R_gt])
                            S.op("dve", TS(gt[:, 1:2], gt[:, 1:2], 1.0, None, ALU.add), r=[R_gt], w=[R_gt])
                            S.op("dve", lambda e: e.reciprocal(out=gt[:, 2:3], in_=gt[:, 1:2]), r=[R_gt], w=[R_gt])
                            S.op("dve", TS(gt[:, 3:4], gt[:, 2:3], -1.0, 1.0, ALU.mult, ALU.add), r=[R_gt], w=[R_gt])
                            S.op("dve", TS(m1[:], lg[:], top8[:, 0:1], gt[:, 2:3], ALU.is_equal, ALU.mult), r=[R_lg, R_gt], w=[R_m])
                            S.op("dve", TS(m2[:], lg[:], top8[:, 1:2], gt[:, 3:4], ALU.is_equal, ALU.mult), r=[R_lg, R_gt], w=[R_m])
                            S.op("dve", TT(wgt[:, b, :], m1[:], m2[:], ALU.add), r=[R_m], w=[R_wgt])
                    for e_ in range(NE):
                        if moe:
                            Wg, Wu, Wd = mg_in[e_], mu_in[e_], md_in[e_]
                        else:
                            Wg, Wu, Wd = fg_in, fu_in, fd_in
                        for fg in range(NF // GF):
                            ab = fg % 2
                            slots = []
                            for fi in range(GF):
                                fc = fg * GF + fi
                                s_ = slot[0] % NSLOT
                                slot[0] += 1
                                slots.append(s_)
                                k = stgc[0] % 3
                                stgc[0] += 1
                                S.dma("sp", "gstg%d" % k, gstg[k][:], Wg[:, fc * 128:(fc + 1) * 128].rearrange("(c p) n -> p c n", p=128), w=[R_gstg[k]])
                                S.dma("sp", "ustg%d" % k, ustg[k][:], Wu[:, fc * 128:(fc + 1) * 128].rearrange("(c p) n -> p c n", p=128), w=[R_ustg[k]])
                                S.dma("sp", "dstg%d" % k, dstg[k][:], Wd[fc * 128:(fc + 1) * 128, :], w=[R_dstg[k]])
                                for (src_, R_s, dst_, R_d) in ((gstg[k], R_gstg[k], wgb[s_], R_wgb[s_]), (ustg[k], R_ustg[k], wub[s_], R_wub[s_])):
                                    ce = cast_eng[castc[0] % 4]
                                    castc[0] += 1
                                    if ce == "act":
                                        S.op("act", ACTF(dst_[:], src_[:], AF.Copy), r=[R_s], w=[R_d])
                                    else:
                                        S.op(ce, CP(dst_[:], src_[:]), r=[R_s], w=[R_d])
                                S.op("pool", TT(wdb[s_][:], dstg[k][:], gfrow[l][:], ALU.mult), r=[R_dstg[k], R_mod], w=[R_wdb[s_]])
                                SW = min(512, TTOK)
                                for sub in range(TTOK // SW):
                                    tcs = slice(sub * SW, (sub + 1) * SW)
                                    pg, rg_ = PS()
                                    S.op("pe", MMS([(pg[:, 0:SW], wgb[s_][:, dch, :], hTt[:, dch, tcs], dch == 0, dch == 7) for dch in range(8)]),
                                         r=[R_wgb[s_], R_hTt], w=[rg_])
                                    pu, ru = PS()
                                    S.op("pe", MMS([(pu[:, 0:SW], wub[s_][:, dch, :], hTt[:, dch, tcs], dch == 0, dch == 7) for dch in range(8)]),
                                         r=[R_wub[s_], R_hTt], w=[ru])
                                    kk = sub % 2
                                    S.op("act", ACTF(sg[kk][:, 0:SW], pg[:, 0:SW], AF.Silu), w=[rg_, R_sg[kk]])
                                    S.op("dve", TT(actT[ab][:, fi, tcs], sg[kk][:, 0:SW], pu[:, 0:SW], ALU.mult), r=[R_sg[kk]], w=[ru, R_actT[ab]])
                            for b in range(NBT):
                                for hf in range(2):
                                    pd, rd = PS()
                                    S.op("pe", MMS([(pd[:, :], actT[ab][:, fi, b * 128:(b + 1) * 128], wdb[slots[fi]][:, hf * 512:(hf + 1) * 512],
                                                     fi == 0, fi == GF - 1) for fi in range(GF)]),
                                         r=[R_actT[ab]] + [R_wdb[s_] for s_ in slots], w=[rd])
                                    xs_ = xt[:, b, hf * 512:(hf + 1) * 512]
                                    if moe:
                                        S.op("dve", STT(xs_, pd[:, :], wgt[:, b, e_:e_ + 1], xs_, ALU.mult, ALU.add), r=[R_wgt], w=[rd, R_xt])
                                    else:
                                        S.op("dve", TT(xs_, pd[:, :], xs_, ALU.add), w=[rd, R_xt])
                    for b in range(NBT):
                        row0 = t * TTOK + b * 128
                        if final:
                            k = b % 2
                            S.op("act", ACTF(junk[:], xt[:, b, :], AF.Square, accum_out=ss[:]), r=[R_xt], w=[R_junk, R_ss])
                            S.op("act", ACTF(lnv[:], ss[:], AF.Ln, bias=epsD[:], scale=1.0 / D), r=[R_ss, R_c2], w=[R_ss])
                            S.op("act", ACTF(rstd[:], lnv[:], AF.Exp, scale=-0.5), r=[R_ss], w=[R_ss])
                            S.op("dve", STT(ob[k][:], xt[:, b, :], rstd[:], fng[:], ALU.mult, ALU.mult), r=[R_xt, R_ss, R_fng], w=[R_ob[k]])
                            S.dma("sp", "cst%d" % k, xdst[row0:row0 + 128, :], ob[k][:], r=[R_ob[k]], w=[R_dst[k]])
                        else:
                            S.dma("sp", "cstx", xdst[row0:row0 + 128, :], xt[:, b, :], r=[R_xt], w=[R_dst[0]])

        def moe_sparse(l, xsrc, R_src, xdst, R_dst, ntok, blend):
            P = lp[l]
            NBL = ntok // 128
            NF = NFM
            GF = 2
            U32 = mybir.dt.uint32
            IOA = bass.IndirectOffsetOnAxis
            with ExitStack() as oe:
                d1i = sbt(oe, "d1i", [128, NBL], I32); d2i = sbt(oe, "d2i", [128, NBL], I32); R_di = S.res()
                g12 = sbt(oe, "g12", [128, NBL, 2]); R_g12 = S.res()
                idxw = sbt(oe, "idxw", [128, NSL, NF], I32); R_idxw = S.res()
                cst2 = sbt(oe, "cst2", [128, NSL + 1 + NF]); R_cst2 = S.res()
                S.dma("sp", "cst2", cst2[:], cst2_in, w=[R_cst2])
                R_hs = S.res(); R_ys = S.res()

                def load_x(b, xt1, R_xt1, xB, R_xB, k):
                    row0 = b * 128
                    S.dma("sp", "mx%d" % k, xt1[k][:], xsrc[row0:row0 + 128, :], r=R_src, w=[R_xt1[k]])
                    if blend:
                        S.dma("sp", "mxB%d" % k, xB[k][:], xsrc[ntok + row0:ntok + row0 + 128, :], r=R_src, w=[R_xB[k]])
                        S.op("dve", TS(xB[k][:], xB[k][:], selt[:, 1:2]), r=[R_xB[k], R_c], w=[R_xB[k]])
                        S.op("dve", STT(xt1[k][:], xt1[k][:], selt[:, 0:1], xB[k][:], ALU.mult, ALU.add), r=[R_xB[k], R_c, R_xt1[k]], w=[R_xt1[k]])

                with ExitStack() as me:
                    H = sbt(me, "H", [128, NBL, D], BF16); R_H = S.res()
                    xt1 = [sbt(me, "m1x%d" % i, [128, D]) for i in range(3)]; R_xt1 = [S.res() for _ in range(3)]
                    xB = [sbt(me, "m1xB%d" % i, [128, D]) for i in range(3)]; R_xB = [S.res() for _ in range(3)]
                    xn = [sbt(me, "m1xn%d" % i, [128, D]) for i in range(3)]; R_xn = [S.res() for _ in range(3)]
                    junk = sbt(me, "m1junk", [128, D], BF16); R_junk = S.res()
                    ss_ = [sbt(me, "m1ss%d" % i, [128, 1]) for i in range(3)]; lnv_ = [sbt(me, "m1lnv%d" % i, [128, 1]) for i in range(3)]
                    rstd_ = [sbt(me, "m1rstd%d" % i, [128, 1]) for i in range(3)]; R_ss_ = [S.res() for _ in range(3)]
                    hTf_ = [sbt(me, "m1hTf%d" % i, [128, 8, 128]) for i in range(3)]; R_hTf_ = [S.res() for _ in range(3)]
                    htmp_ = [sbt(me, "m1htmp%d" % i, [128, D]) for i in range(3)]; R_htmp_ = [S.res() for _ in range(3)]
                    gsrow = sbt(me, "gsrow", [128, D]); shrow = sbt(me, "shrow", [128, D]); R_rows = S.res()
                    dg = sbt(me, "dg", [128, 8, 128]); R_dg = S.res()
                    onesf = sbt(me, "onesf", [128, 128]); usf = sbt(me, "usf", [128, 128]); R_cf = S.res()
                    lg_ = [sbt(me, "m1lg%d" % i, [128, 8]) for i in range(3)]; top8_ = [sbt(me, "m1top8%d" % i, [128, 8]) for i in range(3)]; R_lg_ = [S.res() for _ in range(3)]
                    gt_ = [sbt(me, "m1gt%d" % i, [128, 4]) for i in range(3)]; R_gt_ = [S.res() for _ in range(3)]
                    sel1 = sbt(me, "sel1", [128, NBL, 8]); sel2 = sbt(me, "sel2", [128, NBL, 8]); R_sel = S.res()
                    selS = sbt(me, "selS", [128, NBL, 8]); R_selS = S.res()
                    wit = sbt(me, "wit", [128, NBL, 8]); R_wit = S.res()
                    ca = sbt(me, "ca", [128, NBL, 8]); cb_ = sbt(me, "cb_", [128, NBL, 8]); cnt0 = sbt(me, "cnt0", [128, NBL, 8]); R_cn = S.res()
                    sm = sbt(me, "m1sm", [128, 64]); R_smm = S.res()
                    smi = sbt(me, "m1smi", [128, 8], I32)
                    dstf = sbt(me, "dstf", [128, NBL, 8]); R_dstf = S.res()
                    d1f = sbt(me, "d1f", [128, NBL]); d2f = sbt(me, "d2f", [128, NBL]); R_df = S.res()
                    cmp = sbt(me, "cmp", [128, NSL, 8]); esl = sbt(me, "esl", [128, NSL]); R_es = S.res()
                    idxf = sbt(me, "idxf", [128, NSL, NF]); R_idxf = S.res()
                    zt = sbt(me, "zt", [128, D], BF16); R_zt = S.res()
                    S.op("dve", CP(onesf[:], ones_b), r=[R_c], w=[R_cf])
                    S.op("dve", TT(usf[:], U_b, ident_b, ALU.subtract), r=[R_c], w=[R_cf])
                    S.op("pool", MEMSET(zt[:], 0.0), w=[R_zt])
                    for rb in range(NROWS // 128):
                        S.dma("sp", "hsz", hs_d[rb * 128:(rb + 1) * 128, :], zt[:], r=[R_zt], w=[R_hs])
                    for (dst_, col_t, col0) in ((gsrow, gsf[l], 0), (shrow, modc[l], 24)):
                        S.op("dve", TT(dg[:], ident_f.unsqueeze(1).broadcast_to([128, 8, 128]),
                                       col_t[:, col0:col0 + 8].unsqueeze(2).broadcast_to([128, 8, 128]), ALU.mult), r=[R_c, R_mod], w=[R_dg])
                        d2_ = dg[:].rearrange("p c n -> p (c n)")
                        for hf in range(2):
                            pt, pr = PS()
                            S.op("pe", MMS([(pt[:, :], onesf[:], d2_[:, hf * 512:(hf + 1) * 512], True, True)]), r=[R_dg, R_cf], w=[pr])
                            S.op("act", ACTF(dst_[:, hf * 512:(hf + 1) * 512], pt[:, :], AF.Copy), w=[pr, R_rows])
                    def m1blk(b):
                        k = b % 3
                        ss, lnv, rstd, R_ss = ss_[k], lnv_[k], rstd_[k], R_ss_[k]
                        hTf, R_hTf, htmp, R_htmp = hTf_[k], R_hTf_[k], htmp_[k], R_htmp_[k]
                        lg, top8, R_lg, gt, R_gt = lg_[k], top8_[k], R_lg_[k], gt_[k], R_gt_[k]
                        load_x(b, xt1, R_xt1, xB, R_xB, k)
                        yield None
                        yield S.op("act", ACTF(junk[:], xt1[k][:], AF.Square, accum_out=ss[:]), r=[R_xt1[k]], w=[R_junk, R_ss])
                        yield S.op("act", ACTF(lnv[:], ss[:], AF.Ln, bias=epsD[:], scale=1.0 / D), r=[R_ss, R_c2], w=[R_ss])
                        yield S.op("act", ACTF(rstd[:], lnv[:], AF.Exp, scale=-0.5), r=[R_ss], w=[R_ss])
                        yield S.op("act", ACTF(xn[k][:], xt1[k][:], AF.Identity, scale=rstd[:]), r=[R_xt1[k], R_ss], w=[R_xn[k]])
                        pa, ra = yield from PSG()
                        pb, rb_ = yield from PSG()
                        yield S.op("pe", TRS([(pa[:, j * 128:(j + 1) * 128], xn[k][:, j * 128:(j + 1) * 128], ident_f) for j in range(4)]), r=[R_xn[k], R_c], w=[ra])
                        yield S.op("pe", TRS([(pb[:, j * 128:(j + 1) * 128], xn[k][:, (4 + j) * 128:(5 + j) * 128], ident_f) for j in range(4)]), r=[R_xn[k], R_c], w=[rb_])
                        yield S.op("act", ACTS([(hTf[:, j, :], pa[:, j * 128:(j + 1) * 128], AF.Identity,
                                           dict(scale=gsf[l][:, j:j + 1], bias=modc[l][:, 24 + j:25 + j])) for j in range(4)]), r=[R_mod], w=[ra, R_hTf])
                        yield S.op("act", ACTS([(hTf[:, 4 + j, :], pb[:, j * 128:(j + 1) * 128], AF.Identity,
                                           dict(scale=gsf[l][:, 4 + j:5 + j], bias=modc[l][:, 28 + j:29 + j])) for j in range(4)]), r=[R_mod], w=[rb_, R_hTf])
                        yield S.op("dve", TT(htmp[:], xn[k][:], gsrow[:], ALU.mult), r=[R_xn[k], R_rows], w=[R_htmp])
                        yield S.op("dve", TT(H[:, b, :], htmp[:], shrow[:], ALU.add), r=[R_htmp, R_rows], w=[R_H])
                        pr_, rr_ = yield from PSG()
                        yield S.op("pe", MMS([(pr_[:, 0:8], hTf[:, dch, :], P[:, P_RW + dch * 8:P_RW + dch * 8 + 8], dch == 0, dch == 7) for dch in range(8)]),
                             r=[R_hTf, R_c], w=[rr_])
                        yield S.op("dve", CP(lg[:], pr_[:, 0:8]), w=[rr_, R_lg])
                        yield S.op("dve", lambda e: e.max(out=top8[:], in_=lg[:]), r=[R_lg], w=[R_lg])
                        yield S.op("dve", TT(gt[:, 0:1], top8[:, 1:2], top8[:, 0:1], ALU.subtract), r=[R_lg], w=[R_gt])
                        yield S.op("act", ACTF(gt[:, 1:2], gt[:, 0:1], AF.Exp), r=[R_gt], w=[R_gt])
                        yield S.op("dve", TS(gt[:, 1:2], gt[:, 1:2], 1.0, None, ALU.add), r=[R_gt], w=[R_gt])
                        yield S.op("dve", lambda e, b=b: e.reciprocal(out=g12[:, b, 0:1], in_=gt[:, 1:2]), r=[R_gt], w=[R_g12])
                        yield S.op("dve", TS(g12[:, b, 1:2], g12[:, b, 0:1], -1.0, 1.0, ALU.mult, ALU.add), r=[R_g12], w=[R_g12])
                        yield S.op("dve", TS(sel1[:, b, :], lg[:], top8[:, 0:1], None, ALU.is_equal), r=[R_lg], w=[R_sel])
                        yield S.op("dve", TS(sel2[:, b, :], lg[:], top8[:, 1:2], None, ALU.is_equal), r=[R_lg], w=[R_sel])
                    act_g = []
                    nxt = 0
                    while nxt < NBL or act_g:
                        while len(act_g) < 3 and nxt < NBL:
                            act_g.append(m1blk(nxt))
                            nxt += 1
                        for g_ in list(act_g):
                            try:
                                next(g_)
                            except StopIteration:
                                act_g.remove(g_)
                    fl = lambda t_: t_[:].rearrange("p b e -> p (b e)")
                    S.op("dve", TT(selS[:], sel1[:], sel2[:], ALU.add), r=[R_sel], w=[R_selS])
                    NCOL = NBL * 8
                    for c0 in range(0, NCOL, 512):
                        c1 = min(NCOL, c0 + 512)
                        pw, rw = PS()
                        S.op("pe", MMS([(pw[:, 0:c1 - c0], usf[:], fl(selS)[:, c0:c1], True, True)]), r=[R_selS, R_cf], w=[rw])
                        S.op("act", ACTF(fl(wit)[:, c0:c1], pw[:, 0:c1 - c0], AF.Copy), w=[rw, R_wit])
                        pc_, rc_ = PS()
                        S.op("pe", MMS([(pc_[:, 0:c1 - c0], onesf[:], fl(selS)[:, c0:c1], True, True)]), r=[R_selS, R_cf], w=[rc_])
                        S.op("act", ACTF(fl(cnt0)[:, c0:c1], pc_[:, 0:c1 - c0], AF.Copy), w=[rc_, R_cn])
                    S.op("dve", CP(ca[:], cnt0[:]), r=[R_cn], w=[R_cn])
                    cur, oth = ca, cb_
                    sh = 1
                    while sh < NBL:
                        S.op("dve", CP(oth[:, 0:sh, :], cur[:, 0:sh, :]), r=[R_cn], w=[R_cn])
                        S.op("dve", TT(oth[:, sh:NBL, :], cur[:, sh:NBL, :], cur[:, 0:NBL - sh, :], ALU.add), r=[R_cn], w=[R_cn])
                        cur, oth = oth, cur
                        sh *= 2
                    incl = cur
                    S.op("dve", TT(oth[:], incl[:], cnt0[:], ALU.subtract), r=[R_cn], w=[R_cn])
                    base = oth
                    tot = incl[:, NBL - 1, :]
                    S.op("dve", TS(sm[:, 0:8], tot, float(SLOTR - 1), 1.0 / SLOTR, ALU.add, ALU.mult), r=[R_cn], w=[R_smm])
                    S.op("dve", TS(sm[:, 0:8], sm[:, 0:8], -0.5 + 0.5 / SLOTR, None, ALU.add), r=[R_smm], w=[R_smm])
                    S.op("dve", CP(smi[:], sm[:, 0:8]), r=[R_smm], w=[R_smm])
                    S.op("dve", CP(sm[:, 0:8], smi[:]), r=[R_smm], w=[R_smm])
                    S.op("dve", TS(sm[:, 0:8], sm[:, 0:8], float(SLOTR), None, ALU.mult), r=[R_smm], w=[R_smm])
                    S.op("dve", CP(sm[:, 8:16], sm[:, 0:8]), r=[R_smm], w=[R_smm])
                    a0, b0 = 8, 16
                    for sh in (1, 2, 4):
                        S.op("dve", CP(sm[:, b0:b0 + sh], sm[:, a0:a0 + sh]), r=[R_smm], w=[R_smm])
                        S.op("dve", TT(sm[:, b0 + sh:b0 + 8], sm[:, a0 + sh:a0 + 8], sm[:, a0:a0 + 8 - sh], ALU.add), r=[R_smm], w=[R_smm])
                        a0, b0 = b0, a0
                    pend = sm[:, a0:a0 + 8]
                    S.op("dve", TT(sm[:, 24:32], pend, sm[:, 0:8], ALU.subtract), r=[R_smm], w=[R_smm])
                    pstart = sm[:, 24:32]
                    S.op("dve", TT(dstf[:], base[:], wit[:], ALU.add), r=[R_cn, R_wit], w=[R_dstf])
                    S.op("dve", TT(dstf[:], dstf[:], pstart.unsqueeze(1).broadcast_to([128, NBL, 8]), ALU.add), r=[R_smm, R_dstf], w=[R_dstf])
                    for (sel_, df_, di_) in ((sel1, d1f, d1i), (sel2, d2f, d2i)):
                        S.op("dve", TT(selS[:], sel_[:], dstf[:], ALU.mult), r=[R_sel, R_dstf, R_selS], w=[R_selS])
                        S.op("dve", lambda e, df_=df_: e.tensor_reduce(out=df_[:], in_=selS[:], axis=mybir.AxisListType.X, op=ALU.add), r=[R_selS], w=[R_df])
                        S.op("dve", CP(di_[:], df_[:]), r=[R_df], w=[R_di])
                    S.op("dve", TT(cmp[:], pend.unsqueeze(1).broadcast_to([128, NSL, 8]),
                                   cst2[:, 0:NSL].unsqueeze(2).broadcast_to([128, NSL, 8]), ALU.is_le), r=[R_smm, R_cst2], w=[R_es])
                    S.op("dve", lambda e: e.tensor_reduce(out=esl[:], in_=cmp[:], axis=mybir.AxisListType.X, op=ALU.add), r=[R_es], w=[R_es])
                    S.op("dve", TS(esl[:], esl[:], 7.0, 128.0, ALU.min, ALU.mult), r=[R_es], w=[R_es])
                    S.op("dve", TS(esl[:], esl[:], cst2[:, NSL:NSL + 1], None, ALU.add), r=[R_es, R_cst2], w=[R_es])
                    S.op("dve", TT(idxf[:], esl[:].unsqueeze(2).broadcast_to([128, NSL, NF]),
                                   cst2[:, NSL + 1:NSL + 1 + NF].unsqueeze(1).broadcast_to([128, NSL, NF]), ALU.add), r=[R_es, R_cst2], w=[R_idxf])
                    S.op("dve", CP(idxw[:], idxf[:]), r=[R_idxf], w=[R_idxw])
                    if DBG_M1:
                        dbg = sbt(me, "dbg", [128, D]); R_dbg = S.res()
                        S.op("pool", MEMSET(dbg[:], 0.0), w=[R_dbg])
                        S.op("dve", CP(dbg[:, 0:NBL], d1f[:]), r=[R_df, R_dbg], w=[R_dbg])
                        S.op("dve", CP(dbg[:, 64:64 + NBL], d2f[:]), r=[R_df, R_dbg], w=[R_dbg])
                        S.op("dve", CP(dbg[:, 128:128 + NSL], esl[:]), r=[R_es, R_dbg], w=[R_dbg])
                        S.op("dve", CP(dbg[:, 192:256], sm[:]), r=[R_smm, R_dbg], w=[R_dbg])
                        S.op("dve", CP(dbg[:, 256:256 + NBL * 8], dstf[:].rearrange("p b e -> p (b e)")), r=[R_dstf, R_dbg], w=[R_dbg])
                        S.op("dve", CP(dbg[:, 512:512 + NBL * 8], sel1[:].rearrange("p b e -> p (b e)")), r=[R_sel, R_dbg], w=[R_dbg])
                        S.op("dve", CP(dbg[:, 768:768 + NBL * 8], sel2[:].rearrange("p b e -> p (b e)")), r=[R_sel, R_dbg], w=[R_dbg])
                        S.dma("sp", "dbg", xdst[0:128, :], dbg[:], r=[R_dbg], w=[R_dst[0]])
                        S.dma("sp", "dbg", xdst[128:256, 0:NSL * NF], idxf[:].rearrange("p s f -> p (s f)"), r=[R_idxf], w=[R_dst[0]])
                        return
                    for b in range(NBL):
                        S.idma("sc1", hs_d, IOA(ap=d1i[:, b:b + 1], axis=0), H[:, b, :], None, r=[R_H, R_di], w=[R_hs])
                        S.idma("sc1", hs_d, IOA(ap=d2i[:, b:b + 1], axis=0), H[:, b, :], None, r=[R_H, R_di], w=[R_hs])
                S.barrier()

                with ExitStack() as me:
                    hTt = sbt(me, "shTt", [128, 8, SLOTR], BF16); R_hTt = S.res()
                    hrow = [sbt(me, "hrow%d" % i, [128, D], BF16) for i in range(2)]; R_hrow = [S.res() for _ in range(2)]
                    acc = sbt(me, "sacc", [128, SLOTR // 128, D]); R_acc = S.res()
                    actT = [sbt(me, "sactT%d" % i, [128, GF, SLOTR], BF16) for i in range(2)]; R_actT = [S.res() for _ in range(2)]
                    sg = [sbt(me, "ssg%d" % i, [128, 512], BF16) for i in range(2)]; R_sg = [S.res() for _ in range(2)]
                    NSLOT = 3 * GF
                    gstg = [sbt(me, "sgstg%d" % i, [128, 8, 128]) for i in range(3)]; R_gstg = [S.res() for _ in range(3)]
                    ustg = [sbt(me, "sustg%d" % i, [128, 8, 128]) for i in range(3)]; R_ustg = [S.res() for _ in range(3)]
                    dstg = [sbt(me, "sdstg%d" % i, [128, D]) for i in range(3)]; R_dstg = [S.res() for _ in range(3)]
                    wgb = [sbt(me, "swgb%d" % i, [128, 8, 128], BF16) for i in range(NSLOT)]; R_wgb = [S.res() for _ in range(NSLOT)]
                    wub = [sbt(me, "swub%d" % i, [128, 8, 128], BF16) for i in range(NSLOT)]; R_wub = [S.res() for _ in range(NSLOT)]
                    wdb = [sbt(me, "swdb%d" % i, [128, D], BF16) for i in range(NSLOT)]; R_wdb = [S.res() for _ in range(NSLOT)]
                    slot = [0]; stgc = [0]; castc = [0]
                    NRB = SLOTR // 128
                    for s_i in range(NSL):
                        for rb in range(NRB):
                            k = rb % 2
                            r0 = s_i * SLOTR + rb * 128
                            S.dma("sp", "hrow%d" % k, hrow[k][:], hs_d[r0:r0 + 128, :], r=[R_hs], w=[R_hrow[k]])
                            pt, pr = PS()
                            ptb = pt[:, :].bitcast(BF16)
                            S.op("pe", TRS([(ptb[:, j * 128:(j + 1) * 128], hrow[k][:, j * 128:(j + 1) * 128], ident_b) for j in range(8)]),
                                 r=[R_hrow[k], R_c], w=[pr])
                            S.op("act", ACTF(hTt[:, :, rb * 128:(rb + 1) * 128], ptb[:, :].rearrange("p (c t) -> p c t", c=8), AF.Copy), w=[pr, R_hTt])
                        for fg in range(NF // GF):
                            ab = fg % 2
                            slots = []
                            for fi in range(GF):
                                fc = fg * GF + fi
                                sl_ = slot[0] % NSLOT
                                slot[0] += 1
                                slots.append(sl_)
                                k = stgc[0] % 3
                                stgc[0] += 1
                                io = IOA(ap=idxw[:, s_i, fc:fc + 1], axis=0)
                                S.idma("sgs%d" % k, gstg[k][:].rearrange("p c n -> p (c n)"), None, mgl_in, io, r=[R_idxw], w=[R_gstg[k]])
                                S.idma("sus%d" % k, ustg[k][:].rearrange("p c n -> p (c n)"), None, mul_in, io, r=[R_idxw], w=[R_ustg[k]])
                                S.idma("sds%d" % k, dstg[k][:], None, mdl_in, io, r=[R_idxw], w=[R_dstg[k]])
                                for (src_, R_s, dst_, R_d) in ((gstg[k], R_gstg[k], wgb[sl_], R_wgb[sl_]), (ustg[k], R_ustg[k], wub[sl_], R_wub[sl_])):
                                    ce = ("act", "dve")[castc[0] % 2]
                                    castc[0] += 1
                                    if ce == "act":
                                        S.op("act", ACTF(dst_[:], src_[:], AF.Copy), r=[R_s], w=[R_d])
                                    else:
                                        S.op("dve", CP(dst_[:], src_[:]), r=[R_s], w=[R_d])
                                S.op("dve", TT(wdb[sl_][:], dstg[k][:], gfrow[l][:], ALU.mult), r=[R_dstg[k], R_mod], w=[R_wdb[sl_]])
                                for sub in range(SLOTR // 512):
                                    tcs = slice(sub * 512, (sub + 1) * 512)
                                    pg, rg_ = PS()
                                    S.op("pe", MMS([(pg[:, :], wgb[sl_][:, dch, :], hTt[:, dch, tcs], dch == 0, dch == 7) for dch in range(8)]),
                                         r=[R_wgb[sl_], R_hTt], w=[rg_])
                                    pu, ru = PS()
                                    S.op("pe", MMS([(pu[:, :], wub[sl_][:, dch, :], hTt[:, dch, tcs], dch == 0, dch == 7) for dch in range(8)]),
                                         r=[R_wub[sl_], R_hTt], w=[ru])
                                    kk = sub % 2
                                    S.op("act", ACTF(sg[kk][:], pg[:, :], AF.Silu), w=[rg_, R_sg[kk]])
                                    S.op("dve", TT(actT[ab][:, fi, tcs], sg[kk][:], pu[:, :], ALU.mult), r=[R_sg[kk]], w=[ru, R_actT[ab]])
                            for rb in range(NRB):
                                for hf in range(2):
                                    pd, rd = PS()
                                    S.op("pe", MMS([(pd[:, :], actT[ab][:, fi, rb * 128:(rb + 1) * 128], wdb[slots[fi]][:, hf * 512:(hf + 1) * 512],
                                                     fi == 0, fi == GF - 1) for fi in range(GF)]),
                                         r=[R_actT[ab]] + [R_wdb[x_] for x_ in slots], w=[rd])
                                    xs_ = acc[:, rb, hf * 512:(hf + 1) * 512]
                                    if fg == 0:
                                        S.op("act", ACTF(xs_, pd[:, :], AF.Copy), w=[rd, R_acc])
                                    else:
                                        S.op("dve", TT(xs_, pd[:, :], xs_, ALU.add), w=[rd, R_acc])
                        for rb in range(NRB):
                            r0 = s_i * SLOTR + rb * 128
                            S.dma("sp", "yst", ys_d[r0:r0 + 128, :], acc[:, rb, :], r=[R_acc], w=[R_ys])
                S.barrier()

                with ExitStack() as me:
                    xt1 = [sbt(me, "m3x%d" % i, [128, D]) for i in range(2)]; R_xt1 = [S.res() for _ in range(2)]
                    xB = [sbt(me, "m3xB%d" % i, [128, D]) for i in range(2)]; R_xB = [S.res() for _ in range(2)]
                    y1t = [sbt(me, "m3y1%d" % i, [128, D]) for i in range(2)]; R_y1t = [S.res() for _ in range(2)]
                    y2t = [sbt(me, "m3y2%d" % i, [128, D]) for i in range(2)]; R_y2t = [S.res() for _ in range(2)]
                    ob = [sbt(me, "m3ob%d" % i, [128, D]) for i in range(2)]; R_ob = [S.res() for _ in range(2)]
                    junk = sbt(me, "m3junk", [128, D], BF16); R_junk = S.res()
                    ss = sbt(me, "m3ss", [128, 1]); lnv = sbt(me, "m3lnv", [128, 1]); rstd = sbt(me, "m3rstd", [128, 1]); R_ss = S.res()
                    fng = sbt(me, "m3fng", [128, D]); R_fng = S.res()
                    S.dma("sp", "fng", fng[:], fng_in, w=[R_fng])
                    for b in range(NBL):
                        k = b % 2
                        load_x(b, xt1, R_xt1, xB, R_xB, k)
                        S.idma("g1_%d" % k, y1t[k][:], None, ys_d, IOA(ap=d1i[:, b:b + 1], axis=0), r=[R_ys, R_di], w=[R_y1t[k]])
                        S.idma("g2_%d" % k, y2t[k][:], None, ys_d, IOA(ap=d2i[:, b:b + 1], axis=0), r=[R_ys, R_di], w=[R_y2t[k]])
                        S.op("dve", STT(xt1[k][:], y1t[k][:], g12[:, b, 0:1], xt1[k][:], ALU.mult, ALU.add), r=[R_y1t[k], R_g12, R_xt1[k]], w=[R_xt1[k]])
                        S.op("dve", STT(xt1[k][:], y2t[k][:], g12[:, b, 1:2], xt1[k][:], ALU.mult, ALU.add), r=[R_y2t[k], R_g12, R_xt1[k]], w=[R_xt1[k]])
                        S.op("act", ACTF(junk[:], xt1[k][:], AF.Square, accum_out=ss[:]), r=[R_xt1[k]], w=[R_junk, R_ss])
                        S.op("act", ACTF(lnv[:], ss[:], AF.Ln, bias=epsD[:], scale=1.0 / D), r=[R_ss, R_c2], w=[R_ss])
                        S.op("act", ACTF(rstd[:], lnv[:], AF.Exp, scale=-0.5), r=[R_ss], w=[R_ss])
                        S.op("dve", STT(ob[k][:], xt1[k][:], rstd[:], fng[:], ALU.mult, ALU.mult), r=[R_xt1[k], R_ss, R_fng], w=[R_ob[k]])
                        S.dma("sp", "m3st%d" % k, xdst[b * 128:(b + 1) * 128, :], ob[k][:], r=[R_ob[k]], w=[R_dst[k]])

        R_x = [S.res("x_in")]; R_xa = [S.res("xa0"), S.res("xa1")]; R_xb = [S.res("xb0"), S.res("xb1")]
        R_out = [S.res("out0"), S.res("out1")]
        stages = [
            ("mix0", lambda dst, Rd: mixer(0, x_in, R_x, dst, Rd)),
            ("ffn0", lambda dst, Rd: chanmix(0, xa_d, R_xa, dst, Rd, False, False, L, False)),
            ("mix1", lambda dst, Rd: mixer(1, xb_d, R_xb, dst, Rd)),
            ("moe1", (lambda dst, Rd: moe_sparse(1, xa_d, R_xa, dst, Rd, LM, split)) if sparse else
                     (lambda dst, Rd: chanmix(1, xa_d, R_xa, dst, Rd, True, True, LM, split))),
        ]
        dsts = [(xa_d, R_xa), (xb_d, R_xb), (xa_d, R_xa), (out_d, R_out)]
        for (name, fn), (dst, Rd) in zip(stages, dsts):
            if stop_after == name:
                fn(out_d, R_out)
                break
            fn(dst, Rd)
            S.barrier()
        S.wait_all("sp", R_out)
        with nc.Block() as block:
            S.emit(block)
    return nc


def _consts():
    cstf = np.zeros((128, 130), np.float32)
    cstf[:, :128] = np.eye(128, dtype=np.float32)
    p = np.arange(128)
    cstf[:, 128] = (10000.0 ** (-((p % 32) * 2).astype(np.float64) / 64.0)).astype(np.float32)
    cstf[:, 129] = np.where((p % 64) < 32, -1.0, 1.0)
    cb = np.zeros((128, B_TOT), np.float32)
    cb[:, B_ID:B_ID + 128] = np.eye(128)
    k = np.arange(128)[:, None]
    q = np.arange(128)[None, :]
    cb[:, B_U:B_U + 128] = (k <= q)
    cb[:, B_ONES:B_ONES + 128] = 1.0
    cb[:, B_MCUR:B_MCUR + 512] = np.tile(np.where(k <= q, 0.0, NEG), (1, 4))
    cb[:, B_MPREV:B_MPREV + 512] = np.tile(np.where(k > q, 0.0, NEG), (1, 4))
    return cstf, cb.astype(ml_dtypes.bfloat16)


def _perm_win(w):
    q = w[:, 0:512].reshape(D, 8, 64)
    k = w[:, 512:640].reshape(D, 2, 64)
    v = w[:, 640:768]
    z = w[:, 768:1280]
    xbc = w[:, 1280:2304]
    dt = w[:, 2304:2312]
    sw = lambda a: np.concatenate([a[..., 32:], a[..., :32]], axis=-1)
    qp = np.stack([np.concatenate([q[:, j], q[:, 4 + j]], axis=-1) for j in range(4)], axis=1).reshape(D, 512)
    qs = sw(q)
    qsp = np.stack([np.concatenate([qs[:, j], qs[:, 4 + j]], axis=-1) for j in range(4)], axis=1).reshape(D, 512)
    kp = k.reshape(D, 128)
    ks = sw(k).reshape(D, 128)
    return np.ascontiguousarray(np.concatenate([qp, qsp, kp, ks, xbc, v, z, dt], axis=1))


def _lp(l, norm_mix_g, norm_ffn_g, conv_w, conv_b, attn_sinks, dt_bias, a_log, d_skip, ssm_norm_g, router_w):
    lp = np.zeros((128, P_TOT), np.float32)
    lp[:, P_GM:P_GM + 8] = norm_mix_g[l].reshape(8, 128).T
    lp[:, P_GF:P_GF + 8] = norm_ffn_g[l].reshape(8, 128).T
    lp[:, P_CW:P_CW + 32] = conv_w[l].reshape(4, 8, 128).transpose(2, 1, 0).reshape(128, 32)
    lp[:, P_CB:P_CB + 8] = conv_b[l].reshape(8, 128).T
    lp[:, P_SK:P_SK + 8] = attn_sinks[l][None, :]
    lp[:, P_DTB:P_DTB + 8] = dt_bias[l][None, :]
    lp[:, P_ALOG:P_ALOG + 8] = a_log[l][None, :]
    lp[:, P_DSK:P_DSK + 8] = d_skip[l][None, :]
    lp[:, P_SSMG:P_SSMG + 512] = ssm_norm_g[l][None, :]
    if l == 1:
        lp[:, P_RW:P_RW + 64] = router_w[0].reshape(8, 128, 8).transpose(1, 0, 2).reshape(128, 64)
    return lp


def make_in_maps(inp, L, ncores, split=True, sparse=True):
    f = lambda a: np.ascontiguousarray(np.asarray(a, dtype=np.float32))
    cstf, cstb = _consts()
    shared = {
        "cstf": cstf, "cstb": cstb,
        "fng": np.ascontiguousarray(np.broadcast_to(f(inp["final_norm_g"])[None, :], (128, D))),
        "ffg": f(inp["ffn_w_gate"][0]), "ffu": f(inp["ffn_w_up"][0]), "ffd": f(inp["ffn_w_down"][0]),
    }
    if not sparse:
        shared.update({"mog": f(inp["moe_w_gate"][0]), "mou": f(inp["moe_w_up"][0]), "mod": f(inp["moe_w_down"][0])})
    for l in range(2):
        shared["lp%d" % l] = _lp(l, f(inp["norm_mix_g"]), f(inp["norm_ffn_g"]), f(inp["conv_w"]), f(inp["conv_b"]),
                                 f(inp["attn_sinks"]), f(inp["dt_bias"]), f(inp["a_log"]), f(inp["d_skip"]),
                                 f(inp["ssm_norm_g"]), f(inp["router_w"]))
        shared["adaw%d" % l] = f(inp["ada_w"][l])
        shared["adab%d" % l] = f(inp["ada_b"][l])[None, :]
        shared["win%d" % l] = _perm_win(f(inp["w_in"][l]))
        shared["wout%d" % l] = f(inp["w_out"][l])
    if sparse:
        NFM = EXD // 128
        LM = L // 2 if split else L
        NSL = (2 * LM) // SLOTR + NEXP
        wg = f(inp["moe_w_gate"][0]); wu = f(inp["moe_w_up"][0]); wd = f(inp["moe_w_down"][0])
        lay = lambda w: np.ascontiguousarray(w.reshape(8, 8, 128, NFM, 128).transpose(3, 0, 2, 1, 4).reshape(NFM * 1024, 1024))
        shared["mogl"] = lay(wg)
        shared["moul"] = lay(wu)
        shared["modl"] = np.ascontiguousarray(wd.reshape(8, NFM, 128, 1024).transpose(1, 0, 2, 3).reshape(NFM * 1024, 1024))
        c2 = np.zeros((128, NSL + 1 + NFM), np.float32)
        c2[:, :NSL] = (np.arange(NSL) * SLOTR)[None, :]
        c2[:, NSL] = np.arange(128)
        c2[:, NSL + 1:] = (np.arange(NFM) * 1024)[None, :]
        shared["cst2"] = c2
    x = np.asarray(inp["x"], dtype=np.float32)
    c = np.asarray(inp["c"], dtype=np.float32)
    pos = np.asarray(inp["positions"], dtype=np.int32)
    maps = []
    for r in range(ncores):
        b, half = (r // 2, r % 2) if split else (r, 0)
        m = dict(shared)
        m["x"] = np.ascontiguousarray(x[b, :L])
        m["pos"] = np.ascontiguousarray(pos[b:b + 1, :L])
        m["cT"] = np.ascontiguousarray(c[b].reshape(8, 128).T)
        sel = np.zeros((128, 2), np.float32)
        sel[:, half] = 1.0
        m["sel"] = sel
        maps.append(m)
    return maps


_NC_CACHE = {}


def kernel(**inputs):
    x = np.asarray(inputs["x"])
    B, L, _ = x.shape
    if L not in _NC_CACHE:
        _NC_CACHE[L] = build(L)
    nc = _NC_CACHE[L]
    ncores = 2 * B
    maps = make_in_maps(inputs, L, ncores)
    res = run_bass_kernel_spmd(nc, maps, core_ids=list(range(ncores)))
    out = np.empty((B, L, D), np.float32)
    h = L // 2
    for r in range(ncores):
        out[r // 2, (r % 2) * h:(r % 2 + 1) * h] = np.asarray(res.results[r]["out"])
    return out
```

```python
import math
from contextlib import ExitStack

import numpy as np
import ml_dtypes

import concourse.bass as bass
import concourse.mybir as mybir
from concourse.bass_utils import run_bass_kernel_spmd

F32 = mybir.dt.float32
BF16 = mybir.dt.bfloat16
I32 = mybir.dt.int32
AF = mybir.ActivationFunctionType
ALU = mybir.AluOpType
PI = math.pi

D = 1024
NQH = 8
INW = 2952
C_Q, C_QS, C_K, C_KS, C_X, C_V, C_Z, C_DT = 0, 512, 1024, 1152, 1280, 2304, 2432, 2944
FFN = 2816
EXD = 3584
NEXP = 8
EPS = 1e-6
NEG = -30000.0
P_GM, P_GF, P_CW, P_CB, P_SK, P_DTB, P_ALOG, P_DSK, P_SSMG, P_RW, P_TOT = 0, 8, 16, 48, 56, 64, 72, 80, 88, 600, 664
B_ID, B_U, B_ONES, B_MCUR, B_MPREV, B_TOT = 0, 128, 256, 384, 896, 1408


class Res:
    __slots__ = ("name", "w", "r", "hold")

    def __init__(self, name):
        self.name = name
        self.w = None
        self.r = {}
        self.hold = None


class Sched:
    ENG = ("pe", "act", "dve", "pool", "sp")

    def __init__(self, nc, es):
        self.nc = nc
        self.es = es
        self.prog = {e: [] for e in self.ENG}
        self.sem = {}
        self.cnt = {}
        for e in ("pe", "act", "dve", "pool"):
            self.sem[e] = es.enter_context(nc.semaphore("c_" + e))
            self.cnt[e] = 0
        self.waited = {e: {} for e in self.ENG}
        self.nres = 0

    def res(self, name=None):
        self.nres += 1
        return Res(name or ("r%d" % self.nres))

    def _waits(self, eng, reads, writes):
        deps = []
        for r in reads:
            if r.w is not None:
                deps.append((r.w, True))
        for w in writes:
            if w.w is not None:
                deps.append((w.w, False))
            for k, (v, e) in w.r.items():
                deps.append(((k, v, e), False))
        waits = {}
        for (k, v, e), raw in deps:
            if e == eng:
                if eng == "pe":
                    continue
                if not raw:
                    continue
                if v < self.cnt[eng] - SAME_ENG_DIST:
                    continue
            if self.waited[eng].get(k, 0) >= v:
                continue
            if waits.get(k, 0) < v:
                waits[k] = v
        for k, v in waits.items():
            self.waited[eng][k] = v
        return list(waits.items())

    def _commit(self, ticket, reads, writes):
        k, v, e = ticket
        for r in reads:
            old = r.r.get(k)
            if old is None or old[0] < v:
                r.r[k] = (v, e)
        for w in writes:
            w.w = ticket
            w.r = {}

    def op(self, eng, fn, r=(), w=()):
        if eng != "pe":
            for x in w:
                if x.hold is not None and x.hold[0] > 0:
                    x.hold[0] -= 1
        waits = self._waits(eng, r, w)
        self.cnt[eng] += 1
        self.prog[eng].append((waits, fn, (eng, 1)))
        self._commit((eng, self.cnt[eng], eng), r, w)

    def dma(self, q, sem, out, in_, r=(), w=()):
        if sem not in self.sem:
            self.sem[sem] = self.es.enter_context(self.nc.semaphore("d_" + sem))
            self.cnt[sem] = 0
        waits = self._waits(q, r, w)
        self.cnt[sem] += 16
        self.prog[q].append((waits, (lambda e: e.dma_start(out=out, in_=in_)), (sem, 16)))
        self._commit((sem, self.cnt[sem], "dma"), r, w)

    def idma(self, sem, out, out_off, in_, in_off, r=(), w=()):
        if sem not in self.sem:
            self.sem[sem] = self.es.enter_context(self.nc.semaphore("d_" + sem))
            self.cnt[sem] = 0
        waits = self._waits("pool", r, w)
        hist = self.__dict__.setdefault("idma_hist", [])
        if len(hist) >= IDMA_DEPTH:
            k0, v0 = hist[-IDMA_DEPTH]
            if self.waited["pool"].get(k0, 0) < v0:
                self.waited["pool"][k0] = v0
                waits = [(k, v) for (k, v) in waits if k != k0] + [(k0, max(v0, dict(waits).get(k0, 0)))]
        self.cnt[sem] += 16
        hist.append((sem, self.cnt[sem]))
        self.prog["pool"].append((waits, (lambda e: e.indirect_dma_start(out=out, out_offset=out_off, in_=in_, in_offset=in_off)), (sem, 16)))
        self._commit((sem, self.cnt[sem], "dma"), r, w)

    def barrier(self):
        for eng in self.ENG:
            waits = []
            for k, v in self.cnt.items():
                if v == 0 or k == eng:
                    continue
                if self.waited[eng].get(k, 0) >= v:
                    continue
                self.waited[eng][k] = v
                waits.append((k, v))
            self.prog[eng].append((waits, None, None))

    def wait_all(self, eng, reads):
        waits = self._waits(eng, reads, ())
        self.prog[eng].append((waits, None, None))

    def emit(self, block):
        S = self

        def run(name, e):
            for waits, fn, inc in S.prog[name]:
                for k, v in waits:
                    e.wait_ge(S.sem[k], v)
                if fn is None:
                    continue
                ins = fn(e)
                if inc is not None:
                    ins.then_inc(S.sem[inc[0]], inc[1])

        @block.sync
        def _(e):
            run("sp", e)

        @block.tensor
        def _(e):
            run("pe", e)

        @block.scalar
        def _(e):
            run("act", e)

        @block.vector
        def _(e):
            run("dve", e)

        @block.gpsimd
        def _(e):
            run("pool", e)


def TT(out, in0, in1, op):
    return lambda e: e.tensor_tensor(out=out, in0=in0, in1=in1, op=op)


def TS(out, in0, s1, s2=None, op0=ALU.mult, op1=None):
    if op1 is None:
        return lambda e: e.tensor_scalar(out=out, in0=in0, scalar1=s1, scalar2=None, op0=op0)
    return lambda e: e.tensor_scalar(out=out, in0=in0, scalar1=s1, scalar2=s2, op0=op0, op1=op1)


def STT(out, in0, scalar, in1, op0, op1):
    return lambda e: e.scalar_tensor_tensor(out=out, in0=in0, scalar=scalar, in1=in1, op0=op0, op1=op1)


def ACTF(out, in_, func, bias=None, scale=None, accum_out=None):
    kw = {}
    if bias is not None:
        kw["bias"] = bias
    if scale is not None:
        kw["scale"] = scale
    if accum_out is not None:
        kw["accum_out"] = accum_out
    return lambda e: e.activation(out=out, in_=in_, func=func, **kw)


def ACTS(lst):
    def f(e):
        ins = None
        for (out, in_, func, kw) in lst:
            ins = e.activation(out=out, in_=in_, func=func, **kw)
        return ins
    return f


def CP(out, in_):
    return lambda e: e.tensor_copy(out=out, in_=in_)


def MEMSET(ap, v):
    return lambda e: e.memset(ap, v)


def MMS(lst):
    def f(e):
        ins = None
        for (out, lhsT, rhs, st, sp) in lst:
            ins = e.matmul(out, lhsT=lhsT, rhs=rhs, start=st, stop=sp)
        return ins
    return f


def TRS(lst):
    def f(e):
        ins = None
        for (out, in_, ident) in lst:
            ins = e.transpose(out=out, in_=in_, identity=ident)
        return ins
    return f


SLOTR = 512
DBG_M1 = False
SAME_ENG_DIST = 1000000
ALLOC = {}
IDMA_DEPTH = 6


def build(L, stop_after=None, split=True, sparse=True):
    NB = L // 128
    LM = L // 2 if split else L
    assert (not sparse) or (2 * LM) % SLOTR == 0
    TTOK = min(1024, LM)
    NBT = TTOK // 128
    nc = bass.Bass("TRN2", target_bir_lowering=False)

    def din(name, shape, dt=F32):
        return nc.dram_tensor(name, list(shape), dt, kind="ExternalInput").ap()

    x_in = din("x", [L, D])
    pos_in = din("pos", [1, L], I32)
    cT_in = din("cT", [128, 8])
    sel_in = din("sel", [128, 2])
    cstf_in = din("cstf", [128, 130])
    cstb_in = din("cstb", [128, B_TOT], BF16)
    fng_in = din("fng", [128, D])
    lp_in = [din("lp%d" % l, [128, P_TOT]) for l in range(2)]
    adaw_in = [din("adaw%d" % l, [D, 6 * D]) for l in range(2)]
    adab_in = [din("adab%d" % l, [1, 6 * D]) for l in range(2)]
    win_in = [din("win%d" % l, [D, INW]) for l in range(2)]
    wout_in = [din("wout%d" % l, [D, D]) for l in range(2)]
    fg_in = din("ffg", [D, FFN])
    fu_in = din("ffu", [D, FFN])
    fd_in = din("ffd", [FFN, D])
    mg_in = mu_in = md_in = None
    if not sparse:
        mg_in = din("mog", [NEXP, D, EXD])
        mu_in = din("mou", [NEXP, D, EXD])
        md_in = din("mod", [NEXP, EXD, D])
    NFM = EXD // 128
    NSL = (2 * LM) // SLOTR + NEXP if sparse else 1
    NROWS = NSL * SLOTR
    if sparse:
        mgl_in = din("mogl", [NFM * 1024, 1024])
        mul_in = din("moul", [NFM * 1024, 1024])
        mdl_in = din("modl", [NFM * 1024, 1024])
        cst2_in = din("cst2", [128, NSL + 1 + NFM])
        hs_d = nc.dram_tensor("hs_d", [NROWS, D], BF16, kind="Internal").ap()
        ys_d = nc.dram_tensor("ys_d", [NROWS, D], F32, kind="Internal").ap()
    out_d = nc.dram_tensor("out", [LM, D], F32, kind="ExternalOutput").ap()
    xa_d = nc.dram_tensor("xa", [L, D], F32, kind="Internal").ap()
    xb_d = nc.dram_tensor("xb", [L, D], F32, kind="Internal").ap()
    cos_d = nc.dram_tensor("cosd", [128, L], F32, kind="Internal").ap()
    sin_d = nc.dram_tensor("sind", [128, L], F32, kind="Internal").ap()

    with ExitStack() as es:
        S = Sched(nc, es)

        uniq = [0]

        def sbt(es_, name, shape, dt=F32):
            uniq[0] += 1
            nb = int(np.prod(shape[1:])) * (2 if dt == BF16 else 4)
            ALLOC[id(es_)] = ALLOC.get(id(es_), 0) + ((nb + 31) // 32) * 32
            return es_.enter_context(nc.sbuf_tensor("s%d_%s" % (uniq[0], name), list(shape), dt))

        banks = []
        for i in range(8):
            t = es.enter_context(nc.psum_tensor("ps%d" % i, [128, 512], F32))
            banks.append((t, S.res("ps%d" % i)))
        pscur = [0]
        for (_t, _r) in banks:
            _r.hold = [0]

        def PS_try(n=1):
            for d in range(8):
                t, r = banks[(pscur[0] + d) % 8]
                if r.hold[0] == 0:
                    pscur[0] = pscur[0] + d + 1
                    r.hold[0] = n
                    return t, r
            return None

        def PS(n=1):
            got = PS_try(n)
            assert got is not None, "no free PSUM bank"
            return got

        def PSG(n=1):
            while True:
                got = PS_try(n)
                if got is not None:
                    return got
                yield None

        cstf = sbt(es, "cstf", [128, 130]); R_c = S.res("const")
        cstb = sbt(es, "cstb", [128, B_TOT], BF16)
        lp = [sbt(es, "lp%d" % l, [128, P_TOT]) for l in range(2)]
        cT = sbt(es, "cT", [128, 8])
        modc = [sbt(es, "modc%d" % l, [128, 48]) for l in range(2)]
        gsm = [sbt(es, "gsm%d" % l, [128, 8]) for l in range(2)]
        gsf = [sbt(es, "gsf%d" % l, [128, 8]) for l in range(2)]
        gmrow = [sbt(es, "gmrow%d" % l, [128, D]) for l in range(2)]
        gfrow = [sbt(es, "gfrow%d" % l, [128, D]) for l in range(2)]
        onesr = sbt(es, "onesr", [1, 128])
        one1 = sbt(es, "one1", [1, 1])
        epsD = sbt(es, "epsD", [128, 1])
        epsG = sbt(es, "epsG", [128, 1])
        R_mod = S.res("mod")

        ident_f = cstf[:, 0:128]
        invf = cstf[:, 128:129]
        sgn = cstf[:, 129:130]
        ident_b = cstb[:, B_ID:B_ID + 128]
        U_b = cstb[:, B_U:B_U + 128]
        ones_b = cstb[:, B_ONES:B_ONES + 128]
        mcur_b = cstb[:, B_MCUR:B_MCUR + 512]
        mprev_b = cstb[:, B_MPREV:B_MPREV + 512]

        S.dma("sp", "cst", cstf[:], cstf_in, w=[R_c])
        S.dma("sp", "cst", cstb[:], cstb_in, w=[R_c])
        S.dma("sp", "cst", cT[:], cT_in, w=[R_c])
        selt = sbt(es, "selt", [128, 2])
        S.dma("sp", "cst", selt[:], sel_in, w=[R_c])
        for l in range(2):
            S.dma("sp", "cst", lp[l][:], lp_in[l], w=[R_c])
        R_c2 = S.res("const2")
        S.op("pool", MEMSET(onesr[:], 1.0), w=[R_c2])
        S.op("pool", MEMSET(one1[:], 1.0), w=[R_c2])
        S.op("pool", MEMSET(epsD[:], EPS), w=[R_c2])
        S.op("pool", MEMSET(epsG[:], EPS), w=[R_c2])

        with ExitStack() as pe_:
            cact = sbt(pe_, "cact", [128, 8]); R_cact = S.res()
            ctmp = sbt(pe_, "ctmp", [128, 8])
            modrow = sbt(pe_, "modrow", [1, 6 * D]); R_mr = S.res()
            adab = sbt(pe_, "adab", [1, 6 * D]); R_ab = S.res()
            stg = [sbt(pe_, "adstg%d" % i, [128, 8, 512]) for i in range(2)]
            R_stg = [S.res() for _ in range(2)]
            S.op("act", ACTF(ctmp[:], cT[:], AF.Exp, scale=-1.0), r=[R_c], w=[R_cact])
            S.op("dve", TS(ctmp[:], ctmp[:], 1.0, None, ALU.add), r=[R_cact], w=[R_cact])
            S.op("dve", lambda e: e.reciprocal(out=ctmp[:], in_=ctmp[:]), r=[R_cact], w=[R_cact])
            S.op("dve", TT(cact[:], ctmp[:], cT[:], ALU.mult), r=[R_cact, R_c], w=[R_cact])
            for l in range(2):
                S.dma("sp", "adab", adab[:], adab_in[l], w=[R_ab])
                for cc in range(12):
                    k = cc % 2
                    S.dma("sp", "adstg%d" % k, stg[k][:],
                          adaw_in[l][:, cc * 512:(cc + 1) * 512].rearrange("(c p) n -> p c n", p=128), w=[R_stg[k]])
                    pt, pr = PS()
                    S.op("pe", MMS([(pt[0:1, :], cact[:, dch:dch + 1], stg[k][:, dch, :], dch == 0, dch == 7)
                                    for dch in range(8)]), r=[R_cact, R_stg[k]], w=[pr])
                    S.op("dve", TT(modrow[0:1, cc * 512:(cc + 1) * 512], pt[0:1, :], adab[0:1, cc * 512:(cc + 1) * 512], ALU.add),
                         r=[R_ab], w=[pr, R_mr])
                pt, pr = PS()
                S.op("pe", MMS([(pt[:, j:j + 1], modrow[0:1, j * 128:(j + 1) * 128], one1[0:1, 0:1], True, True)
                                for j in range(48)]), r=[R_mr, R_c2], w=[pr])
                S.op("dve", CP(modc[l][:], pt[:, 0:48]), w=[pr, R_mod])
                for (dst, off) in ((gmrow[l], 2 * D), (gfrow[l], 5 * D)):
                    for hf in range(2):
                        pt, pr = PS()
                        S.op("pe", MMS([(pt[:, :], onesr[0:1, :], modrow[0:1, off + hf * 512: off + (hf + 1) * 512], True, True)]),
                             r=[R_mr, R_c2], w=[pr])
                        S.op("act", ACTF(dst[:, hf * 512:(hf + 1) * 512], pt[:, :], AF.Copy), w=[pr, R_mod])
                S.op("dve", STT(gsm[l][:], modc[l][:, 8:16], 1.0, lp[l][:, P_GM:P_GM + 8], ALU.add, ALU.mult), r=[R_c, R_mod], w=[R_mod])
                S.op("dve", STT(gsf[l][:], modc[l][:, 32:40], 1.0, lp[l][:, P_GF:P_GF + 8], ALU.add, ALU.mult), r=[R_c, R_mod], w=[R_mod])

        S.barrier()
        R_tab = [S.res("ropetab0"), S.res("ropetab1")]
        with ExitStack() as pe_:
            CH = min(1024, L)
            posi = sbt(pe_, "posi", [128, CH], I32); R_pi = S.res()
            ang = sbt(pe_, "ang", [128, CH]); R_ang = S.res()
            t0 = sbt(pe_, "rt0", [128, CH]); R_t0 = S.res()
            ti = sbt(pe_, "rti", [128, CH], I32); R_ti = S.res()
            t1 = sbt(pe_, "rt1", [128, CH]); R_t1 = S.res()
            t2 = sbt(pe_, "rt2", [128, CH]); R_t2 = S.res()
            tabs = [sbt(pe_, "rtab%d" % i, [128, CH]) for i in range(2)]
            R_tabs = [S.res() for _ in range(2)]
            for c0 in range(0, L, CH):
                S.dma("sp", "posi", posi[:], pos_in[0:1, c0:c0 + CH].partition_broadcast(128).rearrange("p o n -> p (o n)"), w=[R_pi])
                S.op("dve", CP(t0[:], posi[:]), r=[R_pi], w=[R_t0])
                S.op("dve", TS(ang[:], t0[:], invf), r=[R_t0, R_c], w=[R_ang])
                for which in range(2):
                    if which == 0:
                        S.op("dve", TS(t1[:], ang[:], PI / 2, None, ALU.add), r=[R_ang], w=[R_t1])
                        src = t1
                        R_src = R_t1
                    else:
                        src = ang
                        R_src = R_ang
                    S.op("dve", TS(t0[:], src[:], 1.0 / (2 * PI), 0.5, ALU.mult, ALU.add), r=[R_src], w=[R_t0])
                    S.op("dve", CP(ti[:], t0[:]), r=[R_t0], w=[R_ti])
                    S.op("dve", CP(t0[:], ti[:]), r=[R_ti], w=[R_t0])
                    S.op("dve", STT(t2[:], t0[:], -6.28125, src[:], ALU.mult, ALU.add), r=[R_t0, R_src], w=[R_t2])
                    S.op("dve", STT(t2[:], t0[:], -0.0019353071795864769, t2[:], ALU.mult, ALU.add), r=[R_t0, R_t2], w=[R_t2])
                    S.op("dve", TS(t0[:], t2[:], -PI, 2 * PI, ALU.is_lt, ALU.mult), r=[R_t2], w=[R_t0])
                    S.op("dve", TT(t2[:], t0[:], t2[:], ALU.add), r=[R_t0, R_t2], w=[R_t2])
                    S.op("dve", TS(t2[:], t2[:], PI, -PI, ALU.min, ALU.max), r=[R_t2], w=[R_t2])
                    if which == 0:
                        S.op("act", ACTF(tabs[0][:], t2[:], AF.Sin), r=[R_t2], w=[R_tabs[0]])
                        S.dma("sp", "tabst0", cos_d[:, c0:c0 + CH], tabs[0][:], r=[R_tabs[0]], w=[R_tab[0]])
                    else:
                        S.op("act", ACTF(t1[:], t2[:], AF.Sin), r=[R_t2], w=[R_t1])
                        S.op("dve", TS(tabs[1][:], t1[:], sgn), r=[R_t1, R_c], w=[R_tabs[1]])
                        S.dma("sp", "tabst1", sin_d[:, c0:c0 + CH], tabs[1][:], r=[R_tabs[1]], w=[R_tab[1]])

        S.barrier()
        def mixer(l, xsrc, R_src, xdst, R_dst):
            P = lp[l]
            with ExitStack() as me:
                winb = sbt(me, "winb", [128, 8, INW], BF16); R_win = S.res()
                woutb = sbt(me, "woutb", [128, 8, D], BF16); R_wout = S.res()
                negA = sbt(me, "negA", [128, 8]); esink = sbt(me, "esink", [128, 8])
                Dexp = sbt(me, "Dexp", [128, 8, 64]); R_lc = S.res()
                hst = sbt(me, "hst", [128, 512]); R_hst = S.res()
                hstb = sbt(me, "hstb", [128, 512], BF16); R_hstb = S.res()
                XD = 6
                kT = [sbt(me, "kT%d" % i, [128, 128], BF16) for i in range(4)]; R_kT = [S.res() for _ in range(4)]
                vaug = [sbt(me, "vaug%d" % i, [128, 2, 65], BF16) for i in range(4)]; R_va = [S.res() for _ in range(4)]
                xpre = [sbt(me, "xpre%d" % i, [128, 8, 132], BF16) for i in range(2)]; R_xp = [S.res() for _ in range(2)]
                dgw = sbt(me, "dgw", [128, 32, 128], BF16)
                xin = [sbt(me, "xin%d" % i, [128, D]) for i in range(XD)]; R_xin = [S.res() for _ in range(XD)]
                cs = [sbt(me, "cs%d" % i, [128, 2, 128]) for i in range(2)]; R_cs = [S.res() for _ in range(2)]
                junk = sbt(me, "junk", [128, D], BF16); R_junk = S.res()
                ss = sbt(me, "ss", [128, 1]); R_ss = S.res()
                lnv = sbt(me, "lnv", [128, 1]); rstd = sbt(me, "rstd", [128, 1]); R_rstd = S.res()
                xn = sbt(me, "xn", [128, D]); R_xn = S.res()
                hT = sbt(me, "hT", [128, 8, 128], BF16); R_hT = S.res()
                rt1 = sbt(me, "rt1", [128, 4, 128]); R_rt1 = S.res()
                rt2 = sbt(me, "rt2", [128, 4, 128]); R_rt2 = S.res()
                qT = [sbt(me, "qT%d" % i, [128, 4, 128], BF16) for i in range(3)]; R_qT = [S.res() for _ in range(3)]
                xdt = sbt(me, "xdt", [128, 8]); R_xdt = S.res()
                sp1 = sbt(me, "sp1", [128, 8]); R_sp1 = S.res()
                dt = [sbt(me, "dt%d" % i, [128, 8]) for i in range(3)]; R_dt = [S.res() for _ in range(3)]
                dtA = [sbt(me, "dtA%d" % i, [128, 8], BF16) for i in range(3)]; R_dtA = [S.res() for _ in range(3)]
                xbcT = [sbt(me, "xbcT%d" % i, [128, 8, 128], BF16) for i in range(3)]; R_xbcT = [S.res() for _ in range(3)]
                pTp = [sbt(me, "pTp%d" % g, [128, 512], BF16) for g in range(2)]; R_pTp = [S.res() for _ in range(2)]
                pTc = [sbt(me, "pTc%d" % g, [128, 512], BF16) for g in range(2)]; R_pTc = [S.res() for _ in range(2)]
                den = sbt(me, "den", [128, 8]); R_den = S.res()
                attn = [sbt(me, "attn%d" % i, [128, 8, 64], BF16) for i in range(3)]; R_attn = [S.res() for _ in range(3)]
                xd = [sbt(me, "xd%d" % i, [128, 8, 64], BF16) for i in range(2)]; R_xd = [S.res() for _ in range(2)]
                xdd = sbt(me, "xdd", [128, 8, 64], BF16); R_xdd = S.res()
                xst = [sbt(me, "xst%d" % i, [128, 512], BF16) for i in range(2)]; R_xst = [S.res() for _ in range(2)]
                Btok = [sbt(me, "Btok%d" % i, [128, 2, 128], BF16) for i in range(2)]; R_Bt = [S.res() for _ in range(2)]
                acs = sbt(me, "acs", [128, 16]); R_acs = S.res()
                nacs = [sbt(me, "nacs%d" % i, [128, 8]) for i in range(2)]; ea = [sbt(me, "ea%d" % i, [128, 8]) for i in range(2)]; dte = [sbt(me, "dte%d" % i, [128, 8]) for i in range(2)]
                cdec = [sbt(me, "cdec%d" % i, [128, 8]) for i in range(2)]; dif = [sbt(me, "dif%d" % i, [128, 8]) for i in range(2)]; R_sm = [S.res() for _ in range(2)]
                udta = sbt(me, "udta", [128, 8, 128], BF16); R_udta = S.res()
                dec = sbt(me, "dec", [128, 8, 128], BF16); R_dec = S.res()
                cbs = sbt(me, "cbs", [128, 2, 128], BF16); R_cbs = S.res()
                mt = [sbt(me, "mt%d" % i, [128, 8, 128], BF16) for i in range(2)]; R_mt = [S.res() for _ in range(2)]
                htmp = sbt(me, "htmp", [128, 512]); R_htmp = S.res()
                y1 = sbt(me, "y1", [128, 512]); R_y1 = S.res()
                y2 = sbt(me, "y2", [128, 512]); R_y2 = S.res()
                sz = [sbt(me, "sz%d" % i, [128, 512]) for i in range(4)]; R_sz = [S.res() for _ in range(4)]
                ssg = sbt(me, "ssg", [128, 2]); rg = sbt(me, "rg", [128, 2]); lng = sbt(me, "lng", [128, 2]); R_ssg = S.res()
                ssm = [sbt(me, "ssm%d" % i, [128, 512], BF16) for i in range(2)]; R_ssm = [S.res() for _ in range(2)]
                mixT = sbt(me, "mixT", [128, 8, 128], BF16); R_mixT = S.res()
                xo = sbt(me, "xo", [128, D]); R_xo = S.res()
                wstg = [sbt(me, "wstg%d" % i, [128, D]) for i in range(2)]; R_wstg = [S.res() for _ in range(2)]

                cast_eng = ["pool", "act", "dve"]
                wc = 0
                for dch in range(8):
                    for pc in range(3):
                        k = wc % 2
                        eng = cast_eng[wc % 3]
                        wc += 1
                        c0, c1 = pc * 984, (pc + 1) * 984
                        S.dma("sp", "wstg%d" % k, wstg[k][:, 0:984], win_in[l][dch * 128:(dch + 1) * 128, c0:c1], w=[R_wstg[k]])
                        if eng == "act":
                            S.op("act", ACTF(winb[:, dch, c0:c1], wstg[k][:, 0:984], AF.Copy), r=[R_wstg[k]], w=[R_win])
                        else:
                            S.op(eng, CP(winb[:, dch, c0:c1], wstg[k][:, 0:984]), r=[R_wstg[k]], w=[R_win])
                for dch in range(8):
                    k = dch % 2
                    S.dma("sp", "wstg%d" % k, wstg[k][:, 0:D], wout_in[l][dch * 128:(dch + 1) * 128, :], w=[R_wstg[k]])
                    eng = cast_eng[dch % 3]
                    if eng == "act":
                        S.op("act", ACTF(woutb[:, dch, :], wstg[k][:, 0:D], AF.Copy), r=[R_wstg[k]], w=[R_wout])
                    else:
                        S.op(eng, CP(woutb[:, dch, :], wstg[k][:, 0:D]), r=[R_wstg[k]], w=[R_wout])
                S.op("act", ACTF(negA[:], P[:, P_ALOG:P_ALOG + 8], AF.Exp), r=[R_c], w=[R_lc])
                S.op("dve", TS(negA[:], negA[:], -1.0), r=[R_lc], w=[R_lc])
                S.op("act", ACTF(esink[:], P[:, P_SK:P_SK + 8], AF.Exp), r=[R_c], w=[R_lc])
                S.op("pool", CP(Dexp[:], P[:, P_DSK:P_DSK + 8].unsqueeze(2).broadcast_to([128, 8, 64])), r=[R_c], w=[R_lc])
                S.op("dve", TT(dgw[:], ident_b.unsqueeze(1).broadcast_to([128, 32, 128]),
                               P[:, P_CW:P_CW + 32].unsqueeze(2).broadcast_to([128, 32, 128]), ALU.mult), r=[R_c], w=[R_lc])
                S.op("pool", MEMSET(hst[:], 0.0), w=[R_hst])
                S.op("pool", MEMSET(hstb[:], 0.0), w=[R_hstb])
                for i in range(4):
                    S.op("pool", MEMSET(vaug[i][:], 1.0), w=[R_va[i]])
                    S.op("pool", MEMSET(kT[i][:], 0.0), w=[R_kT[i]])
                for i in range(2):
                    S.op("pool", MEMSET(xpre[i][:], 0.0), w=[R_xp[i]])

                def load(i):
                    par = i % 2
                    S.dma("sp", "xin%d" % (i % XD), xin[i % XD][:], xsrc[i * 128:(i + 1) * 128, :], r=R_src, w=[R_xin[i % XD]])
                    S.dma("sp", "cs%d" % par, cs[par][:, 0, :], cos_d[:, i * 128:(i + 1) * 128], r=R_tab, w=[R_cs[par]])
                    S.dma("sp", "cs%d" % par, cs[par][:, 1, :], sin_d[:, i * 128:(i + 1) * 128], r=R_tab, w=[R_cs[par]])

                def stageA(i):
                    par = i % 2
                    if i + 1 < NB:
                        load(i + 1)
                    X = xin[i % XD]
                    yield S.op("act", ACTF(junk[:], X[:], AF.Square, accum_out=ss[:]), r=[R_xin[i % XD]], w=[R_junk, R_ss])
                    yield S.op("act", ACTF(lnv[:], ss[:], AF.Ln, bias=epsD[:], scale=1.0 / D), r=[R_ss, R_c2], w=[R_rstd])
                    yield S.op("act", ACTF(rstd[:], lnv[:], AF.Exp, scale=-0.5), r=[R_rstd], w=[R_rstd])
                    yield S.op("act", ACTF(xn[:], X[:], AF.Identity, scale=rstd[:]), r=[R_xin[i % XD], R_rstd], w=[R_xn])
                    pa, ra = yield from PSG()
                    pb, rb = yield from PSG()
                    yield S.op("pe", TRS([(pa[:, j * 128:(j + 1) * 128], xn[:, j * 128:(j + 1) * 128], ident_f) for j in range(4)]),
                         r=[R_xn, R_c], w=[ra])
                    yield S.op("pe", TRS([(pb[:, j * 128:(j + 1) * 128], xn[:, (4 + j) * 128:(5 + j) * 128], ident_f) for j in range(4)]),
                         r=[R_xn, R_c], w=[rb])
                    yield S.op("act", ACTS([(hT[:, j, :], pa[:, j * 128:(j + 1) * 128], AF.Identity,
                                       dict(scale=gsm[l][:, j:j + 1], bias=modc[l][:, j:j + 1])) for j in range(4)]),
                         r=[R_mod], w=[ra, R_hT])
                    yield S.op("act", ACTS([(hT[:, 4 + j, :], pb[:, j * 128:(j + 1) * 128], AF.Identity,
                                       dict(scale=gsm[l][:, 4 + j:5 + j], bias=modc[l][:, 4 + j:5 + j])) for j in range(4)]),
                         r=[R_mod], w=[rb, R_hT])

                    def fm_group(ptile, cols):
                        lst = []
                        for j, c0 in enumerate(cols):
                            for dch in range(8):
                                lst.append((ptile[:, j * 128:(j + 1) * 128], winb[:, dch, c0:c0 + 128], hT[:, dch, :], dch == 0, dch == 7))
                        return MMS(lst)
                    pq, rq = yield from PSG()
                    yield S.op("pe", fm_group(pq, [C_Q + 128 * j for j in range(4)]), r=[R_win, R_hT], w=[rq])
                    pqs, rqs = yield from PSG()
                    yield S.op("pe", fm_group(pqs, [C_QS + 128 * j for j in range(4)]), r=[R_win, R_hT], w=[rqs])
                    pk, rk = yield from PSG(2)
                    yield S.op("pe", fm_group(pk, [C_K, C_KS]), r=[R_win, R_hT], w=[rk])
                    cosb = cs[par][:, 0:1, :].broadcast_to([128, 4, 128])
                    sinb = cs[par][:, 1:2, :].broadcast_to([128, 4, 128])
                    pq3 = pq[:, :].rearrange("p (c t) -> p c t", c=4)
                    pqs3 = pqs[:, :].rearrange("p (c t) -> p c t", c=4)
                    yield S.op("dve", TT(rt1[:], pq3, cosb, ALU.mult), r=[R_cs[par]], w=[rq, R_rt1])
                    yield S.op("dve", TT(rt2[:], pqs3, sinb, ALU.mult), r=[R_cs[par]], w=[rqs, R_rt2])
                    yield S.op("dve", TT(qT[i % 3][:], rt1[:], rt2[:], ALU.add), r=[R_rt1, R_rt2], w=[R_qT[i % 3]])
                    yield S.op("dve", TT(rt1[:, 0, :], pk[:, 0:128], cs[par][:, 0, :], ALU.mult), r=[R_cs[par]], w=[rk, R_rt1])
                    yield S.op("dve", TT(rt2[:, 0, :], pk[:, 128:256], cs[par][:, 1, :], ALU.mult), r=[R_cs[par]], w=[rk, R_rt2])
                    yield S.op("dve", TT(kT[i % 4][:], rt1[:, 0, :], rt2[:, 0, :], ALU.add), r=[R_rt1, R_rt2], w=[R_kT[i % 4]])
                    pxa, rxa = yield from PSG()
                    yield S.op("pe", fm_group(pxa, [C_X + 128 * j for j in range(4)]), r=[R_win, R_hT], w=[rxa])
                    pxb, rxb = yield from PSG()
                    yield S.op("pe", fm_group(pxb, [C_X + 128 * (4 + j) for j in range(4)]), r=[R_win, R_hT], w=[rxb])
                    pv, rv = yield from PSG(2)
                    yield S.op("pe", MMS([(pv[:, 0:128], hT[:, dch, :], winb[:, dch, C_V:C_V + 128], dch == 0, dch == 7) for dch in range(8)]
                                   + [(pv[:, 128:136], hT[:, dch, :], winb[:, dch, C_DT:C_DT + 8], dch == 0, dch == 7) for dch in range(8)]),
                         r=[R_win, R_hT], w=[rv])
                    pz, rz = yield from PSG()
                    yield S.op("pe", MMS([(pz[:, :], hT[:, dch, :], winb[:, dch, C_Z:C_Z + 512], dch == 0, dch == 7) for dch in range(8)]),
                         r=[R_win, R_hT], w=[rz])
                    yield S.op("act", ACTF(vaug[i % 4][:, :, 0:64], pv[:, 0:128].rearrange("p (g d) -> p g d", g=2), AF.Copy), w=[rv, R_va[i % 4]])
                    yield S.op("dve", TT(xdt[:], pv[:, 128:136], P[:, P_DTB:P_DTB + 8], ALU.add), r=[R_c], w=[rv, R_xdt])
                    yield S.op("act", ACTF(sp1[:], xdt[:], AF.Abs), r=[R_xdt], w=[R_sp1])
                    yield S.op("act", ACTF(sp1[:], sp1[:], AF.Exp, scale=-1.0), r=[R_sp1], w=[R_sp1])
                    yield S.op("act", ACTF(sp1[:], sp1[:], AF.Ln, bias=1.0), r=[R_sp1], w=[R_sp1])
                    yield S.op("dve", STT(dt[i % 3][:], xdt[:], 0.0, sp1[:], ALU.max, ALU.add), r=[R_xdt, R_sp1], w=[R_dt[i % 3]])
                    yield S.op("dve", TT(dtA[i % 3][:], dt[i % 3][:], negA[:], ALU.mult), r=[R_dt[i % 3], R_lc], w=[R_dtA[i % 3]])
                    yield S.op("act", ACTF(xpre[par][:, 0:4, 3:131], pxa[:, :].rearrange("p (c t) -> p c t", c=4), AF.Copy), w=[rxa, R_xp[par]])
                    yield S.op("act", ACTF(xpre[par][:, 4:8, 3:131], pxb[:, :].rearrange("p (c t) -> p c t", c=4), AF.Copy), w=[rxb, R_xp[par]])
                    yield S.op("act", ACTF(sz[i % 4][:], pz[:, :], AF.Silu), w=[rz, R_sz[i % 4]])

                def stageA2(i):
                    par = i % 2
                    for hf in range(2):
                        pcv, rcv = yield from PSG()
                        yield S.op("pe", MMS([(pcv[:, j * 128:(j + 1) * 128], dgw[:, (hf * 4 + j) * 4 + k_, :], xpre[par][:, hf * 4 + j, k_:k_ + 128],
                                               k_ == 0, k_ == 3) for j in range(4) for k_ in range(4)]), r=[R_xp[par], R_lc], w=[rcv])
                        if hf == 1:
                            yield S.op("pool", CP(xpre[1 - par][:, :, 0:3], xpre[par][:, :, 128:131]), r=[R_xp[par]], w=[R_xp[1 - par]])
                        yield S.op("act", ACTS([(xbcT[i % 3][:, hf * 4 + j, :], pcv[:, j * 128:(j + 1) * 128], AF.Silu,
                                                 dict(bias=P[:, P_CB + hf * 4 + j:P_CB + hf * 4 + j + 1])) for j in range(4)]),
                             r=[R_c], w=[rcv, R_xbcT[i % 3]])

                def stageB(i):
                    par = i % 2
                    q2 = qT[i % 3][:].rearrange("p c t -> p (c t)")
                    for g in range(2):
                        gs_ = slice(g * 64, (g + 1) * 64)
                        if i > 0:
                            pp, rp = yield from PSG()
                            yield S.op("pe", MMS([(pp[:, :], kT[(i - 1) % 4][gs_, :], q2[gs_, :], True, False),
                                            (pp[:, :], ident_b, mprev_b, False, True)]),
                                 r=[R_kT[(i - 1) % 4], R_qT[i % 3], R_c], w=[rp])
                            yield S.op("act", ACTF(pTp[g][:], pp[:, :], AF.Exp, scale=0.125), w=[rp, R_pTp[g]])
                        pc, rc = yield from PSG()
                        yield S.op("pe", MMS([(pc[:, :], kT[i % 4][gs_, :], q2[gs_, :], True, False),
                                        (pc[:, :], ident_b, mcur_b, False, True)]),
                             r=[R_kT[i % 4], R_qT[i % 3], R_c], w=[rc])
                        yield S.op("act", ACTF(pTc[g][:], pc[:, :], AF.Exp, scale=0.125), w=[rc, R_pTc[g]])
                    for g in range(2):
                        po, ro = yield from PSG(2)
                        lst = []
                        for j in range(4):
                            o_ = po[:, j * 65:(j + 1) * 65]
                            if i > 0:
                                lst.append((o_, pTp[g][:, j * 128:(j + 1) * 128], vaug[(i - 1) % 4][:, g, :], True, False))
                                lst.append((o_, pTc[g][:, j * 128:(j + 1) * 128], vaug[i % 4][:, g, :], False, True))
                            else:
                                lst.append((o_, pTc[g][:, j * 128:(j + 1) * 128], vaug[i % 4][:, g, :], True, True))
                        yield S.op("pe", MMS(lst), r=[R_pTp[g], R_pTc[g], R_va[i % 4], R_va[(i - 1) % 4]], w=[ro])
                        po3 = po[:, 0:260].rearrange("p (h d) -> p h d", h=4)
                        yield S.op("dve", TT(den[:, g * 4:(g + 1) * 4], po3[:, :, 64], esink[:, g * 4:(g + 1) * 4], ALU.add), r=[R_lc], w=[ro, R_den])
                        yield S.op("dve", lambda e, g=g: e.reciprocal(out=den[:, g * 4:(g + 1) * 4], in_=den[:, g * 4:(g + 1) * 4]), r=[R_den], w=[R_den])
                        yield S.op("dve", TT(attn[i % 3][:, g * 4:(g + 1) * 4, :], po3[:, :, 0:64],
                                       den[:, g * 4:(g + 1) * 4].unsqueeze(2).broadcast_to([128, 4, 64]), ALU.mult),
                             r=[R_den], w=[ro, R_attn[i % 3]])

                def stageB2(i):
                    par = i % 2
                    ptx, rtx = yield from PSG(3)
                    ptxb = ptx[:, :].bitcast(BF16)
                    yield S.op("pe", TRS([(ptxb[:, j * 128:(j + 1) * 128], xbcT[i % 3][:, j, :], ident_b) for j in range(6)]), r=[R_xbcT[i % 3], R_c], w=[rtx])
                    yield S.op("dve", TT(xd[par][:], ptxb[:, 0:512].rearrange("p (h d) -> p h d", h=8),
                                   dt[i % 3][:].unsqueeze(2).broadcast_to([128, 8, 64]), ALU.mult), r=[R_dt[i % 3]], w=[rtx, R_xd[par]])
                    yield S.op("act", ACTF(xst[par][:], ptxb[:, 0:512], AF.Copy), w=[rtx, R_xst[par]])
                    yield S.op("act", ACTF(Btok[par][:], ptxb[:, 512:768].rearrange("p (g n) -> p g n", g=2), AF.Copy), w=[rtx, R_Bt[par]])
                    pac, rac = yield from PSG()
                    yield S.op("pe", MMS([(pac[:, 0:8], U_b, dtA[i % 3][:], True, True), (pac[:, 8:16], ones_b, dtA[i % 3][:], True, True)]),
                         r=[R_dtA[i % 3], R_c], w=[rac])
                    yield S.op("act", ACTF(acs[:], pac[:, 0:16], AF.Copy), w=[rac, R_acs])
                    yield S.op("dve", TT(dif[par][:], acs[:, 8:16], acs[:, 0:8], ALU.subtract), r=[R_acs], w=[R_sm[par]])
                    yield S.op("dve", TS(nacs[par][:], acs[:, 0:8], -1.0), r=[R_acs], w=[R_sm[par]])
                    yield S.op("act", ACTF(dte[par][:], dif[par][:], AF.Exp), r=[R_sm[par]], w=[R_sm[par]])
                    yield S.op("act", ACTF(cdec[par][:], acs[:, 8:16], AF.Exp), r=[R_acs], w=[R_sm[par]])
                    yield S.op("act", ACTF(ea[par][:], acs[:, 0:8], AF.Exp), r=[R_acs], w=[R_sm[par]])
                    yield S.op("pool", TT(udta[:], U_b.unsqueeze(1).broadcast_to([128, 8, 128]),
                                    dtA[i % 3][:].unsqueeze(2).broadcast_to([128, 8, 128]), ALU.mult), r=[R_dtA[i % 3], R_c], w=[R_udta])
                    u2 = udta[:].rearrange("p h t -> p (h t)")
                    for hf in range(2):
                        pd, rd = yield from PSG()
                        yield S.op("pe", MMS([(pd[:, :], ones_b, u2[:, hf * 512:(hf + 1) * 512], True, False),
                                        (pd[:, :], ident_b, mcur_b, False, True)]), r=[R_udta, R_c], w=[rd])
                        yield S.op("act", ACTS([(dec[:, hf * 4 + j, :], pd[:, j * 128:(j + 1) * 128], AF.Exp,
                                           dict(bias=nacs[par][:, hf * 4 + j:hf * 4 + j + 1])) for j in range(4)]), r=[R_sm[par]], w=[rd, R_dec])
                    pcb, rcb = yield from PSG()
                    yield S.op("pe", MMS([(pcb[:, g * 128:(g + 1) * 128], xbcT[i % 3][:, 4 + g, :], xbcT[i % 3][:, 6 + g, :], True, True) for g in range(2)]),
                         r=[R_xbcT[i % 3]], w=[rcb])
                    yield S.op("act", ACTF(cbs[:], pcb[:, 0:256].rearrange("p (g t) -> p g t", g=2), AF.Copy), w=[rcb, R_cbs])
                    yield S.op("dve", TT(mt[par][:].rearrange("p (g r) t -> p g r t", g=2), dec[:].rearrange("p (g r) t -> p g r t", g=2),
                                    cbs[:].unsqueeze(2).broadcast_to([128, 2, 4, 128]), ALU.mult), r=[R_dec, R_cbs], w=[R_mt[par]])
                def stageB2b(i):
                    par = i % 2
                    py, ry = yield from PSG()
                    yield S.op("pe", MMS([(py[:, h * 64:(h + 1) * 64], mt[par][:, h, :], xd[par][:, h, :], True, True) for h in range(8)]),
                         r=[R_mt[par], R_xd[par]], w=[ry])
                    pyo, ryo = yield from PSG()
                    yield S.op("pe", MMS([(pyo[:, g * 256:(g + 1) * 256], xbcT[i % 3][:, 6 + g, :], hstb[:, g * 256:(g + 1) * 256], True, True) for g in range(2)]),
                         r=[R_xbcT[i % 3], R_hstb], w=[ryo])
                    yield S.op("pool", TT(xdd[:], xd[par][:], dte[par][:].unsqueeze(2).broadcast_to([128, 8, 64]), ALU.mult), r=[R_xd[par], R_sm[par]], w=[R_xdd])
                    x2 = xdd[:].rearrange("p h d -> p (h d)")
                    pst, rst = yield from PSG()
                    yield S.op("pe", MMS([(pst[:, g * 256:(g + 1) * 256], Btok[par][:, g, :], x2[:, g * 256:(g + 1) * 256], True, True) for g in range(2)]),
                         r=[R_Bt[par], R_xdd], w=[rst])
                    yield S.op("dve", TT(htmp[:].rearrange("p (h d) -> p h d", h=8), hst[:].rearrange("p (h d) -> p h d", h=8),
                                    cdec[par][:].unsqueeze(2).broadcast_to([128, 8, 64]), ALU.mult), r=[R_hst, R_sm[par]], w=[R_htmp])
                    yield S.op("dve", TT(hst[:], htmp[:], pst[:, :], ALU.add), r=[R_htmp], w=[rst, R_hst])
                    yield S.op("dve", TT(y1[:].rearrange("p (h d) -> p h d", h=8), pyo[:, :].rearrange("p (h d) -> p h d", h=8),
                                   ea[par][:].unsqueeze(2).broadcast_to([128, 8, 64]), ALU.mult), r=[R_sm[par]], w=[ryo, R_y1])
                    yield S.op("act", ACTF(hstb[:], hst[:], AF.Copy), r=[R_hst], w=[R_hstb])
                    yield S.op("dve", TT(y1[:], y1[:], py[:, :], ALU.add), r=[R_y1], w=[ry, R_y1])
                    yield S.op("pool", TT(y2[:], xst[par][:], Dexp[:].rearrange("p h d -> p (h d)"), ALU.mult), r=[R_xst[par], R_lc], w=[R_y2])
                    yield S.op("dve", TT(y2[:], y2[:], y1[:], ALU.add), r=[R_y1, R_y2], w=[R_y2])
                    yield S.op("dve", TT(y1[:], y2[:], sz[i % 4][:], ALU.mult), r=[R_y2, R_sz[i % 4]], w=[R_y1])
                    yield S.op("act", ACTS([(junk[:, g * 256:(g + 1) * 256], y1[:, g * 256:(g + 1) * 256], AF.Square,
                                       dict(accum_out=ssg[:, g:g + 1])) for g in range(2)]), r=[R_y1], w=[R_junk, R_ssg])
                    yield S.op("act", ACTF(lng[:], ssg[:], AF.Ln, bias=epsG[:], scale=1.0 / 256), r=[R_ssg, R_c2], w=[R_ssg])
                    yield S.op("act", ACTF(rg[:], lng[:], AF.Exp, scale=-0.5), r=[R_ssg], w=[R_ssg])
                    yield S.op("dve", TT(y2[:].rearrange("p (g c) -> p g c", g=2), y1[:].rearrange("p (g c) -> p g c", g=2),
                                   rg[:].unsqueeze(2).broadcast_to([128, 2, 256]), ALU.mult), r=[R_y1, R_ssg], w=[R_y2])
                    yield S.op("dve", TT(ssm[par][:], y2[:], P[:, P_SSMG:P_SSMG + 512], ALU.mult), r=[R_y2, R_c], w=[R_ssm[par]])

                def stageC(i):
                    par = i % 2
                    X = xin[i % XD]
                    pm, rm = yield from PSG()
                    pmb = pm[:, :].bitcast(BF16)
                    a2 = attn[i % 3][:].rearrange("p h d -> p (h d)")
                    yield S.op("pe", TRS([(pmb[:, j * 128:(j + 1) * 128], a2[:, j * 128:(j + 1) * 128], ident_b) for j in range(4)]
                                   + [(pmb[:, (4 + j) * 128:(5 + j) * 128], ssm[par][:, j * 128:(j + 1) * 128], ident_b) for j in range(4)]),
                         r=[R_attn[i % 3], R_ssm[par], R_c], w=[rm])
                    yield S.op("act", ACTF(mixT[:].rearrange("p c t -> p (c t)"), pmb[:, :], AF.Copy), w=[rm, R_mixT])
                    for hf in range(2):
                        pop, rop = yield from PSG()
                        yield S.op("pe", MMS([(pop[:, :], mixT[:, fch, :], woutb[:, fch, hf * 512:(hf + 1) * 512], fch == 0, fch == 7) for fch in range(8)]),
                             r=[R_mixT, R_wout], w=[rop])
                        yield S.op("dve", TT(xo[:, hf * 512:(hf + 1) * 512], pop[:, :], gmrow[l][:, hf * 512:(hf + 1) * 512], ALU.mult),
                             r=[R_mod], w=[rop, R_xo])
                    yield S.op("dve", TT(X[:], X[:], xo[:], ALU.add), r=[R_xo, R_xin[i % XD]], w=[R_xin[i % XD]])
                    yield S.dma("sp", "xst%d" % (i % XD), xdst[i * 128:(i + 1) * 128, :], X[:], r=[R_xin[i % XD]], w=[R_dst[par]])
                load(0)
                for t in range(NB + 4):
                    gens = []
                    if t < NB:
                        gens.append((stageA(t), 1))
                    if 0 <= t - 1 < NB:
                        gens.append((stageA2(t - 1), 1))
                    if 0 <= t - 2 < NB:
                        gens.append((stageB(t - 2), 1))
                        gens.append((stageB2(t - 2), 1))
                    if 0 <= t - 3 < NB:
                        gens.append((stageB2b(t - 3), 1))
                    if 0 <= t - 4 < NB:
                        gens.append((stageC(t - 4), 1))
                    while gens:
                        for it_ in list(gens):
                            g_, w_ = it_
                            try:
                                for _ in range(w_):
                                    next(g_)
                            except StopIteration:
                                gens.remove(it_)

        def chanmix(l, xsrc, R_src, xdst, R_dst, moe, final, ntok, blend):
            P = lp[l]
            NT = ntok // TTOK
            NF = (EXD if moe else FFN) // 128
            NE = NEXP if moe else 1
            GF = 2
            with ExitStack() as me:
                xt = sbt(me, "xt", [128, NBT, D]); R_xt = S.res()
                hTt = sbt(me, "hTt", [128, 8, TTOK], BF16); R_hTt = S.res()
                xn = [sbt(me, "cxn%d" % i, [128, D]) for i in range(2)]; R_xn = [S.res() for _ in range(2)]
                junk = sbt(me, "cjunk", [128, D], BF16); R_junk = S.res()
                ss = sbt(me, "css", [128, 1]); lnv = sbt(me, "clnv", [128, 1]); rstd = sbt(me, "crstd", [128, 1]); R_ss = S.res()
                ss3 = [sbt(me, "css3_%d" % i, [128, 1]) for i in range(3)]; lnv3 = [sbt(me, "clnv3_%d" % i, [128, 1]) for i in range(3)]
                rstd3 = [sbt(me, "crstd3_%d" % i, [128, 1]) for i in range(3)]; R_ss3 = [S.res() for _ in range(3)]
                xn3 = [sbt(me, "cxn3_%d" % i, [128, D]) for i in range(3)]; R_xn3 = [S.res() for _ in range(3)]
                actT = [sbt(me, "actT%d" % i, [128, GF, TTOK], BF16) for i in range(2)]; R_actT = [S.res() for _ in range(2)]
                sg = [sbt(me, "sg%d" % i, [128, 512], BF16) for i in range(2)]; R_sg = [S.res() for _ in range(2)]
                NSLOT = 3 * GF
                gstg = [sbt(me, "gstg%d" % i, [128, 8, 128]) for i in range(3)]; R_gstg = [S.res() for _ in range(3)]
                ustg = [sbt(me, "ustg%d" % i, [128, 8, 128]) for i in range(3)]; R_ustg = [S.res() for _ in range(3)]
                dstg = [sbt(me, "dstg%d" % i, [128, D]) for i in range(3)]; R_dstg = [S.res() for _ in range(3)]
                wgb = [sbt(me, "wgb%d" % i, [128, 8, 128], BF16) for i in range(NSLOT)]; R_wgb = [S.res() for _ in range(NSLOT)]
                wub = [sbt(me, "wub%d" % i, [128, 8, 128], BF16) for i in range(NSLOT)]; R_wub = [S.res() for _ in range(NSLOT)]
                wdb = [sbt(me, "wdb%d" % i, [128, D], BF16) for i in range(NSLOT)]; R_wdb = [S.res() for _ in range(NSLOT)]
                if moe:
                    hTf = sbt(me, "hTf", [128, 8, 128]); R_hTf = S.res()
                    lg = sbt(me, "lg", [128, 8]); top8 = sbt(me, "top8", [128, 8]); R_lg = S.res()
                    gt = sbt(me, "gt", [128, 4]); R_gt = S.res()
                    m1 = sbt(me, "m1", [128, 8]); m2 = sbt(me, "m2", [128, 8]); R_m = S.res()
                    wgt = sbt(me, "wgt", [128, NBT, 8]); R_wgt = S.res()
                if blend:
                    xB = [sbt(me, "xB%d" % i, [128, D]) for i in range(2)]; R_xB = [S.res() for _ in range(2)]
                if final:
                    fng = sbt(me, "fng", [128, D]); R_fng = S.res()
                    S.dma("sp", "fng", fng[:], fng_in, w=[R_fng])
                    ob = [sbt(me, "ob%d" % i, [128, D]) for i in range(2)]; R_ob = [S.res() for _ in range(2)]
                slot = [0]
                stgc = [0]
                castc = [0]
                cast_eng = ["act", "act", "act", "dve"]
                for t in range(NT):
                    def cblk(b):
                        k = b % 3
                        row0 = t * TTOK + b * 128
                        tc = slice(b * 128, (b + 1) * 128)
                        S.dma("sp", "cxt%d" % b, xt[:, b, :], xsrc[row0:row0 + 128, :], r=R_src, w=[R_xt])
                        yield None
                        yield S.op("act", ACTF(junk[:], xt[:, b, :], AF.Square, accum_out=ss3[k][:]), r=[R_xt], w=[R_junk, R_ss3[k]])
                        yield S.op("act", ACTF(lnv3[k][:], ss3[k][:], AF.Ln, bias=epsD[:], scale=1.0 / D), r=[R_ss3[k], R_c2], w=[R_ss3[k]])
                        yield S.op("act", ACTF(rstd3[k][:], lnv3[k][:], AF.Exp, scale=-0.5), r=[R_ss3[k]], w=[R_ss3[k]])
                        yield S.op("act", ACTF(xn3[k][:], xt[:, b, :], AF.Identity, scale=rstd3[k][:]), r=[R_xt, R_ss3[k]], w=[R_xn3[k]])
                        pa, ra = yield from PSG()
                        pb, rb = yield from PSG()
                        yield S.op("pe", TRS([(pa[:, j * 128:(j + 1) * 128], xn3[k][:, j * 128:(j + 1) * 128], ident_f) for j in range(4)]),
                             r=[R_xn3[k], R_c], w=[ra])
                        yield S.op("pe", TRS([(pb[:, j * 128:(j + 1) * 128], xn3[k][:, (4 + j) * 128:(5 + j) * 128], ident_f) for j in range(4)]),
                             r=[R_xn3[k], R_c], w=[rb])
                        yield S.op("act", ACTS([(hTt[:, j, tc], pa[:, j * 128:(j + 1) * 128], AF.Identity,
                                           dict(scale=gsf[l][:, j:j + 1], bias=modc[l][:, 24 + j:25 + j])) for j in range(4)]),
                             r=[R_mod], w=[ra, R_hTt])
                        yield S.op("act", ACTS([(hTt[:, 4 + j, tc], pb[:, j * 128:(j + 1) * 128], AF.Identity,
                                           dict(scale=gsf[l][:, 4 + j:5 + j], bias=modc[l][:, 28 + j:29 + j])) for j in range(4)]),
                             r=[R_mod], w=[rb, R_hTt])
                    fast = (not moe) and (not blend)
                    if fast:
                        act_g = []
                        nxt = 0
                        while nxt < NBT or act_g:
                            while len(act_g) < 3 and nxt < NBT:
                                act_g.append(cblk(nxt))
                                nxt += 1
                            for g_ in list(act_g):
                                try:
                                    next(g_)
                                except StopIteration:
                                    act_g.remove(g_)
                    for b in (range(0) if fast else range(NBT)):
                        row0 = t * TTOK + b * 128
                        k = b % 2
                        S.dma("sp", "cxt%d" % b, xt[:, b, :], xsrc[row0:row0 + 128, :], r=R_src, w=[R_xt])
                        if blend:
                            S.dma("sp", "cxB%d" % k, xB[k][:], xsrc[ntok + row0:ntok + row0 + 128, :], r=R_src, w=[R_xB[k]])
                            S.op("dve", TS(xB[k][:], xB[k][:], selt[:, 1:2]), r=[R_xB[k], R_c], w=[R_xB[k]])
                            S.op("dve", STT(xt[:, b, :], xt[:, b, :], selt[:, 0:1], xB[k][:], ALU.mult, ALU.add), r=[R_xB[k], R_c, R_xt], w=[R_xt])
                        S.op("act", ACTF(junk[:], xt[:, b, :], AF.Square, accum_out=ss[:]), r=[R_xt], w=[R_junk, R_ss])
                        S.op("act", ACTF(lnv[:], ss[:], AF.Ln, bias=epsD[:], scale=1.0 / D), r=[R_ss, R_c2], w=[R_ss])
                        S.op("act", ACTF(rstd[:], lnv[:], AF.Exp, scale=-0.5), r=[R_ss], w=[R_ss])
                        S.op("act", ACTF(xn[k][:], xt[:, b, :], AF.Identity, scale=rstd[:]), r=[R_xt, R_ss], w=[R_xn[k]])
                        pa, ra = PS()
                        pb, rb = PS()
                        S.op("pe", TRS([(pa[:, j * 128:(j + 1) * 128], xn[k][:, j * 128:(j + 1) * 128], ident_f) for j in range(4)]),
                             r=[R_xn[k], R_c], w=[ra])
                        S.op("pe", TRS([(pb[:, j * 128:(j + 1) * 128], xn[k][:, (4 + j) * 128:(5 + j) * 128], ident_f) for j in range(4)]),
                             r=[R_xn[k], R_c], w=[rb])
                        tc = slice(b * 128, (b + 1) * 128)
                        if not moe:
                            S.op("act", ACTS([(hTt[:, j, tc], pa[:, j * 128:(j + 1) * 128], AF.Identity,
                                               dict(scale=gsf[l][:, j:j + 1], bias=modc[l][:, 24 + j:25 + j])) for j in range(4)]),
                                 r=[R_mod], w=[ra, R_hTt])
                            S.op("act", ACTS([(hTt[:, 4 + j, tc], pb[:, j * 128:(j + 1) * 128], AF.Identity,
                                               dict(scale=gsf[l][:, 4 + j:5 + j], bias=modc[l][:, 28 + j:29 + j])) for j in range(4)]),
                                 r=[R_mod], w=[rb, R_hTt])
                        else:
                            S.op("act", ACTS([(hTf[:, j, :], pa[:, j * 128:(j + 1) * 128], AF.Identity,
                                               dict(scale=gsf[l][:, j:j + 1], bias=modc[l][:, 24 + j:25 + j])) for j in range(4)]),
                                 r=[R_mod], w=[ra, R_hTf])
                            S.op("act", ACTS([(hTf[:, 4 + j, :], pb[:, j * 128:(j + 1) * 128], AF.Identity,
                                               dict(scale=gsf[l][:, 4 + j:5 + j], bias=modc[l][:, 28 + j:29 + j])) for j in range(4)]),
                                 r=[R_mod], w=[rb, R_hTf])
                            S.op("pool", CP(hTt[:, :, tc], hTf[:]), r=[R_hTf], w=[R_hTt])
                            pr_, rr_ = PS()
                            S.op("pe", MMS([(pr_[:, 0:8], hTf[:, dch, :], P[:, P_RW + dch * 8:P_RW + dch * 8 + 8], dch == 0, dch == 7) for dch in range(8)]),
                                 r=[R_hTf, R_c], w=[rr_])
                            S.op("dve", CP(lg[:], pr_[:, 0:8]), w=[rr_, R_lg])
                            S.op("dve", lambda e: e.max(out=top8[:], in_=lg[:]), r=[R_lg], w=[R_lg])
                            S.op("dve", TT(gt[:, 0:1], top8[:, 1:2], top8[:, 0:1], ALU.subtract), r=[R_lg], w=[R_gt])
                            S.op("act", ACTF(gt[:, 1:2], gt[:, 0:1], AF.Exp), r=[R_gt], w=[R_gt])
                            S.op("dve", TS(gt[:, 1:2], gt[:, 1:2], 1.0, None, ALU.add), r=[R_gt], w=[R_gt])
                            S.op("dve", lambda e: e.reciprocal(out=gt[:, 2:3], in_=gt[:, 1:2]), r=[R_gt], w=[R_gt])
                            S.op("dve", TS(gt[:, 3:4], gt[:, 2:3], -1.0, 1.0, ALU.mult, ALU.add), r=[R_gt], w=[R_gt])
                            S.op("dve", TS(m1[:], lg[:], top8[:, 0:1], gt[:, 2:3], ALU.is_equal, ALU.mult), r=[R_lg, R_gt], w=[R_m])
                            S.op("dve", TS(m2[:], lg[:], top8[:, 1:2], gt[:, 3:4], ALU.is_equal, ALU.mult), r=[R_lg, R_gt], w=[R_m])
                            S.op("dve", TT(wgt[:, b, :], m1[:], m2[:], ALU.add), r=[R_m], w=[R_wgt])
                    for e_ in range(NE):
                        if moe:
                            Wg, Wu, Wd = mg_in[e_], mu_in[e_], md_in[e_]
                        else:
                            Wg, Wu, Wd = fg_in, fu_in, fd_in
                        for fg in range(NF // GF):
                            ab = fg % 2
                            slots = []
                            for fi in range(GF):
                                fc = fg * GF + fi
                                s_ = slot[0] % NSLOT
                                slot[0] += 1
                                slots.append(s_)
                                k = stgc[0] % 3
                                stgc[0] += 1
                                S.dma("sp", "gstg%d" % k, gstg[k][:], Wg[:, fc * 128:(fc + 1) * 128].rearrange("(c p) n -> p c n", p=128), w=[R_gstg[k]])
                                S.dma("sp", "ustg%d" % k, ustg[k][:], Wu[:, fc * 128:(fc + 1) * 128].rearrange("(c p) n -> p c n", p=128), w=[R_ustg[k]])
                                S.dma("sp", "dstg%d" % k, dstg[k][:], Wd[fc * 128:(fc + 1) * 128, :], w=[R_dstg[k]])
                                for (src_, R_s, dst_, R_d) in ((gstg[k], R_gstg[k], wgb[s_], R_wgb[s_]), (ustg[k], R_ustg[k], wub[s_], R_wub[s_])):
                                    ce = cast_eng[castc[0] % 4]
                                    castc[0] += 1
                                    if ce == "act":
                                        S.op("act", ACTF(dst_[:], src_[:], AF.Copy), r=[R_s], w=[R_d])
                                    else:
                                        S.op(ce, CP(dst_[:], src_[:]), r=[R_s], w=[R_d])
                                S.op("dve", TT(wdb[s_][:], dstg[k][:], gfrow[l][:], ALU.mult), r=[R_dstg[k], R_mod], w=[R_wdb[s_]])
                                SW = min(512, TTOK)
                                for sub in range(TTOK // SW):
                                    tcs = slice(sub * SW, (sub + 1) * SW)
                                    pg, rg_ = PS()
                                    S.op("pe", MMS([(pg[:, 0:SW], wgb[s_][:, dch, :], hTt[:, dch, tcs], dch == 0, dch == 7) for dch in range(8)]),
                                         r=[R_wgb[s_], R_hTt], w=[rg_])
                                    pu, ru = PS()
                                    S.op("pe", MMS([(pu[:, 0:SW], wub[s_][:, dch, :], hTt[:, dch, tcs], dch == 0, dch == 7) for dch in range(8)]),
                                         r=[R_wub[s_], R_hTt], w=[ru])
                                    kk = sub % 2
                                    S.op("act", ACTF(sg[kk][:, 0:SW], pg[:, 0:SW], AF.Silu), w=[rg_, R_sg[kk]])
                                    S.op("dve", TT(actT[ab][:, fi, tcs], sg[kk][:, 0:SW], pu[:, 0:SW], ALU.mult), r=[R_sg[kk]], w=[ru, R_actT[ab]])
                            for b in range(NBT):
                                for hf in range(2):
                                    pd, rd = PS()
                                    S.op("pe", MMS([(pd[:, :], actT[ab][:, fi, b * 128:(b + 1) * 128], wdb[slots[fi]][:, hf * 512:(hf + 1) * 512],
                                                     fi == 0, fi == GF - 1) for fi in range(GF)]),
                                         r=[R_actT[ab]] + [R_wdb[s_] for s_ in slots], w=[rd])
                                    xs_ = xt[:, b, hf * 512:(hf + 1) * 512]
                                    if moe:
                                        S.op("dve", STT(xs_, pd[:, :], wgt[:, b, e_:e_ + 1], xs_, ALU.mult, ALU.add), r=[R_wgt], w=[rd, R_xt])
                                    else:
                                        S.op("dve", TT(xs_, pd[:, :], xs_, ALU.add), w=[rd, R_xt])
                    for b in range(NBT):
                        row0 = t * TTOK + b * 128
                        if final:
                            k = b % 2
                            S.op("act", ACTF(junk[:], xt[:, b, :], AF.Square, accum_out=ss[:]), r=[R_xt], w=[R_junk, R_ss])
                            S.op("act", ACTF(lnv[:], ss[:], AF.Ln, bias=epsD[:], scale=1.0 / D), r=[R_ss, R_c2], w=[R_ss])
                            S.op("act", ACTF(rstd[:], lnv[:], AF.Exp, scale=-0.5), r=[R_ss], w=[R_ss])
                            S.op("dve", STT(ob[k][:], xt[:, b, :], rstd[:], fng[:], ALU.mult, ALU.mult), r=[R_xt, R_ss, R_fng], w=[R_ob[k]])
                            S.dma("sp", "cst%d" % k, xdst[row0:row0 + 128, :], ob[k][:], r=[R_ob[k]], w=[R_dst[k]])
                        else:
                            S.dma("sp", "cstx", xdst[row0:row0 + 128, :], xt[:, b, :], r=[R_xt], w=[R_dst[0]])

        def moe_sparse(l, xsrc, R_src, xdst, R_dst, ntok, blend):
            P = lp[l]
            NBL = ntok // 128
            NF = NFM
            GF = 4
            U32 = mybir.dt.uint32
            IOA = bass.IndirectOffsetOnAxis
            with ExitStack() as oe:
                d1i = sbt(oe, "d1i", [128, NBL], I32); d2i = sbt(oe, "d2i", [128, NBL], I32); R_di = S.res()
                g12 = sbt(oe, "g12", [128, NBL, 2]); R_g12 = S.res()
                idxw = sbt(oe, "idxw", [128, NSL, NF], I32); R_idxw = S.res()
                cst2 = sbt(oe, "cst2", [128, NSL + 1 + NF]); R_cst2 = S.res()
                S.dma("sp", "cst2", cst2[:], cst2_in, w=[R_cst2])
                R_hs = S.res(); R_ys = S.res()

                def load_x(b, xt1, R_xt1, xB, R_xB, k):
                    row0 = b * 128
                    S.dma("sp", "mx%d" % k, xt1[k][:], xsrc[row0:row0 + 128, :], r=R_src, w=[R_xt1[k]])
                    if blend:
                        S.dma("sp", "mxB%d" % k, xB[k][:], xsrc[ntok + row0:ntok + row0 + 128, :], r=R_src, w=[R_xB[k]])
                        S.op("dve", TS(xB[k][:], xB[k][:], selt[:, 1:2]), r=[R_xB[k], R_c], w=[R_xB[k]])
                        S.op("dve", STT(xt1[k][:], xt1[k][:], selt[:, 0:1], xB[k][:], ALU.mult, ALU.add), r=[R_xB[k], R_c, R_xt1[k]], w=[R_xt1[k]])

                with ExitStack() as me:
                    H = sbt(me, "H", [128, NBL, D], BF16); R_H = S.res()
                    xt1 = [sbt(me, "m1x%d" % i, [128, D]) for i in range(3)]; R_xt1 = [S.res() for _ in range(3)]
                    xB = [sbt(me, "m1xB%d" % i, [128, D]) for i in range(3)]; R_xB = [S.res() for _ in range(3)]
                    xn = [sbt(me, "m1xn%d" % i, [128, D]) for i in range(3)]; R_xn = [S.res() for _ in range(3)]
                    junk = sbt(me, "m1junk", [128, D], BF16); R_junk = S.res()
                    ss_ = [sbt(me, "m1ss%d" % i, [128, 1]) for i in range(3)]; lnv_ = [sbt(me, "m1lnv%d" % i, [128, 1]) for i in range(3)]
                    rstd_ = [sbt(me, "m1rstd%d" % i, [128, 1]) for i in range(3)]; R_ss_ = [S.res() for _ in range(3)]
                    hTf_ = [sbt(me, "m1hTf%d" % i, [128, 8, 128]) for i in range(3)]; R_hTf_ = [S.res() for _ in range(3)]
                    htmp_ = [sbt(me, "m1htmp%d" % i, [128, D]) for i in range(3)]; R_htmp_ = [S.res() for _ in range(3)]
                    gsrow = sbt(me, "gsrow", [128, D]); shrow = sbt(me, "shrow", [128, D]); R_rows = S.res()
                    dg = sbt(me, "dg", [128, 8, 128]); R_dg = S.res()
                    onesf = sbt(me, "onesf", [128, 128]); usf = sbt(me, "usf", [128, 128]); R_cf = S.res()
                    lg_ = [sbt(me, "m1lg%d" % i, [128, 8]) for i in range(3)]; top8_ = [sbt(me, "m1top8%d" % i, [128, 8]) for i in range(3)]; R_lg_ = [S.res() for _ in range(3)]
                    gt_ = [sbt(me, "m1gt%d" % i, [128, 4]) for i in range(3)]; R_gt_ = [S.res() for _ in range(3)]
                    sel1 = sbt(me, "sel1", [128, NBL, 8]); sel2 = sbt(me, "sel2", [128, NBL, 8]); R_sel = S.res()
                    selS = sbt(me, "selS", [128, NBL, 8]); R_selS = S.res()
                    wit = sbt(me, "wit", [128, NBL, 8]); R_wit = S.res()
                    ca = sbt(me, "ca", [128, NBL, 8]); cb_ = sbt(me, "cb_", [128, NBL, 8]); cnt0 = sbt(me, "cnt0", [128, NBL, 8]); R_cn = S.res()
                    sm = sbt(me, "m1sm", [128, 64]); R_smm = S.res()
                    smi = sbt(me, "m1smi", [128, 8], I32)
                    dstf = sbt(me, "dstf", [128, NBL, 8]); R_dstf = S.res()
                    d1f = sbt(me, "d1f", [128, NBL]); d2f = sbt(me, "d2f", [128, NBL]); R_df = S.res()
                    cmp = sbt(me, "cmp", [128, NSL, 8]); esl = sbt(me, "esl", [128, NSL]); R_es = S.res()
                    idxf = sbt(me, "idxf", [128, NSL, NF]); R_idxf = S.res()
                    zt = sbt(me, "zt", [128, D], BF16); R_zt = S.res()
                    S.op("dve", CP(onesf[:], ones_b), r=[R_c], w=[R_cf])
                    S.op("dve", TT(usf[:], U_b, ident_b, ALU.subtract), r=[R_c], w=[R_cf])
                    S.op("pool", MEMSET(zt[:], 0.0), w=[R_zt])
                    for rb in range(NROWS // 128):
                        S.dma("sp", "hsz", hs_d[rb * 128:(rb + 1) * 128, :], zt[:], r=[R_zt], w=[R_hs])
                    for (dst_, col_t, col0) in ((gsrow, gsf[l], 0), (shrow, modc[l], 24)):
                        S.op("dve", TT(dg[:], ident_f.unsqueeze(1).broadcast_to([128, 8, 128]),
                                       col_t[:, col0:col0 + 8].unsqueeze(2).broadcast_to([128, 8, 128]), ALU.mult), r=[R_c, R_mod], w=[R_dg])
                        d2_ = dg[:].rearrange("p c n -> p (c n)")
                        for hf in range(2):
                            pt, pr = PS()
                            S.op("pe", MMS([(pt[:, :], onesf[:], d2_[:, hf * 512:(hf + 1) * 512], True, True)]), r=[R_dg, R_cf], w=[pr])
                            S.op("act", ACTF(dst_[:, hf * 512:(hf + 1) * 512], pt[:, :], AF.Copy), w=[pr, R_rows])
                    def m1blk(b):
                        k = b % 3
                        ss, lnv, rstd, R_ss = ss_[k], lnv_[k], rstd_[k], R_ss_[k]
                        hTf, R_hTf, htmp, R_htmp = hTf_[k], R_hTf_[k], htmp_[k], R_htmp_[k]
                        lg, top8, R_lg, gt, R_gt = lg_[k], top8_[k], R_lg_[k], gt_[k], R_gt_[k]
                        load_x(b, xt1, R_xt1, xB, R_xB, k)
                        yield None
                        yield S.op("act", ACTF(junk[:], xt1[k][:], AF.Square, accum_out=ss[:]), r=[R_xt1[k]], w=[R_junk, R_ss])
                        yield S.op("act", ACTF(lnv[:], ss[:], AF.Ln, bias=epsD[:], scale=1.0 / D), r=[R_ss, R_c2], w=[R_ss])
                        yield S.op("act", ACTF(rstd[:], lnv[:], AF.Exp, scale=-0.5), r=[R_ss], w=[R_ss])
                        yield S.op("act", ACTF(xn[k][:], xt1[k][:], AF.Identity, scale=rstd[:]), r=[R_xt1[k], R_ss], w=[R_xn[k]])
                        pa, ra = yield from PSG()
                        pb, rb_ = yield from PSG()
                        yield S.op("pe", TRS([(pa[:, j * 128:(j + 1) * 128], xn[k][:, j * 128:(j + 1) * 128], ident_f) for j in range(4)]), r=[R_xn[k], R_c], w=[ra])
                        yield S.op("pe", TRS([(pb[:, j * 128:(j + 1) * 128], xn[k][:, (4 + j) * 128:(5 + j) * 128], ident_f) for j in range(4)]), r=[R_xn[k], R_c], w=[rb_])
                        yield S.op("act", ACTS([(hTf[:, j, :], pa[:, j * 128:(j + 1) * 128], AF.Identity,
                                           dict(scale=gsf[l][:, j:j + 1], bias=modc[l][:, 24 + j:25 + j])) for j in range(4)]), r=[R_mod], w=[ra, R_hTf])
                        yield S.op("act", ACTS([(hTf[:, 4 + j, :], pb[:, j * 128:(j + 1) * 128], AF.Identity,
                                           dict(scale=gsf[l][:, 4 + j:5 + j], bias=modc[l][:, 28 + j:29 + j])) for j in range(4)]), r=[R_mod], w=[rb_, R_hTf])
                        yield S.op("dve", TT(htmp[:], xn[k][:], gsrow[:], ALU.mult), r=[R_xn[k], R_rows], w=[R_htmp])
                        yield S.op("dve", TT(H[:, b, :], htmp[:], shrow[:], ALU.add), r=[R_htmp, R_rows], w=[R_H])
                        pr_, rr_ = yield from PSG()
                        yield S.op("pe", MMS([(pr_[:, 0:8], hTf[:, dch, :], P[:, P_RW + dch * 8:P_RW + dch * 8 + 8], dch == 0, dch == 7) for dch in range(8)]),
                             r=[R_hTf, R_c], w=[rr_])
                        yield S.op("dve", CP(lg[:], pr_[:, 0:8]), w=[rr_, R_lg])
                        yield S.op("dve", lambda e: e.max(out=top8[:], in_=lg[:]), r=[R_lg], w=[R_lg])
                        yield S.op("dve", TT(gt[:, 0:1], top8[:, 1:2], top8[:, 0:1], ALU.subtract), r=[R_lg], w=[R_gt])
                        yield S.op("act", ACTF(gt[:, 1:2], gt[:, 0:1], AF.Exp), r=[R_gt], w=[R_gt])
                        yield S.op("dve", TS(gt[:, 1:2], gt[:, 1:2], 1.0, None, ALU.add), r=[R_gt], w=[R_gt])
                        yield S.op("dve", lambda e, b=b: e.reciprocal(out=g12[:, b, 0:1], in_=gt[:, 1:2]), r=[R_gt], w=[R_g12])
                        yield S.op("dve", TS(g12[:, b, 1:2], g12[:, b, 0:1], -1.0, 1.0, ALU.mult, ALU.add), r=[R_g12], w=[R_g12])
                        yield S.op("dve", TS(sel1[:, b, :], lg[:], top8[:, 0:1], None, ALU.is_equal), r=[R_lg], w=[R_sel])
                        yield S.op("dve", TS(sel2[:, b, :], lg[:], top8[:, 1:2], None, ALU.is_equal), r=[R_lg], w=[R_sel])
                    act_g = []
                    nxt = 0
                    while nxt < NBL or act_g:
                        while len(act_g) < 3 and nxt < NBL:
                            act_g.append(m1blk(nxt))
                            nxt += 1
                        for g_ in list(act_g):
                            try:
                                next(g_)
                            except StopIteration:
                                act_g.remove(g_)
                    fl = lambda t_: t_[:].rearrange("p b e -> p (b e)")
                    S.op("dve", TT(selS[:], sel1[:], sel2[:], ALU.add), r=[R_sel], w=[R_selS])
                    NCOL = NBL * 8
                    for c0 in range(0, NCOL, 512):
                        c1 = min(NCOL, c0 + 512)
                        pw, rw = PS()
                        S.op("pe", MMS([(pw[:, 0:c1 - c0], usf[:], fl(selS)[:, c0:c1], True, True)]), r=[R_selS, R_cf], w=[rw])
                        S.op("act", ACTF(fl(wit)[:, c0:c1], pw[:, 0:c1 - c0], AF.Copy), w=[rw, R_wit])
                        pc_, rc_ = PS()
                        S.op("pe", MMS([(pc_[:, 0:c1 - c0], onesf[:], fl(selS)[:, c0:c1], True, True)]), r=[R_selS, R_cf], w=[rc_])
                        S.op("act", ACTF(fl(cnt0)[:, c0:c1], pc_[:, 0:c1 - c0], AF.Copy), w=[rc_, R_cn])
                    S.op("dve", CP(ca[:], cnt0[:]), r=[R_cn], w=[R_cn])
                    cur, oth = ca, cb_
                    sh = 1
                    while sh < NBL:
                        S.op("dve", CP(oth[:, 0:sh, :], cur[:, 0:sh, :]), r=[R_cn], w=[R_cn])
                        S.op("dve", TT(oth[:, sh:NBL, :], cur[:, sh:NBL, :], cur[:, 0:NBL - sh, :], ALU.add), r=[R_cn], w=[R_cn])
                        cur, oth = oth, cur
                        sh *= 2
                    incl = cur
                    S.op("dve", TT(oth[:], incl[:], cnt0[:], ALU.subtract), r=[R_cn], w=[R_cn])
                    base = oth
                    tot = incl[:, NBL - 1, :]
                    S.op("dve", TS(sm[:, 0:8], tot, float(SLOTR - 1), 1.0 / SLOTR, ALU.add, ALU.mult), r=[R_cn], w=[R_smm])
                    S.op("dve", TS(sm[:, 0:8], sm[:, 0:8], -0.5 + 0.5 / SLOTR, None, ALU.add), r=[R_smm], w=[R_smm])
                    S.op("dve", CP(smi[:], sm[:, 0:8]), r=[R_smm], w=[R_smm])
                    S.op("dve", CP(sm[:, 0:8], smi[:]), r=[R_smm], w=[R_smm])
                    S.op("dve", TS(sm[:, 0:8], sm[:, 0:8], float(SLOTR), None, ALU.mult), r=[R_smm], w=[R_smm])
                    S.op("dve", CP(sm[:, 8:16], sm[:, 0:8]), r=[R_smm], w=[R_smm])
                    a0, b0 = 8, 16
                    for sh in (1, 2, 4):
                        S.op("dve", CP(sm[:, b0:b0 + sh], sm[:, a0:a0 + sh]), r=[R_smm], w=[R_smm])
                        S.op("dve", TT(sm[:, b0 + sh:b0 + 8], sm[:, a0 + sh:a0 + 8], sm[:, a0:a0 + 8 - sh], ALU.add), r=[R_smm], w=[R_smm])
                        a0, b0 = b0, a0
                    pend = sm[:, a0:a0 + 8]
                    S.op("dve", TT(sm[:, 24:32], pend, sm[:, 0:8], ALU.subtract), r=[R_smm], w=[R_smm])
                    pstart = sm[:, 24:32]
                    S.op("dve", TT(dstf[:], base[:], wit[:], ALU.add), r=[R_cn, R_wit], w=[R_dstf])
                    S.op("dve", TT(dstf[:], dstf[:], pstart.unsqueeze(1).broadcast_to([128, NBL, 8]), ALU.add), r=[R_smm, R_dstf], w=[R_dstf])
                    for (sel_, df_, di_) in ((sel1, d1f, d1i), (sel2, d2f, d2i)):
                        S.op("dve", TT(selS[:], sel_[:], dstf[:], ALU.mult), r=[R_sel, R_dstf, R_selS], w=[R_selS])
                        S.op("dve", lambda e, df_=df_: e.tensor_reduce(out=df_[:], in_=selS[:], axis=mybir.AxisListType.X, op=ALU.add), r=[R_selS], w=[R_df])
                        S.op("dve", CP(di_[:], df_[:]), r=[R_df], w=[R_di])
                    S.op("dve", TT(cmp[:], pend.unsqueeze(1).broadcast_to([128, NSL, 8]),
                                   cst2[:, 0:NSL].unsqueeze(2).broadcast_to([128, NSL, 8]), ALU.is_le), r=[R_smm, R_cst2], w=[R_es])
                    S.op("dve", lambda e: e.tensor_reduce(out=esl[:], in_=cmp[:], axis=mybir.AxisListType.X, op=ALU.add), r=[R_es], w=[R_es])
                    S.op("dve", TS(esl[:], esl[:], 7.0, 128.0, ALU.min, ALU.mult), r=[R_es], w=[R_es])
                    S.op("dve", TS(esl[:], esl[:], cst2[:, NSL:NSL + 1], None, ALU.add), r=[R_es, R_cst2], w=[R_es])
                    S.op("dve", TT(idxf[:], esl[:].unsqueeze(2).broadcast_to([128, NSL, NF]),
                                   cst2[:, NSL + 1:NSL + 1 + NF].unsqueeze(1).broadcast_to([128, NSL, NF]), ALU.add), r=[R_es, R_cst2], w=[R_idxf])
                    S.op("dve", CP(idxw[:], idxf[:]), r=[R_idxf], w=[R_idxw])
                    if DBG_M1:
                        dbg = sbt(me, "dbg", [128, D]); R_dbg = S.res()
                        S.op("pool", MEMSET(dbg[:], 0.0), w=[R_dbg])
                        S.op("dve", CP(dbg[:, 0:NBL], d1f[:]), r=[R_df, R_dbg], w=[R_dbg])
                        S.op("dve", CP(dbg[:, 64:64 + NBL], d2f[:]), r=[R_df, R_dbg], w=[R_dbg])
                        S.op("dve", CP(dbg[:, 128:128 + NSL], esl[:]), r=[R_es, R_dbg], w=[R_dbg])
                        S.op("dve", CP(dbg[:, 192:256], sm[:]), r=[R_smm, R_dbg], w=[R_dbg])
                        S.op("dve", CP(dbg[:, 256:256 + NBL * 8], dstf[:].rearrange("p b e -> p (b e)")), r=[R_dstf, R_dbg], w=[R_dbg])
                        S.op("dve", CP(dbg[:, 512:512 + NBL * 8], sel1[:].rearrange("p b e -> p (b e)")), r=[R_sel, R_dbg], w=[R_dbg])
                        S.op("dve", CP(dbg[:, 768:768 + NBL * 8], sel2[:].rearrange("p b e -> p (b e)")), r=[R_sel, R_dbg], w=[R_dbg])
                        S.dma("sp", "dbg", xdst[0:128, :], dbg[:], r=[R_dbg], w=[R_dst[0]])
                        S.dma("sp", "dbg", xdst[128:256, 0:NSL * NF], idxf[:].rearrange("p s f -> p (s f)"), r=[R_idxf], w=[R_dst[0]])
                        return
                    for b in range(NBL):
                        S.idma("sc1", hs_d, IOA(ap=d1i[:, b:b + 1], axis=0), H[:, b, :], None, r=[R_H, R_di], w=[R_hs])
                        S.idma("sc1", hs_d, IOA(ap=d2i[:, b:b + 1], axis=0), H[:, b, :], None, r=[R_H, R_di], w=[R_hs])
                S.barrier()

                with ExitStack() as me:
                    hTt = sbt(me, "shTt", [128, 8, SLOTR], BF16); R_hTt = S.res()
                    hrow = [sbt(me, "hrow%d" % i, [128, D], BF16) for i in range(2)]; R_hrow = [S.res() for _ in range(2)]
                    acc = sbt(me, "sacc", [128, SLOTR // 128, D]); R_acc = S.res()
                    actT = [sbt(me, "sactT%d" % i, [128, GF, SLOTR], BF16) for i in range(2)]; R_actT = [S.res() for _ in range(2)]
                    sg = [sbt(me, "ssg%d" % i, [128, 512], BF16) for i in range(2)]; R_sg = [S.res() for _ in range(2)]
                    NSLOT = 2 * GF
                    gstg = [sbt(me, "sgstg%d" % i, [128, 8, 128]) for i in range(3)]; R_gstg = [S.res() for _ in range(3)]
                    ustg = [sbt(me, "sustg%d" % i, [128, 8, 128]) for i in range(3)]; R_ustg = [S.res() for _ in range(3)]
                    dstg = [sbt(me, "sdstg%d" % i, [128, D]) for i in range(3)]; R_dstg = [S.res() for _ in range(3)]
                    wgb = [sbt(me, "swgb%d" % i, [128, 8, 128], BF16) for i in range(NSLOT)]; R_wgb = [S.res() for _ in range(NSLOT)]
                    wub = [sbt(me, "swub%d" % i, [128, 8, 128], BF16) for i in range(NSLOT)]; R_wub = [S.res() for _ in range(NSLOT)]
                    wdb = [sbt(me, "swdb%d" % i, [128, D], BF16) for i in range(NSLOT)]; R_wdb = [S.res() for _ in range(NSLOT)]
                    slot = [0]; stgc = [0]; castc = [0]
                    NRB = SLOTR // 128
                    for s_i in range(NSL):
                        for rb in range(NRB):
                            k = rb % 2
                            r0 = s_i * SLOTR + rb * 128
                            S.dma("sp", "hrow%d" % k, hrow[k][:], hs_d[r0:r0 + 128, :], r=[R_hs], w=[R_hrow[k]])
                            pt, pr = PS()
                            ptb = pt[:, :].bitcast(BF16)
                            S.op("pe", TRS([(ptb[:, j * 128:(j + 1) * 128], hrow[k][:, j * 128:(j + 1) * 128], ident_b) for j in range(8)]),
                                 r=[R_hrow[k], R_c], w=[pr])
                            S.op("act", ACTF(hTt[:, :, rb * 128:(rb + 1) * 128], ptb[:, :].rearrange("p (c t) -> p c t", c=8), AF.Copy), w=[pr, R_hTt])
                        for fg in range(NF // GF):
                            ab = fg % 2
                            slots = []
                            for fi in range(GF):
                                fc = fg * GF + fi
                                sl_ = slot[0] % NSLOT
                                slot[0] += 1
                                slots.append(sl_)
                                k = stgc[0] % 3
                                stgc[0] += 1
                                io = IOA(ap=idxw[:, s_i, fc:fc + 1], axis=0)
                                S.idma("sgs%d" % k, gstg[k][:].rearrange("p c n -> p (c n)"), None, mgl_in, io, r=[R_idxw], w=[R_gstg[k]])
                                S.idma("sus%d" % k, ustg[k][:].rearrange("p c n -> p (c n)"), None, mul_in, io, r=[R_idxw], w=[R_ustg[k]])
                                S.idma("sds%d" % k, dstg[k][:], None, mdl_in, io, r=[R_idxw], w=[R_dstg[k]])
                                for (src_, R_s, dst_, R_d) in ((gstg[k], R_gstg[k], wgb[sl_], R_wgb[sl_]), (ustg[k], R_ustg[k], wub[sl_], R_wub[sl_])):
                                    ce = ("act", "dve")[castc[0] % 2]
                                    castc[0] += 1
                                    if ce == "act":
                                        S.op("act", ACTF(dst_[:], src_[:], AF.Copy), r=[R_s], w=[R_d])
                                    else:
                                        S.op("dve", CP(dst_[:], src_[:]), r=[R_s], w=[R_d])
                                S.op("dve", TT(wdb[sl_][:], dstg[k][:], gfrow[l][:], ALU.mult), r=[R_dstg[k], R_mod], w=[R_wdb[sl_]])
                                for sub in range(SLOTR // 512):
                                    tcs = slice(sub * 512, (sub + 1) * 512)
                                    pg, rg_ = PS()
                                    S.op("pe", MMS([(pg[:, :], wgb[sl_][:, dch, :], hTt[:, dch, tcs], dch == 0, dch == 7) for dch in range(8)]),
                                         r=[R_wgb[sl_], R_hTt], w=[rg_])
                                    pu, ru = PS()
                                    S.op("pe", MMS([(pu[:, :], wub[sl_][:, dch, :], hTt[:, dch, tcs], dch == 0, dch == 7) for dch in range(8)]),
                                         r=[R_wub[sl_], R_hTt], w=[ru])
                                    kk = sub % 2
                                    S.op("act", ACTF(sg[kk][:], pg[:, :], AF.Silu), w=[rg_, R_sg[kk]])
                                    S.op("dve", TT(actT[ab][:, fi, tcs], sg[kk][:], pu[:, :], ALU.mult), r=[R_sg[kk]], w=[ru, R_actT[ab]])
                            for rb in range(NRB):
                                for hf in range(2):
                                    pd, rd = PS()
                                    S.op("pe", MMS([(pd[:, :], actT[ab][:, fi, rb * 128:(rb + 1) * 128], wdb[slots[fi]][:, hf * 512:(hf + 1) * 512],
                                                     fi == 0, fi == GF - 1) for fi in range(GF)]),
                                         r=[R_actT[ab]] + [R_wdb[x_] for x_ in slots], w=[rd])
                                    xs_ = acc[:, rb, hf * 512:(hf + 1) * 512]
                                    if fg == 0:
                                        S.op("act", ACTF(xs_, pd[:, :], AF.Copy), w=[rd, R_acc])
                                    else:
                                        S.op("dve", TT(xs_, pd[:, :], xs_, ALU.add), w=[rd, R_acc])
                        for rb in range(NRB):
                            r0 = s_i * SLOTR + rb * 128
                            S.dma("sp", "yst", ys_d[r0:r0 + 128, :], acc[:, rb, :], r=[R_acc], w=[R_ys])
                S.barrier()

                with ExitStack() as me:
                    xt1 = [sbt(me, "m3x%d" % i, [128, D]) for i in range(2)]; R_xt1 = [S.res() for _ in range(2)]
                    xB = [sbt(me, "m3xB%d" % i, [128, D]) for i in range(2)]; R_xB = [S.res() for _ in range(2)]
                    y1t = [sbt(me, "m3y1%d" % i, [128, D]) for i in range(2)]; R_y1t = [S.res() for _ in range(2)]
                    y2t = [sbt(me, "m3y2%d" % i, [128, D]) for i in range(2)]; R_y2t = [S.res() for _ in range(2)]
                    ob = [sbt(me, "m3ob%d" % i, [128, D]) for i in range(2)]; R_ob = [S.res() for _ in range(2)]
                    junk = sbt(me, "m3junk", [128, D], BF16); R_junk = S.res()
                    ss = sbt(me, "m3ss", [128, 1]); lnv = sbt(me, "m3lnv", [128, 1]); rstd = sbt(me, "m3rstd", [128, 1]); R_ss = S.res()
                    fng = sbt(me, "m3fng", [128, D]); R_fng = S.res()
                    S.dma("sp", "fng", fng[:], fng_in, w=[R_fng])
                    for b in range(NBL):
                        k = b % 2
                        load_x(b, xt1, R_xt1, xB, R_xB, k)
                        S.idma("g1_%d" % k, y1t[k][:], None, ys_d, IOA(ap=d1i[:, b:b + 1], axis=0), r=[R_ys, R_di], w=[R_y1t[k]])
                        S.idma("g2_%d" % k, y2t[k][:], None, ys_d, IOA(ap=d2i[:, b:b + 1], axis=0), r=[R_ys, R_di], w=[R_y2t[k]])
                        S.op("dve", STT(xt1[k][:], y1t[k][:], g12[:, b, 0:1], xt1[k][:], ALU.mult, ALU.add), r=[R_y1t[k], R_g12, R_xt1[k]], w=[R_xt1[k]])
                        S.op("dve", STT(xt1[k][:], y2t[k][:], g12[:, b, 1:2], xt1[k][:], ALU.mult, ALU.add), r=[R_y2t[k], R_g12, R_xt1[k]], w=[R_xt1[k]])
                        S.op("act", ACTF(junk[:], xt1[k][:], AF.Square, accum_out=ss[:]), r=[R_xt1[k]], w=[R_junk, R_ss])
                        S.op("act", ACTF(lnv[:], ss[:], AF.Ln, bias=epsD[:], scale=1.0 / D), r=[R_ss, R_c2], w=[R_ss])
                        S.op("act", ACTF(rstd[:], lnv[:], AF.Exp, scale=-0.5), r=[R_ss], w=[R_ss])
                        S.op("dve", STT(ob[k][:], xt1[k][:], rstd[:], fng[:], ALU.mult, ALU.mult), r=[R_xt1[k], R_ss, R_fng], w=[R_ob[k]])
                        S.dma("sp", "m3st%d" % k, xdst[b * 128:(b + 1) * 128, :], ob[k][:], r=[R_ob[k]], w=[R_dst[k]])

        R_x = [S.res("x_in")]; R_xa = [S.res("xa0"), S.res("xa1")]; R_xb = [S.res("xb0"), S.res("xb1")]
        R_out = [S.res("out0"), S.res("out1")]
        stages = [
            ("mix0", lambda dst, Rd: mixer(0, x_in, R_x, dst, Rd)),
            ("ffn0", lambda dst, Rd: chanmix(0, xa_d, R_xa, dst, Rd, False, False, L, False)),
            ("mix1", lambda dst, Rd: mixer(1, xb_d, R_xb, dst, Rd)),
            ("moe1", (lambda dst, Rd: moe_sparse(1, xa_d, R_xa, dst, Rd, LM, split)) if sparse else
                     (lambda dst, Rd: chanmix(1, xa_d, R_xa, dst, Rd, True, True, LM, split))),
        ]
        dsts = [(xa_d, R_xa), (xb_d, R_xb), (xa_d, R_xa), (out_d, R_out)]
        for (name, fn), (dst, Rd) in zip(stages, dsts):
            if stop_after == name:
                fn(out_d, R_out)
                break
            fn(dst, Rd)
            S.barrier()
        S.wait_all("sp", R_out)
        with nc.Block() as block:
            S.emit(block)
    return nc


def _consts():
    cstf = np.zeros((128, 130), np.float32)
    cstf[:, :128] = np.eye(128, dtype=np.float32)
    p = np.arange(128)
    cstf[:, 128] = (10000.0 ** (-((p % 32) * 2).astype(np.float64) / 64.0)).astype(np.float32)
    cstf[:, 129] = np.where((p % 64) < 32, -1.0, 1.0)
    cb = np.zeros((128, B_TOT), np.float32)
    cb[:, B_ID:B_ID + 128] = np.eye(128)
    k = np.arange(128)[:, None]
    q = np.arange(128)[None, :]
    cb[:, B_U:B_U + 128] = (k <= q)
    cb[:, B_ONES:B_ONES + 128] = 1.0
    cb[:, B_MCUR:B_MCUR + 512] = np.tile(np.where(k <= q, 0.0, NEG), (1, 4))
    cb[:, B_MPREV:B_MPREV + 512] = np.tile(np.where(k > q, 0.0, NEG), (1, 4))
    return cstf, cb.astype(ml_dtypes.bfloat16)


def _perm_win(w):
    q = w[:, 0:512].reshape(D, 8, 64)
    k = w[:, 512:640].reshape(D, 2, 64)
    v = w[:, 640:768]
    z = w[:, 768:1280]
    xbc = w[:, 1280:2304]
    dt = w[:, 2304:2312]
    sw = lambda a: np.concatenate([a[..., 32:], a[..., :32]], axis=-1)
    qp = np.stack([np.concatenate([q[:, j], q[:, 4 + j]], axis=-1) for j in range(4)], axis=1).reshape(D, 512)
    qs = sw(q)
    qsp = np.stack([np.concatenate([qs[:, j], qs[:, 4 + j]], axis=-1) for j in range(4)], axis=1).reshape(D, 512)
    kp = k.reshape(D, 128)
    ks = sw(k).reshape(D, 128)
    return np.ascontiguousarray(np.concatenate([qp, qsp, kp, ks, xbc, v, z, dt], axis=1))


def _lp(l, norm_mix_g, norm_ffn_g, conv_w, conv_b, attn_sinks, dt_bias, a_log, d_skip, ssm_norm_g, router_w):
    lp = np.zeros((128, P_TOT), np.float32)
    lp[:, P_GM:P_GM + 8] = norm_mix_g[l].reshape(8, 128).T
    lp[:, P_GF:P_GF + 8] = norm_ffn_g[l].reshape(8, 128).T
    lp[:, P_CW:P_CW + 32] = conv_w[l].reshape(4, 8, 128).transpose(2, 1, 0).reshape(128, 32)
    lp[:, P_CB:P_CB + 8] = conv_b[l].reshape(8, 128).T
    lp[:, P_SK:P_SK + 8] = attn_sinks[l][None, :]
    lp[:, P_DTB:P_DTB + 8] = dt_bias[l][None, :]
    lp[:, P_ALOG:P_ALOG + 8] = a_log[l][None, :]
    lp[:, P_DSK:P_DSK + 8] = d_skip[l][None, :]
    lp[:, P_SSMG:P_SSMG + 512] = ssm_norm_g[l][None, :]
    if l == 1:
        lp[:, P_RW:P_RW + 64] = router_w[0].reshape(8, 128, 8).transpose(1, 0, 2).reshape(128, 64)
    return lp


def make_in_maps(inp, L, ncores, split=True, sparse=True):
    f = lambda a: np.ascontiguousarray(np.asarray(a, dtype=np.float32))
    cstf, cstb = _consts()
    shared = {
        "cstf": cstf, "cstb": cstb,
        "fng": np.ascontiguousarray(np.broadcast_to(f(inp["final_norm_g"])[None, :], (128, D))),
        "ffg": f(inp["ffn_w_gate"][0]), "ffu": f(inp["ffn_w_up"][0]), "ffd": f(inp["ffn_w_down"][0]),
    }
    if not sparse:
        shared.update({"mog": f(inp["moe_w_gate"][0]), "mou": f(inp["moe_w_up"][0]), "mod": f(inp["moe_w_down"][0])})
    for l in range(2):
        shared["lp%d" % l] = _lp(l, f(inp["norm_mix_g"]), f(inp["norm_ffn_g"]), f(inp["conv_w"]), f(inp["conv_b"]),
                                 f(inp["attn_sinks"]), f(inp["dt_bias"]), f(inp["a_log"]), f(inp["d_skip"]),
                                 f(inp["ssm_norm_g"]), f(inp["router_w"]))
        shared["adaw%d" % l] = f(inp["ada_w"][l])
        shared["adab%d" % l] = f(inp["ada_b"][l])[None, :]
        shared["win%d" % l] = _perm_win(f(inp["w_in"][l]))
        shared["wout%d" % l] = f(inp["w_out"][l])
    if sparse:
        NFM = EXD // 128
        LM = L // 2 if split else L
        NSL = (2 * LM) // SLOTR + NEXP
        wg = f(inp["moe_w_gate"][0]); wu = f(inp["moe_w_up"][0]); wd = f(inp["moe_w_down"][0])
        lay = lambda w: np.ascontiguousarray(w.reshape(8, 8, 128, NFM, 128).transpose(3, 0, 2, 1, 4).reshape(NFM * 1024, 1024))
        shared["mogl"] = lay(wg)
        shared["moul"] = lay(wu)
        shared["modl"] = np.ascontiguousarray(wd.reshape(8, NFM, 128, 1024).transpose(1, 0, 2, 3).reshape(NFM * 1024, 1024))
        c2 = np.zeros((128, NSL + 1 + NFM), np.float32)
        c2[:, :NSL] = (np.arange(NSL) * SLOTR)[None, :]
        c2[:, NSL] = np.arange(128)
        c2[:, NSL + 1:] = (np.arange(NFM) * 1024)[None, :]
        shared["cst2"] = c2
    x = np.asarray(inp["x"], dtype=np.float32)
    c = np.asarray(inp["c"], dtype=np.float32)
    pos = np.asarray(inp["positions"], dtype=np.int32)
    maps = []
    for r in range(ncores):
        b, half = (r // 2, r % 2) if split else (r, 0)
        m = dict(shared)
        m["x"] = np.ascontiguousarray(x[b, :L])
        m["pos"] = np.ascontiguousarray(pos[b:b + 1, :L])
        m["cT"] = np.ascontiguousarray(c[b].reshape(8, 128).T)
        sel = np.zeros((128, 2), np.float32)
        sel[:, half] = 1.0
        m["sel"] = sel
        maps.append(m)
    return maps


_NC_CACHE = {}


def kernel(**inputs):
    x = np.asarray(inputs["x"])
    B, L, _ = x.shape
    if L not in _NC_CACHE:
        _NC_CACHE[L] = build(L)
    nc = _NC_CACHE[L]
    ncores = 2 * B
    maps = make_in_maps(inputs, L, ncores)
    res = run_bass_kernel_spmd(nc, maps, core_ids=list(range(ncores)))
    out = np.empty((B, L, D), np.float32)
    h = L // 2
    for r in range(ncores):
        out[r // 2, (r % 2) * h:(r % 2 + 1) * h] = np.asarray(res.results[r]["out"])
    return out
```

```python
import math
from contextlib import ExitStack

import numpy as np
import ml_dtypes

import concourse.bass as bass
import concourse.mybir as mybir
from concourse.bass_utils import run_bass_kernel_spmd

F32 = mybir.dt.float32
BF16 = mybir.dt.bfloat16
I32 = mybir.dt.int32
AF = mybir.ActivationFunctionType
ALU = mybir.AluOpType
PI = math.pi

D = 1024
NQH = 8
INW = 2952
C_Q, C_QS, C_K, C_KS, C_X, C_V, C_Z, C_DT = 0, 512, 1024, 1152, 1280, 2304, 2432, 2944
FFN = 2816
EXD = 3584
NEXP = 8
EPS = 1e-6
NEG = -30000.0
P_GM, P_GF, P_CW, P_CB, P_SK, P_DTB, P_ALOG, P_DSK, P_SSMG, P_RW, P_TOT = 0, 8, 16, 48, 56, 64, 72, 80, 88, 600, 664
B_ID, B_U, B_ONES, B_MCUR, B_MPREV, B_TOT = 0, 128, 256, 384, 896, 1408


class Res:
    __slots__ = ("name", "w", "r", "hold")

    def __init__(self, name):
        self.name = name
        self.w = None
        self.r = {}
        self.hold = None


class Sched:
    ENG = ("pe", "act", "dve", "pool", "sp")

    def __init__(self, nc, es):
        self.nc = nc
        self.es = es
        self.prog = {e: [] for e in self.ENG}
        self.sem = {}
        self.cnt = {}
        for e in ("pe", "act", "dve", "pool"):
            self.sem[e] = es.enter_context(nc.semaphore("c_" + e))
            self.cnt[e] = 0
        self.waited = {e: {} for e in self.ENG}
        self.nres = 0

    def res(self, name=None):
        self.nres += 1
        return Res(name or ("r%d" % self.nres))

    def _waits(self, eng, reads, writes):
        deps = []
        for r in reads:
            if r.w is not None:
                deps.append((r.w, True))
        for w in writes:
            if w.w is not None:
                deps.append((w.w, False))
            for k, (v, e) in w.r.items():
                deps.append(((k, v, e), False))
        waits = {}
        for (k, v, e), raw in deps:
            if e == eng:
                if eng == "pe":
                    continue
                if not raw:
                    continue
                if v < self.cnt[eng] - SAME_ENG_DIST:
                    continue
            if self.waited[eng].get(k, 0) >= v:
                continue
            if waits.get(k, 0) < v:
                waits[k] = v
        for k, v in waits.items():
            self.waited[eng][k] = v
        return list(waits.items())

    def _commit(self, ticket, reads, writes):
        k, v, e = ticket
        for r in reads:
            old = r.r.get(k)
            if old is None or old[0] < v:
                r.r[k] = (v, e)
        for w in writes:
            w.w = ticket
            w.r = {}

    def op(self, eng, fn, r=(), w=()):
        if eng != "pe":
            for x in w:
                if x.hold is not None and x.hold[0] > 0:
                    x.hold[0] -= 1
        waits = self._waits(eng, r, w)
        self.cnt[eng] += 1
        self.prog[eng].append((waits, fn, (eng, 1)))
        self._commit((eng, self.cnt[eng], eng), r, w)

    def dma(self, q, sem, out, in_, r=(), w=()):
        if sem not in self.sem:
            self.sem[sem] = self.es.enter_context(self.nc.semaphore("d_" + sem))
            self.cnt[sem] = 0
        waits = self._waits(q, r, w)
        self.cnt[sem] += 16
        self.prog[q].append((waits, (lambda e: e.dma_start(out=out, in_=in_)), (sem, 16)))
        self._commit((sem, self.cnt[sem], "dma"), r, w)

    def idma(self, sem, out, out_off, in_, in_off, r=(), w=()):
        if sem not in self.sem:
            self.sem[sem] = self.es.enter_context(self.nc.semaphore("d_" + sem))
            self.cnt[sem] = 0
        waits = self._waits("pool", r, w)
        hist = self.__dict__.setdefault("idma_hist", [])
        if len(hist) >= IDMA_DEPTH:
            k0, v0 = hist[-IDMA_DEPTH]
            if self.waited["pool"].get(k0, 0) < v0:
                self.waited["pool"][k0] = v0
                waits = [(k, v) for (k, v) in waits if k != k0] + [(k0, max(v0, dict(waits).get(k0, 0)))]
        self.cnt[sem] += 16
        hist.append((sem, self.cnt[sem]))
        self.prog["pool"].append((waits, (lambda e: e.indirect_dma_start(out=out, out_offset=out_off, in_=in_, in_offset=in_off)), (sem, 16)))
        self._commit((sem, self.cnt[sem], "dma"), r, w)

    def barrier(self):
        for eng in self.ENG:
            waits = []
            for k, v in self.cnt.items():
                if v == 0 or k == eng:
                    continue
                if self.waited[eng].get(k, 0) >= v:
                    continue
                self.waited[eng][k] = v
                waits.append((k, v))
            self.prog[eng].append((waits, None, None))

    def wait_all(self, eng, reads):
        waits = self._waits(eng, reads, ())
        self.prog[eng].append((waits, None, None))

    def emit(self, block):
        S = self

        def run(name, e):
            for waits, fn, inc in S.prog[name]:
                for k, v in waits:
                    e.wait_ge(S.sem[k], v)
                if fn is None:
                    continue
                ins = fn(e)
                if inc is not None:
                    ins.then_inc(S.sem[inc[0]], inc[1])

        @block.sync
        def _(e):
            run("sp", e)

        @block.tensor
        def _(e):
            run("pe", e)

        @block.scalar
        def _(e):
            run("act", e)

        @block.vector
        def _(e):
            run("dve", e)

        @block.gpsimd
        def _(e):
            run("pool", e)


def TT(out, in0, in1, op):
    return lambda e: e.tensor_tensor(out=out, in0=in0, in1=in1, op=op)


def TS(out, in0, s1, s2=None, op0=ALU.mult, op1=None):
    if op1 is None:
        return lambda e: e.tensor_scalar(out=out, in0=in0, scalar1=s1, scalar2=None, op0=op0)
    return lambda e: e.tensor_scalar(out=out, in0=in0, scalar1=s1, scalar2=s2, op0=op0, op1=op1)


def STT(out, in0, scalar, in1, op0, op1):
    return lambda e: e.scalar_tensor_tensor(out=out, in0=in0, scalar=scalar, in1=in1, op0=op0, op1=op1)


def ACTF(out, in_, func, bias=None, scale=None, accum_out=None):
    kw = {}
    if bias is not None:
        kw["bias"] = bias
    if scale is not None:
        kw["scale"] = scale
    if accum_out is not None:
        kw["accum_out"] = accum_out
    return lambda e: e.activation(out=out, in_=in_, func=func, **kw)


def ACTS(lst):
    def f(e):
        ins = None
        for (out, in_, func, kw) in lst:
            ins = e.activation(out=out, in_=in_, func=func, **kw)
        return ins
    return f


def CP(out, in_):
    return lambda e: e.tensor_copy(out=out, in_=in_)


def MEMSET(ap, v):
    return lambda e: e.memset(ap, v)


def MMS(lst):
    def f(e):
        ins = None
        for (out, lhsT, rhs, st, sp) in lst:
            ins = e.matmul(out, lhsT=lhsT, rhs=rhs, start=st, stop=sp)
        return ins
    return f


def TRS(lst):
    def f(e):
        ins = None
        for (out, in_, ident) in lst:
            ins = e.transpose(out=out, in_=in_, identity=ident)
        return ins
    return f


SLOTR = 512
DBG_M1 = False
SAME_ENG_DIST = 1000000
ALLOC = {}
IDMA_DEPTH = 6


def build(L, stop_after=None, split=True, sparse=True):
    NB = L // 128
    LM = L // 2 if split else L
    assert (not sparse) or (2 * LM) % SLOTR == 0
    TTOK = min(1024, LM)
    NBT = TTOK // 128
    nc = bass.Bass("TRN2", target_bir_lowering=False)

    def din(name, shape, dt=F32):
        return nc.dram_tensor(name, list(shape), dt, kind="ExternalInput").ap()

    x_in = din("x", [L, D])
    pos_in = din("pos", [1, L], I32)
    cT_in = din("cT", [128, 8])
    sel_in = din("sel", [128, 2])
    cstf_in = din("cstf", [128, 130])
    cstb_in = din("cstb", [128, B_TOT], BF16)
    fng_in = din("fng", [128, D])
    lp_in = [din("lp%d" % l, [128, P_TOT]) for l in range(2)]
    adaw_in = [din("adaw%d" % l, [D, 6 * D]) for l in range(2)]
    adab_in = [din("adab%d" % l, [1, 6 * D]) for l in range(2)]
    win_in = [din("win%d" % l, [D, INW]) for l in range(2)]
    wout_in = [din("wout%d" % l, [D, D]) for l in range(2)]
    fg_in = din("ffg", [D, FFN])
    fu_in = din("ffu", [D, FFN])
    fd_in = din("ffd", [FFN, D])
    mg_in = mu_in = md_in = None
    if not sparse:
        mg_in = din("mog", [NEXP, D, EXD])
        mu_in = din("mou", [NEXP, D, EXD])
        md_in = din("mod", [NEXP, EXD, D])
    NFM = EXD // 128
    NSL = (2 * LM) // SLOTR + NEXP if sparse else 1
    NROWS = NSL * SLOTR
    if sparse:
        mgl_in = din("mogl", [NFM * 1024, 1024])
        mul_in = din("moul", [NFM * 1024, 1024])
        mdl_in = din("modl", [NFM * 1024, 1024])
        cst2_in = din("cst2", [128, NSL + 1 + NFM])
        hs_d = nc.dram_tensor("hs_d", [NROWS, D], BF16, kind="Internal").ap()
        ys_d = nc.dram_tensor("ys_d", [NROWS, D], F32, kind="Internal").ap()
    out_d = nc.dram_tensor("out", [LM, D], F32, kind="ExternalOutput").ap()
    xa_d = nc.dram_tensor("xa", [L, D], F32, kind="Internal").ap()
    xb_d = nc.dram_tensor("xb", [L, D], F32, kind="Internal").ap()
    cos_d = nc.dram_tensor("cosd", [128, L], F32, kind="Internal").ap()
    sin_d = nc.dram_tensor("sind", [128, L], F32, kind="Internal").ap()

    with ExitStack() as es:
        S = Sched(nc, es)

        uniq = [0]

        def sbt(es_, name, shape, dt=F32):
            uniq[0] += 1
            nb = int(np.prod(shape[1:])) * (2 if dt == BF16 else 4)
            ALLOC[id(es_)] = ALLOC.get(id(es_), 0) + ((nb + 31) // 32) * 32
            return es_.enter_context(nc.sbuf_tensor("s%d_%s" % (uniq[0], name), list(shape), dt))

        banks = []
        for i in range(8):
            t = es.enter_context(nc.psum_tensor("ps%d" % i, [128, 512], F32))
            banks.append((t, S.res("ps%d" % i)))
        pscur = [0]
        for (_t, _r) in banks:
            _r.hold = [0]

        def PS_try(n=1):
            for d in range(8):
                t, r = banks[(pscur[0] + d) % 8]
                if r.hold[0] == 0:
                    pscur[0] = pscur[0] + d + 1
                    r.hold[0] = n
                    return t, r
            return None

        def PS(n=1):
            got = PS_try(n)
            assert got is not None, "no free PSUM bank"
            return got

        def PSG(n=1):
            while True:
                got = PS_try(n)
                if got is not None:
                    return got
                yield None

        cstf = sbt(es, "cstf", [128, 130]); R_c = S.res("const")
        cstb = sbt(es, "cstb", [128, B_TOT], BF16)
        lp = [sbt(es, "lp%d" % l, [128, P_TOT]) for l in range(2)]
        cT = sbt(es, "cT", [128, 8])
        modc = [sbt(es, "modc%d" % l, [128, 48]) for l in range(2)]
        gsm = [sbt(es, "gsm%d" % l, [128, 8]) for l in range(2)]
        gsf = [sbt(es, "gsf%d" % l, [128, 8]) for l in range(2)]
        gmrow = [sbt(es, "gmrow%d" % l, [128, D]) for l in range(2)]
        gfrow = [sbt(es, "gfrow%d" % l, [128, D]) for l in range(2)]
        onesr = sbt(es, "onesr", [1, 128])
        one1 = sbt(es, "one1", [1, 1])
        epsD = sbt(es, "epsD", [128, 1])
        epsG = sbt(es, "epsG", [128, 1])
        R_mod = S.res("mod")

        ident_f = cstf[:, 0:128]
        invf = cstf[:, 128:129]
        sgn = cstf[:, 129:130]
        ident_b = cstb[:, B_ID:B_ID + 128]
        U_b = cstb[:, B_U:B_U + 128]
        ones_b = cstb[:, B_ONES:B_ONES + 128]
        mcur_b = cstb[:, B_MCUR:B_MCUR + 512]
        mprev_b = cstb[:, B_MPREV:B_MPREV + 512]

        S.dma("sp", "cst", cstf[:], cstf_in, w=[R_c])
        S.dma("sp", "cst", cstb[:], cstb_in, w=[R_c])
        S.dma("sp", "cst", cT[:], cT_in, w=[R_c])
        selt = sbt(es, "selt", [128, 2])
        S.dma("sp", "cst", selt[:], sel_in, w=[R_c])
        for l in range(2):
            S.dma("sp", "cst", lp[l][:], lp_in[l], w=[R_c])
        R_c2 = S.res("const2")
        S.op("pool", MEMSET(onesr[:], 1.0), w=[R_c2])
        S.op("pool", MEMSET(one1[:], 1.0), w=[R_c2])
        S.op("pool", MEMSET(epsD[:], EPS), w=[R_c2])
        S.op("pool", MEMSET(epsG[:], EPS), w=[R_c2])

        with ExitStack() as pe_:
            cact = sbt(pe_, "cact", [128, 8]); R_cact = S.res()
            ctmp = sbt(pe_, "ctmp", [128, 8])
            modrow = sbt(pe_, "modrow", [1, 6 * D]); R_mr = S.res()
            adab = sbt(pe_, "adab", [1, 6 * D]); R_ab = S.res()
            stg = [sbt(pe_, "adstg%d" % i, [128, 8, 512]) for i in range(2)]
            R_stg = [S.res() for _ in range(2)]
            S.op("act", ACTF(ctmp[:], cT[:], AF.Exp, scale=-1.0), r=[R_c], w=[R_cact])
            S.op("dve", TS(ctmp[:], ctmp[:], 1.0, None, ALU.add), r=[R_cact], w=[R_cact])
            S.op("dve", lambda e: e.reciprocal(out=ctmp[:], in_=ctmp[:]), r=[R_cact], w=[R_cact])
            S.op("dve", TT(cact[:], ctmp[:], cT[:], ALU.mult), r=[R_cact, R_c], w=[R_cact])
            for l in range(2):
                S.dma("sp", "adab", adab[:], adab_in[l], w=[R_ab])
                for cc in range(12):
                    k = cc % 2
                    S.dma("sp", "adstg%d" % k, stg[k][:],
                          adaw_in[l][:, cc * 512:(cc + 1) * 512].rearrange("(c p) n -> p c n", p=128), w=[R_stg[k]])
                    pt, pr = PS()
                    S.op("pe", MMS([(pt[0:1, :], cact[:, dch:dch + 1], stg[k][:, dch, :], dch == 0, dch == 7)
                                    for dch in range(8)]), r=[R_cact, R_stg[k]], w=[pr])
                    S.op("dve", TT(modrow[0:1, cc * 512:(cc + 1) * 512], pt[0:1, :], adab[0:1, cc * 512:(cc + 1) * 512], ALU.add),
                         r=[R_ab], w=[pr, R_mr])
                pt, pr = PS()
                S.op("pe", MMS([(pt[:, j:j + 1], modrow[0:1, j * 128:(j + 1) * 128], one1[0:1, 0:1], True, True)
                                for j in range(48)]), r=[R_mr, R_c2], w=[pr])
                S.op("dve", CP(modc[l][:], pt[:, 0:48]), w=[pr, R_mod])
                for (dst, off) in ((gmrow[l], 2 * D), (gfrow[l], 5 * D)):
                    for hf in range(2):
                        pt, pr = PS()
                        S.op("pe", MMS([(pt[:, :], onesr[0:1, :], modrow[0:1, off + hf * 512: off + (hf + 1) * 512], True, True)]),
                             r=[R_mr, R_c2], w=[pr])
                        S.op("act", ACTF(dst[:, hf * 512:(hf + 1) * 512], pt[:, :], AF.Copy), w=[pr, R_mod])
                S.op("dve", STT(gsm[l][:], modc[l][:, 8:16], 1.0, lp[l][:, P_GM:P_GM + 8], ALU.add, ALU.mult), r=[R_c, R_mod], w=[R_mod])
                S.op("dve", STT(gsf[l][:], modc[l][:, 32:40], 1.0, lp[l][:, P_GF:P_GF + 8], ALU.add, ALU.mult), r=[R_c, R_mod], w=[R_mod])

        S.barrier()
        R_tab = [S.res("ropetab0"), S.res("ropetab1")]
        with ExitStack() as pe_:
            CH = min(1024, L)
            posi = sbt(pe_, "posi", [128, CH], I32); R_pi = S.res()
            ang = sbt(pe_, "ang", [128, CH]); R_ang = S.res()
            t0 = sbt(pe_, "rt0", [128, CH]); R_t0 = S.res()
            ti = sbt(pe_, "rti", [128, CH], I32); R_ti = S.res()
            t1 = sbt(pe_, "rt1", [128, CH]); R_t1 = S.res()
            t2 = sbt(pe_, "rt2", [128, CH]); R_t2 = S.res()
            tabs = [sbt(pe_, "rtab%d" % i, [128, CH]) for i in range(2)]
            R_tabs = [S.res() for _ in range(2)]
            for c0 in range(0, L, CH):
                S.dma("sp", "posi", posi[:], pos_in[0:1, c0:c0 + CH].partition_broadcast(128).rearrange("p o n -> p (o n)"), w=[R_pi])
                S.op("dve", CP(t0[:], posi[:]), r=[R_pi], w=[R_t0])
                S.op("dve", TS(ang[:], t0[:], invf), r=[R_t0, R_c], w=[R_ang])
                for which in range(2):
                    if which == 0:
                        S.op("dve", TS(t1[:], ang[:], PI / 2, None, ALU.add), r=[R_ang], w=[R_t1])
                        src = t1
                        R_src = R_t1
                    else:
                        src = ang
                        R_src = R_ang
                    S.op("dve", TS(t0[:], src[:], 1.0 / (2 * PI), 0.5, ALU.mult, ALU.add), r=[R_src], w=[R_t0])
                    S.op("dve", CP(ti[:], t0[:]), r=[R_t0], w=[R_ti])
                    S.op("dve", CP(t0[:], ti[:]), r=[R_ti], w=[R_t0])
                    S.op("dve", STT(t2[:], t0[:], -6.28125, src[:], ALU.mult, ALU.add), r=[R_t0, R_src], w=[R_t2])
                    S.op("dve", STT(t2[:], t0[:], -0.0019353071795864769, t2[:], ALU.mult, ALU.add), r=[R_t0, R_t2], w=[R_t2])
                    S.op("dve", TS(t0[:], t2[:], -PI, 2 * PI, ALU.is_lt, ALU.mult), r=[R_t2], w=[R_t0])
                    S.op("dve", TT(t2[:], t0[:], t2[:], ALU.add), r=[R_t0, R_t2], w=[R_t2])
                    S.op("dve", TS(t2[:], t2[:], PI, -PI, ALU.min, ALU.max), r=[R_t2], w=[R_t2])
                    if which == 0:
                        S.op("act", ACTF(tabs[0][:], t2[:], AF.Sin), r=[R_t2], w=[R_tabs[0]])
                        S.dma("sp", "tabst0", cos_d[:, c0:c0 + CH], tabs[0][:], r=[R_tabs[0]], w=[R_tab[0]])
                    else:
                        S.op("act", ACTF(t1[:], t2[:], AF.Sin), r=[R_t2], w=[R_t1])
                        S.op("dve", TS(tabs[1][:], t1[:], sgn), r=[R_t1, R_c], w=[R_tabs[1]])
                        S.dma("sp", "tabst1", sin_d[:, c0:c0 + CH], tabs[1][:], r=[R_tabs[1]], w=[R_tab[1]])

        S.barrier()
        def mixer(l, xsrc, R_src, xdst, R_dst):
            P = lp[l]
            with ExitStack() as me:
                winb = sbt(me, "winb", [128, 8, INW], BF16); R_win = S.res()
                woutb = sbt(me, "woutb", [128, 8, D], BF16); R_wout = S.res()
                negA = sbt(me, "negA", [128, 8]); esink = sbt(me, "esink", [128, 8])
                Dexp = sbt(me, "Dexp", [128, 8, 64]); R_lc = S.res()
                hst = sbt(me, "hst", [128, 512]); R_hst = S.res()
                hstb = sbt(me, "hstb", [128, 512], BF16); R_hstb = S.res()
                XD = 6
                kT = [sbt(me, "kT%d" % i, [128, 128], BF16) for i in range(4)]; R_kT = [S.res() for _ in range(4)]
                vaug = [sbt(me, "vaug%d" % i, [128, 2, 65], BF16) for i in range(4)]; R_va = [S.res() for _ in range(4)]
                xpre = [sbt(me, "xpre%d" % i, [128, 8, 132], BF16) for i in range(2)]; R_xp = [S.res() for _ in range(2)]
                dgw = sbt(me, "dgw", [128, 32, 128], BF16)
                xin = [sbt(me, "xin%d" % i, [128, D]) for i in range(XD)]; R_xin = [S.res() for _ in range(XD)]
                cs = [sbt(me, "cs%d" % i, [128, 2, 128]) for i in range(2)]; R_cs = [S.res() for _ in range(2)]
                junk = sbt(me, "junk", [128, D], BF16); R_junk = S.res()
                ss = sbt(me, "ss", [128, 1]); R_ss = S.res()
                lnv = sbt(me, "lnv", [128, 1]); rstd = sbt(me, "rstd", [128, 1]); R_rstd = S.res()
                xn = sbt(me, "xn", [128, D]); R_xn = S.res()
                hT = sbt(me, "hT", [128, 8, 128], BF16); R_hT = S.res()
                rt1 = sbt(me, "rt1", [128, 4, 128]); R_rt1 = S.res()
                rt2 = sbt(me, "rt2", [128, 4, 128]); R_rt2 = S.res()
                qT = [sbt(me, "qT%d" % i, [128, 4, 128], BF16) for i in range(3)]; R_qT = [S.res() for _ in range(3)]
                xdt = sbt(me, "xdt", [128, 8]); R_xdt = S.res()
                sp1 = sbt(me, "sp1", [128, 8]); R_sp1 = S.res()
                dt = [sbt(me, "dt%d" % i, [128, 8]) for i in range(3)]; R_dt = [S.res() for _ in range(3)]
                dtA = [sbt(me, "dtA%d" % i, [128, 8], BF16) for i in range(3)]; R_dtA = [S.res() for _ in range(3)]
                xbcT = [sbt(me, "xbcT%d" % i, [128, 8, 128], BF16) for i in range(3)]; R_xbcT = [S.res() for _ in range(3)]
                pTp = [sbt(me, "pTp%d" % g, [128, 512], BF16) for g in range(2)]; R_pTp = [S.res() for _ in range(2)]
                pTc = [sbt(me, "pTc%d" % g, [128, 512], BF16) for g in range(2)]; R_pTc = [S.res() for _ in range(2)]
                den = sbt(me, "den", [128, 8]); R_den = S.res()
                attn = [sbt(me, "attn%d" % i, [128, 8, 64], BF16) for i in range(3)]; R_attn = [S.res() for _ in range(3)]
                xd = [sbt(me, "xd%d" % i, [128, 8, 64], BF16) for i in range(2)]; R_xd = [S.res() for _ in range(2)]
                xdd = sbt(me, "xdd", [128, 8, 64], BF16); R_xdd = S.res()
                xst = [sbt(me, "xst%d" % i, [128, 512], BF16) for i in range(2)]; R_xst = [S.res() for _ in range(2)]
                Btok = [sbt(me, "Btok%d" % i, [128, 2, 128], BF16) for i in range(2)]; R_Bt = [S.res() for _ in range(2)]
                acs = sbt(me, "acs", [128, 16]); R_acs = S.res()
                nacs = [sbt(me, "nacs%d" % i, [128, 8]) for i in range(2)]; ea = [sbt(me, "ea%d" % i, [128, 8]) for i in range(2)]; dte = [sbt(me, "dte%d" % i, [128, 8]) for i in range(2)]
                cdec = [sbt(me, "cdec%d" % i, [128, 8]) for i in range(2)]; dif = [sbt(me, "dif%d" % i, [128, 8]) for i in range(2)]; R_sm = [S.res() for _ in range(2)]
                udta = sbt(me, "udta", [128, 8, 128], BF16); R_udta = S.res()
                dec = sbt(me, "dec", [128, 8, 128], BF16); R_dec = S.res()
                cbs = sbt(me, "cbs", [128, 2, 128], BF16); R_cbs = S.res()
                mt = [sbt(me, "mt%d" % i, [128, 8, 128], BF16) for i in range(2)]; R_mt = [S.res() for _ in range(2)]
                htmp = sbt(me, "htmp", [128, 512]); R_htmp = S.res()
                y1 = sbt(me, "y1", [128, 512]); R_y1 = S.res()
                y2 = sbt(me, "y2", [128, 512]); R_y2 = S.res()
                sz = [sbt(me, "sz%d" % i, [128, 512]) for i in range(4)]; R_sz = [S.res() for _ in range(4)]
                ssg = sbt(me, "ssg", [128, 2]); rg = sbt(me, "rg", [128, 2]); lng = sbt(me, "lng", [128, 2]); R_ssg = S.res()
                ssm = [sbt(me, "ssm%d" % i, [128, 512], BF16) for i in range(2)]; R_ssm = [S.res() for _ in range(2)]
                mixT = sbt(me, "mixT", [128, 8, 128], BF16); R_mixT = S.res()
                xo = sbt(me, "xo", [128, D]); R_xo = S.res()
                wstg = [sbt(me, "wstg%d" % i, [128, D]) for i in range(2)]; R_wstg = [S.res() for _ in range(2)]

                cast_eng = ["pool", "act", "dve"]
                wc = 0
                for dch in range(8):
                    for pc in range(3):
                        k = wc % 2
                        eng = cast_eng[wc % 3]
                        wc += 1
                        c0, c1 = pc * 984, (pc + 1) * 984
                        S.dma("sp", "wstg%d" % k, wstg[k][:, 0:984], win_in[l][dch * 128:(dch + 1) * 128, c0:c1], w=[R_wstg[k]])
                        if eng == "act":
                            S.op("act", ACTF(winb[:, dch, c0:c1], wstg[k][:, 0:984], AF.Copy), r=[R_wstg[k]], w=[R_win])
                        else:
                            S.op(eng, CP(winb[:, dch, c0:c1], wstg[k][:, 0:984]), r=[R_wstg[k]], w=[R_win])
                for dch in range(8):
                    k = dch % 2
                    S.dma("sp", "wstg%d" % k, wstg[k][:, 0:D], wout_in[l][dch * 128:(dch + 1) * 128, :], w=[R_wstg[k]])
                    eng = cast_eng[dch % 3]
                    if eng == "act":
                        S.op("act", ACTF(woutb[:, dch, :], wstg[k][:, 0:D], AF.Copy), r=[R_wstg[k]], w=[R_wout])
                    else:
                        S.op(eng, CP(woutb[:, dch, :], wstg[k][:, 0:D]), r=[R_wstg[k]], w=[R_wout])
                S.op("act", ACTF(negA[:], P[:, P_ALOG:P_ALOG + 8], AF.Exp), r=[R_c], w=[R_lc])
                S.op("dve", TS(negA[:], negA[:], -1.0), r=[R_lc], w=[R_lc])
                S.op("act", ACTF(esink[:], P[:, P_SK:P_SK + 8], AF.Exp), r=[R_c], w=[R_lc])
                S.op("pool", CP(Dexp[:], P[:, P_DSK:P_DSK + 8].unsqueeze(2).broadcast_to([128, 8, 64])), r=[R_c], w=[R_lc])
                S.op("dve", TT(dgw[:], ident_b.unsqueeze(1).broadcast_to([128, 32, 128]),
                               P[:, P_CW:P_CW + 32].unsqueeze(2).broadcast_to([128, 32, 128]), ALU.mult), r=[R_c], w=[R_lc])
                S.op("pool", MEMSET(hst[:], 0.0), w=[R_hst])
                S.op("pool", MEMSET(hstb[:], 0.0), w=[R_hstb])
                for i in range(4):
                    S.op("pool", MEMSET(vaug[i][:], 1.0), w=[R_va[i]])
                    S.op("pool", MEMSET(kT[i][:], 0.0), w=[R_kT[i]])
                for i in range(2):
                    S.op("pool", MEMSET(xpre[i][:], 0.0), w=[R_xp[i]])

                def load(i):
                    par = i % 2
                    S.dma("sp", "xin%d" % (i % XD), xin[i % XD][:], xsrc[i * 128:(i + 1) * 128, :], r=R_src, w=[R_xin[i % XD]])
                    S.dma("sp", "cs%d" % par, cs[par][:, 0, :], cos_d[:, i * 128:(i + 1) * 128], r=R_tab, w=[R_cs[par]])
                    S.dma("sp", "cs%d" % par, cs[par][:, 1, :], sin_d[:, i * 128:(i + 1) * 128], r=R_tab, w=[R_cs[par]])

                def stageA(i):
                    par = i % 2
                    if i + 1 < NB:
                        load(i + 1)
                    X = xin[i % XD]
                    yield S.op("act", ACTF(junk[:], X[:], AF.Square, accum_out=ss[:]), r=[R_xin[i % XD]], w=[R_junk, R_ss])
                    yield S.op("act", ACTF(lnv[:], ss[:], AF.Ln, bias=epsD[:], scale=1.0 / D), r=[R_ss, R_c2], w=[R_rstd])
                    yield S.op("act", ACTF(rstd[:], lnv[:], AF.Exp, scale=-0.5), r=[R_rstd], w=[R_rstd])
                    yield S.op("act", ACTF(xn[:], X[:], AF.Identity, scale=rstd[:]), r=[R_xin[i % XD], R_rstd], w=[R_xn])
                    pa, ra = yield from PSG()
                    pb, rb = yield from PSG()
                    yield S.op("pe", TRS([(pa[:, j * 128:(j + 1) * 128], xn[:, j * 128:(j + 1) * 128], ident_f) for j in range(4)]),
                         r=[R_xn, R_c], w=[ra])
                    yield S.op("pe", TRS([(pb[:, j * 128:(j + 1) * 128], xn[:, (4 + j) * 128:(5 + j) * 128], ident_f) for j in range(4)]),
                         r=[R_xn, R_c], w=[rb])
                    yield S.op("act", ACTS([(hT[:, j, :], pa[:, j * 128:(j + 1) * 128], AF.Identity,
                                       dict(scale=gsm[l][:, j:j + 1], bias=modc[l][:, j:j + 1])) for j in range(4)]),
                         r=[R_mod], w=[ra, R_hT])
                    yield S.op("act", ACTS([(hT[:, 4 + j, :], pb[:, j * 128:(j + 1) * 128], AF.Identity,
                                       dict(scale=gsm[l][:, 4 + j:5 + j], bias=modc[l][:, 4 + j:5 + j])) for j in range(4)]),
                         r=[R_mod], w=[rb, R_hT])

                    def fm_group(ptile, cols):
                        lst = []
                        for j, c0 in enumerate(cols):
                            for dch in range(8):
                                lst.append((ptile[:, j * 128:(j + 1) * 128], winb[:, dch, c0:c0 + 128], hT[:, dch, :], dch == 0, dch == 7))
                        return MMS(lst)
                    pq, rq = yield from PSG()
                    yield S.op("pe", fm_group(pq, [C_Q + 128 * j for j in range(4)]), r=[R_win, R_hT], w=[rq])
                    pqs, rqs = yield from PSG()
                    yield S.op("pe", fm_group(pqs, [C_QS + 128 * j for j in range(4)]), r=[R_win, R_hT], w=[rqs])
                    pk, rk = yield from PSG(2)
                    yield S.op("pe", fm_group(pk, [C_K, C_KS]), r=[R_win, R_hT], w=[rk])
                    cosb = cs[par][:, 0:1, :].broadcast_to([128, 4, 128])
                    sinb = cs[par][:, 1:2, :].broadcast_to([128, 4, 128])
                    pq3 = pq[:, :].rearrange("p (c t) -> p c t", c=4)
                    pqs3 = pqs[:, :].rearrange("p (c t) -> p c t", c=4)
                    yield S.op("dve", TT(rt1[:], pq3, cosb, ALU.mult), r=[R_cs[par]], w=[rq, R_rt1])
                    yield S.op("dve", TT(rt2[:], pqs3, sinb, ALU.mult), r=[R_cs[par]], w=[rqs, R_rt2])
                    yield S.op("dve", TT(qT[i % 3][:], rt1[:], rt2[:], ALU.add), r=[R_rt1, R_rt2], w=[R_qT[i % 3]])
                    yield S.op("dve", TT(rt1[:, 0, :], pk[:, 0:128], cs[par][:, 0, :], ALU.mult), r=[R_cs[par]], w=[rk, R_rt1])
                    yield S.op("dve", TT(rt2[:, 0, :], pk[:, 128:256], cs[par][:, 1, :], ALU.mult), r=[R_cs[par]], w=[rk, R_rt2])
                    yield S.op("dve", TT(kT[i % 4][:], rt1[:, 0, :], rt2[:, 0, :], ALU.add), r=[R_rt1, R_rt2], w=[R_kT[i % 4]])
                    pxa, rxa = yield from PSG()
                    yield S.op("pe", fm_group(pxa, [C_X + 128 * j for j in range(4)]), r=[R_win, R_hT], w=[rxa])
                    pxb, rxb = yield from PSG()
                    yield S.op("pe", fm_group(pxb, [C_X + 128 * (4 + j) for j in range(4)]), r=[R_win, R_hT], w=[rxb])
                    pv, rv = yield from PSG(2)
                    yield S.op("pe", MMS([(pv[:, 0:128], hT[:, dch, :], winb[:, dch, C_V:C_V + 128], dch == 0, dch == 7) for dch in range(8)]
                                   + [(pv[:, 128:136], hT[:, dch, :], winb[:, dch, C_DT:C_DT + 8], dch == 0, dch == 7) for dch in range(8)]),
                         r=[R_win, R_hT], w=[rv])
                    pz, rz = yield from PSG()
                    yield S.op("pe", MMS([(pz[:, :], hT[:, dch, :], winb[:, dch, C_Z:C_Z + 512], dch == 0, dch == 7) for dch in range(8)]),
                         r=[R_win, R_hT], w=[rz])
                    yield S.op("act", ACTF(vaug[i % 4][:, :, 0:64], pv[:, 0:128].rearrange("p (g d) -> p g d", g=2), AF.Copy), w=[rv, R_va[i % 4]])
                    yield S.op("dve", TT(xdt[:], pv[:, 128:136], P[:, P_DTB:P_DTB + 8], ALU.add), r=[R_c], w=[rv, R_xdt])
                    yield S.op("act", ACTF(sp1[:], xdt[:], AF.Abs), r=[R_xdt], w=[R_sp1])
                    yield S.op("act", ACTF(sp1[:], sp1[:], AF.Exp, scale=-1.0), r=[R_sp1], w=[R_sp1])
                    yield S.op("act", ACTF(sp1[:], sp1[:], AF.Ln, bias=1.0), r=[R_sp1], w=[R_sp1])
                    yield S.op("dve", STT(dt[i % 3][:], xdt[:], 0.0, sp1[:], ALU.max, ALU.add), r=[R_xdt, R_sp1], w=[R_dt[i % 3]])
                    yield S.op("dve", TT(dtA[i % 3][:], dt[i % 3][:], negA[:], ALU.mult), r=[R_dt[i % 3], R_lc], w=[R_dtA[i % 3]])
                    yield S.op("act", ACTF(xpre[par][:, 0:4, 3:131], pxa[:, :].rearrange("p (c t) -> p c t", c=4), AF.Copy), w=[rxa, R_xp[par]])
                    yield S.op("act", ACTF(xpre[par][:, 4:8, 3:131], pxb[:, :].rearrange("p (c t) -> p c t", c=4), AF.Copy), w=[rxb, R_xp[par]])
                    yield S.op("act", ACTF(sz[i % 4][:], pz[:, :], AF.Silu), w=[rz, R_sz[i % 4]])

                def stageA2(i):
                    par = i % 2
                    for hf in range(2):
                        pcv, rcv = yield from PSG()
                        yield S.op("pe", MMS([(pcv[:, j * 128:(j + 1) * 128], dgw[:, (hf * 4 + j) * 4 + k_, :], xpre[par][:, hf * 4 + j, k_:k_ + 128],
                                               k_ == 0, k_ == 3) for j in range(4) for k_ in range(4)]), r=[R_xp[par], R_lc], w=[rcv])
                        if hf == 1:
                            yield S.op("pool", CP(xpre[1 - par][:, :, 0:3], xpre[par][:, :, 128:131]), r=[R_xp[par]], w=[R_xp[1 - par]])
                        yield S.op("act", ACTS([(xbcT[i % 3][:, hf * 4 + j, :], pcv[:, j * 128:(j + 1) * 128], AF.Silu,
                                                 dict(bias=P[:, P_CB + hf * 4 + j:P_CB + hf * 4 + j + 1])) for j in range(4)]),
                             r=[R_c], w=[rcv, R_xbcT[i % 3]])

                def stageB(i):
                    par = i % 2
                    q2 = qT[i % 3][:].rearrange("p c t -> p (c t)")
                    for g in range(2):
                        gs_ = slice(g * 64, (g + 1) * 64)
                        if i > 0:
                            pp, rp = yield from PSG()
                            yield S.op("pe", MMS([(pp[:, :], kT[(i - 1) % 4][gs_, :], q2[gs_, :], True, False),
                                            (pp[:, :], ident_b, mprev_b, False, True)]),
                                 r=[R_kT[(i - 1) % 4], R_qT[i % 3], R_c], w=[rp])
                            yield S.op("act", ACTF(pTp[g][:], pp[:, :], AF.Exp, scale=0.125), w=[rp, R_pTp[g]])
                        pc, rc = yield from PSG()
                        yield S.op("pe", MMS([(pc[:, :], kT[i % 4][gs_, :], q2[gs_, :], True, False),
                                        (pc[:, :], ident_b, mcur_b, False, True)]),
                             r=[R_kT[i % 4], R_qT[i % 3], R_c], w=[rc])
                        yield S.op("act", ACTF(pTc[g][:], pc[:, :], AF.Exp, scale=0.125), w=[rc, R_pTc[g]])
                    for g in range(2):
                        po, ro = yield from PSG(2)
                        lst = []
                        for j in range(4):
                            o_ = po[:, j * 65:(j + 1) * 65]
                            if i > 0:
                                lst.append((o_, pTp[g][:, j * 128:(j + 1) * 128], vaug[(i - 1) % 4][:, g, :], True, False))
                                lst.append((o_, pTc[g][:, j * 128:(j + 1) * 128], vaug[i % 4][:, g, :], False, True))
                            else:
                                lst.append((o_, pTc[g][:, j * 128:(j + 1) * 128], vaug[i % 4][:, g, :], True, True))
                        yield S.op("pe", MMS(lst), r=[R_pTp[g], R_pTc[g], R_va[i % 4], R_va[(i - 1) % 4]], w=[ro])
                        po3 = po[:, 0:260].rearrange("p (h d) -> p h d", h=4)
                        yield S.op("dve", TT(den[:, g * 4:(g + 1) * 4], po3[:, :, 64], esink[:, g * 4:(g + 1) * 4], ALU.add), r=[R_lc], w=[ro, R_den])
                        yield S.op("dve", lambda e, g=g: e.reciprocal(out=den[:, g * 4:(g + 1) * 4], in_=den[:, g * 4:(g + 1) * 4]), r=[R_den], w=[R_den])
                        yield S.op("dve", TT(attn[i % 3][:, g * 4:(g + 1) * 4, :], po3[:, :, 0:64],
                                       den[:, g * 4:(g + 1) * 4].unsqueeze(2).broadcast_to([128, 4, 64]), ALU.mult),
                             r=[R_den], w=[ro, R_attn[i % 3]])

                def stageB2(i):
                    par = i % 2
                    ptx, rtx = yield from PSG(3)
                    ptxb = ptx[:, :].bitcast(BF16)
                    yield S.op("pe", TRS([(ptxb[:, j * 128:(j + 1) * 128], xbcT[i % 3][:, j, :], ident_b) for j in range(6)]), r=[R_xbcT[i % 3], R_c], w=[rtx])
                    yield S.op("dve", TT(xd[par][:], ptxb[:, 0:512].rearrange("p (h d) -> p h d", h=8),
                                   dt[i % 3][:].unsqueeze(2).broadcast_to([128, 8, 64]), ALU.mult), r=[R_dt[i % 3]], w=[rtx, R_xd[par]])
                    yield S.op("act", ACTF(xst[par][:], ptxb[:, 0:512], AF.Copy), w=[rtx, R_xst[par]])
                    yield S.op("act", ACTF(Btok[par][:], ptxb[:, 512:768].rearrange("p (g n) -> p g n", g=2), AF.Copy), w=[rtx, R_Bt[par]])
                    pac, rac = yield from PSG()
                    yield S.op("pe", MMS([(pac[:, 0:8], U_b, dtA[i % 3][:], True, True), (pac[:, 8:16], ones_b, dtA[i % 3][:], True, True)]),
                         r=[R_dtA[i % 3], R_c], w=[rac])
                    yield S.op("act", ACTF(acs[:], pac[:, 0:16], AF.Copy), w=[rac, R_acs])
                    yield S.op("dve", TT(dif[par][:], acs[:, 8:16], acs[:, 0:8], ALU.subtract), r=[R_acs], w=[R_sm[par]])
                    yield S.op("dve", TS(nacs[par][:], acs[:, 0:8], -1.0), r=[R_acs], w=[R_sm[par]])
                    yield S.op("act", ACTF(dte[par][:], dif[par][:], AF.Exp), r=[R_sm[par]], w=[R_sm[par]])
                    yield S.op("act", ACTF(cdec[par][:], acs[:, 8:16], AF.Exp), r=[R_acs], w=[R_sm[par]])
                    yield S.op("act", ACTF(ea[par][:], acs[:, 0:8], AF.Exp), r=[R_acs], w=[R_sm[par]])
                    yield S.op("pool", TT(udta[:], U_b.unsqueeze(1).broadcast_to([128, 8, 128]),
                                    dtA[i % 3][:].unsqueeze(2).broadcast_to([128, 8, 128]), ALU.mult), r=[R_dtA[i % 3], R_c], w=[R_udta])
                    u2 = udta[:].rearrange("p h t -> p (h t)")
                    for hf in range(2):
                        pd, rd = yield from PSG()
                        yield S.op("pe", MMS([(pd[:, :], ones_b, u2[:, hf * 512:(hf + 1) * 512], True, False),
                                        (pd[:, :], ident_b, mcur_b, False, True)]), r=[R_udta, R_c], w=[rd])
                        yield S.op("act", ACTS([(dec[:, hf * 4 + j, :], pd[:, j * 128:(j + 1) * 128], AF.Exp,
                                           dict(bias=nacs[par][:, hf * 4 + j:hf * 4 + j + 1])) for j in range(4)]), r=[R_sm[par]], w=[rd, R_dec])
                    pcb, rcb = yield from PSG()
                    yield S.op("pe", MMS([(pcb[:, g * 128:(g + 1) * 128], xbcT[i % 3][:, 4 + g, :], xbcT[i % 3][:, 6 + g, :], True, True) for g in range(2)]),
                         r=[R_xbcT[i % 3]], w=[rcb])
                    yield S.op("act", ACTF(cbs[:], pcb[:, 0:256].rearrange("p (g t) -> p g t", g=2), AF.Copy), w=[rcb, R_cbs])
                    yield S.op("dve", TT(mt[par][:].rearrange("p (g r) t -> p g r t", g=2), dec[:].rearrange("p (g r) t -> p g r t", g=2),
                                    cbs[:].unsqueeze(2).broadcast_to([128, 2, 4, 128]), ALU.mult), r=[R_dec, R_cbs], w=[R_mt[par]])
                def stageB2b(i):
                    par = i % 2
                    py, ry = yield from PSG()
                    yield S.op("pe", MMS([(py[:, h * 64:(h + 1) * 64], mt[par][:, h, :], xd[par][:, h, :], True, True) for h in range(8)]),
                         r=[R_mt[par], R_xd[par]], w=[ry])
                    pyo, ryo = yield from PSG()
                    yield S.op("pe", MMS([(pyo[:, g * 256:(g + 1) * 256], xbcT[i % 3][:, 6 + g, :], hstb[:, g * 256:(g + 1) * 256], True, True) for g in range(2)]),
                         r=[R_xbcT[i % 3], R_hstb], w=[ryo])
                    yield S.op("pool", TT(xdd[:], xd[par][:], dte[par][:].unsqueeze(2).broadcast_to([128, 8, 64]), ALU.mult), r=[R_xd[par], R_sm[par]], w=[R_xdd])
                    x2 = xdd[:].rearrange("p h d -> p (h d)")
                    pst, rst = yield from PSG()
                    yield S.op("pe", MMS([(pst[:, g * 256:(g + 1) * 256], Btok[par][:, g, :], x2[:, g * 256:(g + 1) * 256], True, True) for g in range(2)]),
                         r=[R_Bt[par], R_xdd], w=[rst])
                    yield S.op("dve", TT(htmp[:].rearrange("p (h d) -> p h d", h=8), hst[:].rearrange("p (h d) -> p h d", h=8),
                                    cdec[par][:].unsqueeze(2).broadcast_to([128, 8, 64]), ALU.mult), r=[R_hst, R_sm[par]], w=[R_htmp])
                    yield S.op("dve", TT(hst[:], htmp[:], pst[:, :], ALU.add), r=[R_htmp], w=[rst, R_hst])
                    yield S.op("dve", TT(y1[:].rearrange("p (h d) -> p h d", h=8), pyo[:, :].rearrange("p (h d) -> p h d", h=8),
                                   ea[par][:].unsqueeze(2).broadcast_to([128, 8, 64]), ALU.mult), r=[R_sm[par]], w=[ryo, R_y1])
                    yield S.op("act", ACTF(hstb[:], hst[:], AF.Copy), r=[R_hst], w=[R_hstb])
                    yield S.op("dve", TT(y1[:], y1[:], py[:, :], ALU.add), r=[R_y1], w=[ry, R_y1])
                    yield S.op("pool", TT(y2[:], xst[par][:], Dexp[:].rearrange("p h d -> p (h d)"), ALU.mult), r=[R_xst[par], R_lc], w=[R_y2])
                    yield S.op("dve", TT(y2[:], y2[:], y1[:], ALU.add), r=[R_y1, R_y2], w=[R_y2])
                    yield S.op("dve", TT(y1[:], y2[:], sz[i % 4][:], ALU.mult), r=[R_y2, R_sz[i % 4]], w=[R_y1])
                    yield S.op("act", ACTS([(junk[:, g * 256:(g + 1) * 256], y1[:, g * 256:(g + 1) * 256], AF.Square,
                                       dict(accum_out=ssg[:, g:g + 1])) for g in range(2)]), r=[R_y1], w=[R_junk, R_ssg])
                    yield S.op("act", ACTF(lng[:], ssg[:], AF.Ln, bias=epsG[:], scale=1.0 / 256), r=[R_ssg, R_c2], w=[R_ssg])
                    yield S.op("act", ACTF(rg[:], lng[:], AF.Exp, scale=-0.5), r=[R_ssg], w=[R_ssg])
                    yield S.op("dve", TT(y2[:].rearrange("p (g c) -> p g c", g=2), y1[:].rearrange("p (g c) -> p g c", g=2),
                                   rg[:].unsqueeze(2).broadcast_to([128, 2, 256]), ALU.mult), r=[R_y1, R_ssg], w=[R_y2])
                    yield S.op("dve", TT(ssm[par][:], y2[:], P[:, P_SSMG:P_SSMG + 512], ALU.mult), r=[R_y2, R_c], w=[R_ssm[par]])

                def stageC(i):
                    par = i % 2
                    X = xin[i % XD]
                    pm, rm = yield from PSG()
                    pmb = pm[:, :].bitcast(BF16)
                    a2 = attn[i % 3][:].rearrange("p h d -> p (h d)")
                    yield S.op("pe", TRS([(pmb[:, j * 128:(j + 1) * 128], a2[:, j * 128:(j + 1) * 128], ident_b) for j in range(4)]
                                   + [(pmb[:, (4 + j) * 128:(5 + j) * 128], ssm[par][:, j * 128:(j + 1) * 128], ident_b) for j in range(4)]),
                         r=[R_attn[i % 3], R_ssm[par], R_c], w=[rm])
                    yield S.op("act", ACTF(mixT[:].rearrange("p c t -> p (c t)"), pmb[:, :], AF.Copy), w=[rm, R_mixT])
                    for hf in range(2):
                        pop, rop = yield from PSG()
                        yield S.op("pe", MMS([(pop[:, :], mixT[:, fch, :], woutb[:, fch, hf * 512:(hf + 1) * 512], fch == 0, fch == 7) for fch in range(8)]),
                             r=[R_mixT, R_wout], w=[rop])
                        yield S.op("dve", TT(xo[:, hf * 512:(hf + 1) * 512], pop[:, :], gmrow[l][:, hf * 512:(hf + 1) * 512], ALU.mult),
                             r=[R_mod], w=[rop, R_xo])
                    yield S.op("dve", TT(X[:], X[:], xo[:], ALU.add), r=[R_xo, R_xin[i % XD]], w=[R_xin[i % XD]])
                    yield S.dma("sp", "xst%d" % (i % XD), xdst[i * 128:(i + 1) * 128, :], X[:], r=[R_xin[i % XD]], w=[R_dst[par]])
                load(0)
                for t in range(NB + 4):
                    gens = []
                    if t < NB:
                        gens.append((stageA(t), 1))
                    if 0 <= t - 1 < NB:
                        gens.append((stageA2(t - 1), 1))
                    if 0 <= t - 2 < NB:
                        gens.append((stageB(t - 2), 1))
                        gens.append((stageB2(t - 2), 1))
                    if 0 <= t - 3 < NB:
                        gens.append((stageB2b(t - 3), 1))
                    if 0 <= t - 4 < NB:
                        gens.append((stageC(t - 4), 1))
                    while gens:
                        for it_ in list(gens):
                            g_, w_ = it_
                            try:
                                for _ in range(w_):
                                    next(g_)
                            except StopIteration:
                                gens.remove(it_)

        def chanmix(l, xsrc, R_src, xdst, R_dst, moe, final, ntok, blend):
            P = lp[l]
            NT = ntok // TTOK
            NF = (EXD if moe else FFN) // 128
            NE = NEXP if moe else 1
            GF = 2
            with ExitStack() as me:
                xt = sbt(me, "xt", [128, NBT, D]); R_xt = S.res()
                hTt = sbt(me, "hTt", [128, 8, TTOK], BF16); R_hTt = S.res()
                xn = [sbt(me, "cxn%d" % i, [128, D]) for i in range(2)]; R_xn = [S.res() for _ in range(2)]
                junk = sbt(me, "cjunk", [128, D], BF16); R_junk = S.res()
                ss = sbt(me, "css", [128, 1]); lnv = sbt(me, "clnv", [128, 1]); rstd = sbt(me, "crstd", [128, 1]); R_ss = S.res()
                ss3 = [sbt(me, "css3_%d" % i, [128, 1]) for i in range(3)]; lnv3 = [sbt(me, "clnv3_%d" % i, [128, 1]) for i in range(3)]
                rstd3 = [sbt(me, "crstd3_%d" % i, [128, 1]) for i in range(3)]; R_ss3 = [S.res() for _ in range(3)]
                xn3 = [sbt(me, "cxn3_%d" % i, [128, D]) for i in range(3)]; R_xn3 = [S.res() for _ in range(3)]
                actT = [sbt(me, "actT%d" % i, [128, GF, TTOK], BF16) for i in range(2)]; R_actT = [S.res() for _ in range(2)]
                sg = [sbt(me, "sg%d" % i, [128, 512], BF16) for i in range(2)]; R_sg = [S.res() for _ in range(2)]
                NSLOT = 3 * GF
                gstg = [sbt(me, "gstg%d" % i, [128, 8, 128]) for i in range(3)]; R_gstg = [S.res() for _ in range(3)]
                ustg = [sbt(me, "ustg%d" % i, [128, 8, 128]) for i in range(3)]; R_ustg = [S.res() for _ in range(3)]
                dstg = [sbt(me, "dstg%d" % i, [128, D]) for i in range(3)]; R_dstg = [S.res() for _ in range(3)]
                wgb = [sbt(me, "wgb%d" % i, [128, 8, 128], BF16) for i in range(NSLOT)]; R_wgb = [S.res() for _ in range(NSLOT)]
                wub = [sbt(me, "wub%d" % i, [128, 8, 128], BF16) for i in range(NSLOT)]; R_wub = [S.res() for _ in range(NSLOT)]
                wdb = [sbt(me, "wdb%d" % i, [128, D], BF16) for i in range(NSLOT)]; R_wdb = [S.res() for _ in range(NSLOT)]
                if moe:
                    hTf = sbt(me, "hTf", [128, 8, 128]); R_hTf = S.res()
                    lg = sbt(me, "lg", [128, 8]); top8 = sbt(me, "top8", [128, 8]); R_lg = S.res()
                    gt = sbt(me, "gt", [128, 4]); R_gt = S.res()
                    m1 = sbt(me, "m1", [128, 8]); m2 = sbt(me, "m2", [128, 8]); R_m = S.res()
                    wgt = sbt(me, "wgt", [128, NBT, 8]); R_wgt = S.res()
                if blend:
                    xB = [sbt(me, "xB%d" % i, [128, D]) for i in range(2)]; R_xB = [S.res() for _ in range(2)]
                if final:
                    fng = sbt(me, "fng", [128, D]); R_fng = S.res()
                    S.dma("sp", "fng", fng[:], fng_in, w=[R_fng])
                    ob = [sbt(me, "ob%d" % i, [128, D]) for i in range(2)]; R_ob = [S.res() for _ in range(2)]
                slot = [0]
                stgc = [0]
                castc = [0]
                cast_eng = ["act", "act", "act", "dve"]
                for t in range(NT):
                    def cblk(b):
                        k = b % 3
                        row0 = t * TTOK + b * 128
                        tc = slice(b * 128, (b + 1) * 128)
                        S.dma("sp", "cxt%d" % b, xt[:, b, :], xsrc[row0:row0 + 128, :], r=R_src, w=[R_xt])
                        yield None
                        yield S.op("act", ACTF(junk[:], xt[:, b, :], AF.Square, accum_out=ss3[k][:]), r=[R_xt], w=[R_junk, R_ss3[k]])
                        yield S.op("act", ACTF(lnv3[k][:], ss3[k][:], AF.Ln, bias=epsD[:], scale=1.0 / D), r=[R_ss3[k], R_c2], w=[R_ss3[k]])
                        yield S.op("act", ACTF(rstd3[k][:], lnv3[k][:], AF.Exp, scale=-0.5), r=[R_ss3[k]], w=[R_ss3[k]])
                        yield S.op("act", ACTF(xn3[k][:], xt[:, b, :], AF.Identity, scale=rstd3[k][:]), r=[R_xt, R_ss3[k]], w=[R_xn3[k]])
                        pa, ra = yield from PSG()
                        pb, rb = yield from PSG()
                        yield S.op("pe", TRS([(pa[:, j * 128:(j + 1) * 128], xn3[k][:, j * 128:(j + 1) * 128], ident_f) for j in range(4)]),
                             r=[R_xn3[k], R_c], w=[ra])
                        yield S.op("pe", TRS([(pb[:, j * 128:(j + 1) * 128], xn3[k][:, (4 + j) * 128:(5 + j) * 128], ident_f) for j in range(4)]),
                             r=[R_xn3[k], R_c], w=[rb])
                        yield S.op("act", ACTS([(hTt[:, j, tc], pa[:, j * 128:(j + 1) * 128], AF.Identity,
                                           dict(scale=gsf[l][:, j:j + 1], bias=modc[l][:, 24 + j:25 + j])) for j in range(4)]),
                             r=[R_mod], w=[ra, R_hTt])
                        yield S.op("act", ACTS([(hTt[:, 4 + j, tc], pb[:, j * 128:(j + 1) * 128], AF.Identity,
                                           dict(scale=gsf[l][:, 4 + j:5 + j], bias=modc[l][:, 28 + j:29 + j])) for j in range(4)]),
                             r=[R_mod], w=[rb, R_hTt])
                    fast = (not moe) and (not blend)
                    if fast:
                        act_g = []
                        nxt = 0
                        while nxt < NBT or act_g:
                            while len(act_g) < 3 and nxt < NBT:
                                act_g.append(cblk(nxt))
                                nxt += 1
                            for g_ in list(act_g):
                                try:
                                    next(g_)
                                except StopIteration:
                                    act_g.remove(g_)
                    for b in (range(0) if fast else range(NBT)):
                        row0 = t * TTOK + b * 128
                        k = b % 2
                        S.dma("sp", "cxt%d" % b, xt[:, b, :], xsrc[row0:row0 + 128, :], r=R_src, w=[R_xt])
                        if blend:
                            S.dma("sp", "cxB%d" % k, xB[k][:], xsrc[ntok + row0:ntok + row0 + 128, :], r=R_src, w=[R_xB[k]])
                            S.op("dve", TS(xB[k][:], xB[k][:], selt[:, 1:2]), r=[R_xB[k], R_c], w=[R_xB[k]])
                            S.op("dve", STT(xt[:, b, :], xt[:, b, :], selt[:, 0:1], xB[k][:], ALU.mult, ALU.add), r=[R_xB[k], R_c, R_xt], w=[R_xt])
                        S.op("act", ACTF(junk[:], xt[:, b, :], AF.Square, accum_out=ss[:]), r=[R_xt], w=[R_junk, R_ss])
                        S.op("act", ACTF(lnv[:], ss[:], AF.Ln, bias=epsD[:], scale=1.0 / D), r=[R_ss, R_c2], w=[R_ss])
                        S.op("act", ACTF(rstd[:], lnv[:], AF.Exp, scale=-0.5), r=[R_ss], w=[R_ss])
                        S.op("act", ACTF(xn[k][:], xt[:, b, :], AF.Identity, scale=rstd[:]), r=[R_xt, R_ss], w=[R_xn[k]])
                        pa, ra = PS()
                        pb, rb = PS()
                        S.op("pe", TRS([(pa[:, j * 128:(j + 1) * 128], xn[k][:, j * 128:(j + 1) * 128], ident_f) for j in range(4)]),
                             r=[R_xn[k], R_c], w=[ra])
                        S.op("pe", TRS([(pb[:, j * 128:(j + 1) * 128], xn[k][:, (4 + j) * 128:(5 + j) * 128], ident_f) for j in range(4)]),
                             r=[R_xn[k], R_c], w=[rb])
                        tc = slice(b * 128, (b + 1) * 128)
                        if not moe:
                            S.op("act", ACTS([(hTt[:, j, tc], pa[:, j * 128:(j + 1) * 128], AF.Identity,
                                               dict(scale=gsf[l][:, j:j + 1], bias=modc[l][:, 24 + j:25 + j])) for j in range(4)]),
                                 r=[R_mod], w=[ra, R_hTt])
                            S.op("act", ACTS([(hTt[:, 4 + j, tc], pb[:, j * 128:(j + 1) * 128], AF.Identity,
                                               dict(scale=gsf[l][:, 4 + j:5 + j], bias=modc[l][:, 28 + j:29 + j])) for j in range(4)]),
                                 r=[R_mod], w=[rb, R_hTt])
                        else:
                            S.op("act", ACTS([(hTf[:, j, :], pa[:, j * 128:(j + 1) * 128], AF.Identity,
                                               dict(scale=gsf[l][:, j:j + 1], bias=modc[l][:, 24 + j:25 + j])) for j in range(4)]),
                                 r=[R_mod], w=[ra, R_hTf])
                            S.op("act", ACTS([(hTf[:, 4 + j, :], pb[:, j * 128:(j + 1) * 128], AF.Identity,
                                               dict(scale=gsf[l][:, 4 + j:5 + j], bias=modc[l][:, 28 + j:29 + j])) for j in range(4)]),
                                 r=[R_mod], w=[rb, R_hTf])
                            S.op("pool", CP(hTt[:, :, tc], hTf[:]), r=[R_hTf], w=[R_hTt])
                            pr_, rr_ = PS()
                            S.op("pe", MMS([(pr_[:, 0:8], hTf[:, dch, :], P[:, P_RW + dch * 8:P_RW + dch * 8 + 8], dch == 0, dch == 7) for dch in range(8)]),
                                 r=[R_hTf, R_c], w=[rr_])
                            S.op("dve", CP(lg[:], pr_[:, 0:8]), w=[rr_, R_lg])
                            S.op("dve", lambda e: e.max(out=top8[:], in_=lg[:]), r=[R_lg], w=[R_lg])
                            S.op("dve", TT(gt[:, 0:1], top8[:, 1:2], top8[:, 0:1], ALU.subtract), r=[R_lg], w=[R_gt])
                            S.op("act", ACTF(gt[:, 1:2], gt[:, 0:1], AF.Exp), r=[R_gt], w=[R_gt])
                            S.op("dve", TS(gt[:, 1:2], gt[:, 1:2], 1.0, None, ALU.add), r=[R_gt], w=[R_gt])
                            S.op("dve", lambda e: e.reciprocal(out=gt[:, 2:3], in_=gt[:, 1:2]), r=[R_gt], w=[R_gt])
                            S.op("dve", TS(gt[:, 3:4], gt[:, 2:3], -1.0, 1.0, ALU.mult, ALU.add), r=[R_gt], w=[R_gt])
                            S.op("dve", TS(m1[:], lg[:], top8[:, 0:1], gt[:, 2:3], ALU.is_equal, ALU.mult), r=[R_lg, R_gt], w=[R_m])
                            S.op("dve", TS(m2[:], lg[:], top8[:, 1:2], gt[:, 3:4], ALU.is_equal, ALU.mult), r=[R_lg, R_gt], w=[R_m])
                            S.op("dve", TT(wgt[:, b, :], m1[:], m2[:], ALU.add), r=[R_m], w=[R_wgt])
                    for e_ in range(NE):
                        if moe:
                            Wg, Wu, Wd = mg_in[e_], mu_in[e_], md_in[e_]
                        else:
                            Wg, Wu, Wd = fg_in, fu_in, fd_in
                        for fg in range(NF // GF):
                            ab = fg % 2
                            slots = []
                            for fi in range(GF):
                                fc = fg * GF + fi
                                s_ = slot[0] % NSLOT
                                slot[0] += 1
                                slots.append(s_)
                                k = stgc[0] % 3
                                stgc[0] += 1
                                S.dma("sp", "gstg%d" % k, gstg[k][:], Wg[:, fc * 128:(fc + 1) * 128].rearrange("(c p) n -> p c n", p=128), w=[R_gstg[k]])
                                S.dma("sp", "ustg%d" % k, ustg[k][:], Wu[:, fc * 128:(fc + 1) * 128].rearrange("(c p) n -> p c n", p=128), w=[R_ustg[k]])
                                S.dma("sp", "dstg%d" % k, dstg[k][:], Wd[fc * 128:(fc + 1) * 128, :], w=[R_dstg[k]])
                                for (src_, R_s, dst_, R_d) in ((gstg[k], R_gstg[k], wgb[s_], R_wgb[s_]), (ustg[k], R_ustg[k], wub[s_], R_wub[s_])):
                                    ce = cast_eng[castc[0] % 4]
                                    castc[0] += 1
                                    if ce == "act":
                                        S.op("act", ACTF(dst_[:], src_[:], AF.Copy), r=[R_s], w=[R_d])
                                    else:
                                        S.op(ce, CP(dst_[:], src_[:]), r=[R_s], w=[R_d])
                                S.op("dve", TT(wdb[s_][:], dstg[k][:], gfrow[l][:], ALU.mult), r=[R_dstg[k], R_mod], w=[R_wdb[s_]])
                                SW = min(512, TTOK)
                                for sub in range(TTOK // SW):
                                    tcs = slice(sub * SW, (sub + 1) * SW)
                                    pg, rg_ = PS()
                                    S.op("pe", MMS([(pg[:, 0:SW], wgb[s_][:, dch, :], hTt[:, dch, tcs], dch == 0, dch == 7) for dch in range(8)]),
                                         r=[R_wgb[s_], R_hTt], w=[rg_])
                                    pu, ru = PS()
                                    S.op("pe", MMS([(pu[:, 0:SW], wub[s_][:, dch, :], hTt[:, dch, tcs], dch == 0, dch == 7) for dch in range(8)]),
                                         r=[R_wub[s_], R_hTt], w=[ru])
                                    kk = sub % 2
                                    S.op("act", ACTF(sg[kk][:, 0:SW], pg[:, 0:SW], AF.Silu), w=[rg_, R_sg[kk]])
                                    S.op("dve", TT(actT[ab][:, fi, tcs], sg[kk][:, 0:SW], pu[:, 0:SW], ALU.mult), r=[R_sg[kk]], w=[ru, R_actT[ab]])
                            for b in range(NBT):
                                for hf in range(2):
                                    pd, rd = PS()
                                    S.op("pe", MMS([(pd[:, :], actT[ab][:, fi, b * 128:(b + 1) * 128], wdb[slots[fi]][:, hf * 512:(hf + 1) * 512],
                                                     fi == 0, fi == GF - 1) for fi in range(GF)]),
                                         r=[R_actT[ab]] + [R_wdb[s_] for s_ in slots], w=[rd])
                                    xs_ = xt[:, b, hf * 512:(hf + 1) * 512]
                                    if moe:
                                        S.op("dve", STT(xs_, pd[:, :], wgt[:, b, e_:e_ + 1], xs_, ALU.mult, ALU.add), r=[R_wgt], w=[rd, R_xt])
                                    else:
                                        S.op("dve", TT(xs_, pd[:, :], xs_, ALU.add), w=[rd, R_xt])
                    for b in range(NBT):
                        row0 = t * TTOK + b * 128
                        if final:
                            k = b % 2
                            S.op("act", ACTF(junk[:], xt[:, b, :], AF.Square, accum_out=ss[:]), r=[R_xt], w=[R_junk, R_ss])
                            S.op("act", ACTF(lnv[:], ss[:], AF.Ln, bias=epsD[:], scale=1.0 / D), r=[R_ss, R_c2], w=[R_ss])
                            S.op("act", ACTF(rstd[:], lnv[:], AF.Exp, scale=-0.5), r=[R_ss], w=[R_ss])
                            S.op("dve", STT(ob[k][:], xt[:, b, :], rstd[:], fng[:], ALU.mult, ALU.mult), r=[R_xt, R_ss, R_fng], w=[R_ob[k]])
                            S.dma("sp", "cst%d" % k, xdst[row0:row0 + 128, :], ob[k][:], r=[R_ob[k]], w=[R_dst[k]])
                        else:
                            S.dma("sp", "cstx", xdst[row0:row0 + 128, :], xt[:, b, :], r=[R_xt], w=[R_dst[0]])

        def moe_sparse(l, xsrc, R_src, xdst, R_dst, ntok, blend):
            P = lp[l]
            NBL = ntok // 128
            NF = NFM
            GF = 4
            U32 = mybir.dt.uint32
            IOA = bass.IndirectOffsetOnAxis
            with ExitStack() as oe:
                d1i = sbt(oe, "d1i", [128, NBL], I32); d2i = sbt(oe, "d2i", [128, NBL], I32); R_di = S.res()
                g12 = sbt(oe, "g12", [128, NBL, 2]); R_g12 = S.res()
                idxw = sbt(oe, "idxw", [128, NSL, NF], I32); R_idxw = S.res()
                cst2 = sbt(oe, "cst2", [128, NSL + 1 + NF]); R_cst2 = S.res()
                S.dma("sp", "cst2", cst2[:], cst2_in, w=[R_cst2])
                R_hs = S.res(); R_ys = S.res()

                def load_x(b, xt1, R_xt1, xB, R_xB, k):
                    row0 = b * 128
                    S.dma("sp", "mx%d" % k, xt1[k][:], xsrc[row0:row0 + 128, :], r=R_src, w=[R_xt1[k]])
                    if blend:
                        S.dma("sp", "mxB%d" % k, xB[k][:], xsrc[ntok + row0:ntok + row0 + 128, :], r=R_src, w=[R_xB[k]])
                        S.op("dve", TS(xB[k][:], xB[k][:], selt[:, 1:2]), r=[R_xB[k], R_c], w=[R_xB[k]])
                        S.op("dve", STT(xt1[k][:], xt1[k][:], selt[:, 0:1], xB[k][:], ALU.mult, ALU.add), r=[R_xB[k], R_c, R_xt1[k]], w=[R_xt1[k]])

                with ExitStack() as me:
                    H = sbt(me, "H", [128, NBL, D], BF16); R_H = S.res()
                    xt1 = [sbt(me, "m1x%d" % i, [128, D]) for i in range(3)]; R_xt1 = [S.res() for _ in range(3)]
                    xB = [sbt(me, "m1xB%d" % i, [128, D]) for i in range(3)]; R_xB = [S.res() for _ in range(3)]
                    xn = [sbt(me, "m1xn%d" % i, [128, D]) for i in range(3)]; R_xn = [S.res() for _ in range(3)]
                    junk = sbt(me, "m1junk", [128, D], BF16); R_junk = S.res()
                    ss_ = [sbt(me, "m1ss%d" % i, [128, 1]) for i in range(3)]; lnv_ = [sbt(me, "m1lnv%d" % i, [128, 1]) for i in range(3)]
                    rstd_ = [sbt(me, "m1rstd%d" % i, [128, 1]) for i in range(3)]; R_ss_ = [S.res() for _ in range(3)]
                    hTf_ = [sbt(me, "m1hTf%d" % i, [128, 8, 128]) for i in range(3)]; R_hTf_ = [S.res() for _ in range(3)]
                    htmp_ = [sbt(me, "m1htmp%d" % i, [128, D]) for i in range(3)]; R_htmp_ = [S.res() for _ in range(3)]
                    gsrow = sbt(me, "gsrow", [128, D]); shrow = sbt(me, "shrow", [128, D]); R_rows = S.res()
                    dg = sbt(me, "dg", [128, 8, 128]); R_dg = S.res()
                    onesf = sbt(me, "onesf", [128, 128]); usf = sbt(me, "usf", [128, 128]); R_cf = S.res()
                    lg_ = [sbt(me, "m1lg%d" % i, [128, 8]) for i in range(3)]; top8_ = [sbt(me, "m1top8%d" % i, [128, 8]) for i in range(3)]; R_lg_ = [S.res() for _ in range(3)]
                    gt_ = [sbt(me, "m1gt%d" % i, [128, 4]) for i in range(3)]; R_gt_ = [S.res() for _ in range(3)]
                    sel1 = sbt(me, "sel1", [128, NBL, 8]); sel2 = sbt(me, "sel2", [128, NBL, 8]); R_sel = S.res()
                    selS = sbt(me, "selS", [128, NBL, 8]); R_selS = S.res()
                    wit = sbt(me, "wit", [128, NBL, 8]); R_wit = S.res()
                    ca = sbt(me, "ca", [128, NBL, 8]); cb_ = sbt(me, "cb_", [128, NBL, 8]); cnt0 = sbt(me, "cnt0", [128, NBL, 8]); R_cn = S.res()
                    sm = sbt(me, "m1sm", [128, 64]); R_smm = S.res()
                    smi = sbt(me, "m1smi", [128, 8], I32)
                    dstf = sbt(me, "dstf", [128, NBL, 8]); R_dstf = S.res()
                    d1f = sbt(me, "d1f", [128, NBL]); d2f = sbt(me, "d2f", [128, NBL]); R_df = S.res()
                    cmp = sbt(me, "cmp", [128, NSL, 8]); esl = sbt(me, "esl", [128, NSL]); R_es = S.res()
                    idxf = sbt(me, "idxf", [128, NSL, NF]); R_idxf = S.res()
                    zt = sbt(me, "zt", [128, D], BF16); R_zt = S.res()
                    S.op("dve", CP(onesf[:], ones_b), r=[R_c], w=[R_cf])
                    S.op("dve", TT(usf[:], U_b, ident_b, ALU.subtract), r=[R_c], w=[R_cf])
                    S.op("pool", MEMSET(zt[:], 0.0), w=[R_zt])
                    for rb in range(NROWS // 128):
                        S.dma("sp", "hsz", hs_d[rb * 128:(rb + 1) * 128, :], zt[:], r=[R_zt], w=[R_hs])
                    for (dst_, col_t, col0) in ((gsrow, gsf[l], 0), (shrow, modc[l], 24)):
                        S.op("dve", TT(dg[:], ident_f.unsqueeze(1).broadcast_to([128, 8, 128]),
                                       col_t[:, col0:col0 + 8].unsqueeze(2).broadcast_to([128, 8, 128]), ALU.mult), r=[R_c, R_mod], w=[R_dg])
                        d2_ = dg[:].rearrange("p c n -> p (c n)")
                        for hf in range(2):
                            pt, pr = PS()
                            S.op("pe", MMS([(pt[:, :], onesf[:], d2_[:, hf * 512:(hf + 1) * 512], True, True)]), r=[R_dg, R_cf], w=[pr])
                            S.op("act", ACTF(dst_[:, hf * 512:(hf + 1) * 512], pt[:, :], AF.Copy), w=[pr, R_rows])
                    def m1blk(b):
                        k = b % 3
                        ss, lnv, rstd, R_ss = ss_[k], lnv_[k], rstd_[k], R_ss_[k]
                        hTf, R_hTf, htmp, R_htmp = hTf_[k], R_hTf_[k], htmp_[k], R_htmp_[k]
                        lg, top8, R_lg, gt, R_gt = lg_[k], top8_[k], R_lg_[k], gt_[k], R_gt_[k]
                        load_x(b, xt1, R_xt1, xB, R_xB, k)
                        yield None
                        yield S.op("act", ACTF(junk[:], xt1[k][:], AF.Square, accum_out=ss[:]), r=[R_xt1[k]], w=[R_junk, R_ss])
                        yield S.op("act", ACTF(lnv[:], ss[:], AF.Ln, bias=epsD[:], scale=1.0 / D), r=[R_ss, R_c2], w=[R_ss])
                        yield S.op("act", ACTF(rstd[:], lnv[:], AF.Exp, scale=-0.5), r=[R_ss], w=[R_ss])
                        yield S.op("act", ACTF(xn[k][:], xt1[k][:], AF.Identity, scale=rstd[:]), r=[R_xt1[k], R_ss], w=[R_xn[k]])
                        pa, ra = yield from PSG()
                        pb, rb_ = yield from PSG()
                        yield S.op("pe", TRS([(pa[:, j * 128:(j + 1) * 128], xn[k][:, j * 128:(j + 1) * 128], ident_f) for j in range(4)]), r=[R_xn[k], R_c], w=[ra])
                        yield S.op("pe", TRS([(pb[:, j * 128:(j + 1) * 128], xn[k][:, (4 + j) * 128:(5 + j) * 128], ident_f) for j in range(4)]), r=[R_xn[k], R_c], w=[rb_])
                        yield S.op("act", ACTS([(hTf[:, j, :], pa[:, j * 128:(j + 1) * 128], AF.Identity,
                                           dict(scale=gsf[l][:, j:j + 1], bias=modc[l][:, 24 + j:25 + j])) for j in range(4)]), r=[R_mod], w=[ra, R_hTf])
                        yield S.op("act", ACTS([(hTf[:, 4 + j, :], pb[:, j * 128:(j + 1) * 128], AF.Identity,
                                           dict(scale=gsf[l][:, 4 + j:5 + j], bias=modc[l][:, 28 + j:29 + j])) for j in range(4)]), r=[R_mod], w=[rb_, R_hTf])
                        yield S.op("dve", TT(htmp[:], xn[k][:], gsrow[:], ALU.mult), r=[R_xn[k], R_rows], w=[R_htmp])
                        yield S.op("dve", TT(H[:, b, :], htmp[:], shrow[:], ALU.add), r=[R_htmp, R_rows], w=[R_H])
                        pr_, rr_ = yield from PSG()
                        yield S.op("pe", MMS([(pr_[:, 0:8], hTf[:, dch, :], P[:, P_RW + dch * 8:P_RW + dch * 8 + 8], dch == 0, dch == 7) for dch in range(8)]),
                             r=[R_hTf, R_c], w=[rr_])
                        yield S.op("dve", CP(lg[:], pr_[:, 0:8]), w=[rr_, R_lg])
                        yield S.op("dve", lambda e: e.max(out=top8[:], in_=lg[:]), r=[R_lg], w=[R_lg])
                        yield S.op("dve", TT(gt[:, 0:1], top8[:, 1:2], top8[:, 0:1], ALU.subtract), r=[R_lg], w=[R_gt])
                        yield S.op("act", ACTF(gt[:, 1:2], gt[:, 0:1], AF.Exp), r=[R_gt], w=[R_gt])
                        yield S.op("dve", TS(gt[:, 1:2], gt[:, 1:2], 1.0, None, ALU.add), r=[R_gt], w=[R_gt])
                        yield S.op("dve", lambda e, b=b: e.reciprocal(out=g12[:, b, 0:1], in_=gt[:, 1:2]), r=[R_gt], w=[R_g12])
                        yield S.op("dve", TS(g12[:, b, 1:2], g12[:, b, 0:1], -1.0, 1.0, ALU.mult, ALU.add), r=[R_g12], w=[R_g12])
                        yield S.op("dve", TS(sel1[:, b, :], lg[:], top8[:, 0:1], None, ALU.is_equal), r=[R_lg], w=[R_sel])
                        yield S.op("dve", TS(sel2[:, b, :], lg[:], top8[:, 1:2], None, ALU.is_equal), r=[R_lg], w=[R_sel])
                    act_g = []
                    nxt = 0
                    while nxt < NBL or act_g:
                        while len(act_g) < 3 and nxt < NBL:
                            act_g.append(m1blk(nxt))
                            nxt += 1
                        for g_ in list(act_g):
                            try:
                                next(g_)
                            except StopIteration:
                                act_g.remove(g_)
                    fl = lambda t_: t_[:].rearrange("p b e -> p (b e)")
                    S.op("dve", TT(selS[:], sel1[:], sel2[:], ALU.add), r=[R_sel], w=[R_selS])
                    NCOL = NBL * 8
                    for c0 in range(0, NCOL, 512):
                        c1 = min(NCOL, c0 + 512)
                        pw, rw = PS()
                        S.op("pe", MMS([(pw[:, 0:c1 - c0], usf[:], fl(selS)[:, c0:c1], True, True)]), r=[R_selS, R_cf], w=[rw])
                        S.op("act", ACTF(fl(wit)[:, c0:c1], pw[:, 0:c1 - c0], AF.Copy), w=[rw, R_wit])
                        pc_, rc_ = PS()
                        S.op("pe", MMS([(pc_[:, 0:c1 - c0], onesf[:], fl(selS)[:, c0:c1], True, True)]), r=[R_selS, R_cf], w=[rc_])
                        S.op("act", ACTF(fl(cnt0)[:, c0:c1], pc_[:, 0:c1 - c0], AF.Copy), w=[rc_, R_cn])
                    S.op("dve", CP(ca[:], cnt0[:]), r=[R_cn], w=[R_cn])
                    cur, oth = ca, cb_
                    sh = 1
                    while sh < NBL:
                        S.op("dve", CP(oth[:, 0:sh, :], cur[:, 0:sh, :]), r=[R_cn], w=[R_cn])
                        S.op("dve", TT(oth[:, sh:NBL, :], cur[:, sh:NBL, :], cur[:, 0:NBL - sh, :], ALU.add), r=[R_cn], w=[R_cn])
                        cur, oth = oth, cur
                        sh *= 2
                    incl = cur
                    S.op("dve", TT(oth[:], incl[:], cnt0[:], ALU.subtract), r=[R_cn], w=[R_cn])
                    base = oth
                    tot = incl[:, NBL - 1, :]
                    S.op("dve", TS(sm[:, 0:8], tot, float(SLOTR - 1), 1.0 / SLOTR, ALU.add, ALU.mult), r=[R_cn], w=[R_smm])
                    S.op("dve", TS(sm[:, 0:8], sm[:, 0:8], -0.5 + 0.5 / SLOTR, None, ALU.add), r=[R_smm], w=[R_smm])
                    S.op("dve", CP(smi[:], sm[:, 0:8]), r=[R_smm], w=[R_smm])
                    S.op("dve", CP(sm[:, 0:8], smi[:]), r=[R_smm], w=[R_smm])
                    S.op("dve", TS(sm[:, 0:8], sm[:, 0:8], float(SLOTR), None, ALU.mult), r=[R_smm], w=[R_smm])
                    S.op("dve", CP(sm[:, 8:16], sm[:, 0:8]), r=[R_smm], w=[R_smm])
                    a0, b0 = 8, 16
                    for sh in (1, 2, 4):
                        S.op("dve", CP(sm[:, b0:b0 + sh], sm[:, a0:a0 + sh]), r=[R_smm], w=[R_smm])
                        S.op("dve", TT(sm[:, b0 + sh:b0 + 8], sm[:, a0 + sh:a0 + 8], sm[:, a0:a0 + 8 - sh], ALU.add), r=[R_smm], w=[R_smm])
                        a0, b0 = b0, a0
                    pend = sm[:, a0:a0 + 8]
                    S.op("dve", TT(sm[:, 24:32], pend, sm[:, 0:8], ALU.subtract), r=[R_smm], w=[R_smm])
                    pstart = sm[:, 24:32]
                    S.op("dve", TT(dstf[:], base[:], wit[:], ALU.add), r=[R_cn, R_wit], w=[R_dstf])
                    S.op("dve", TT(dstf[:], dstf[:], pstart.unsqueeze(1).broadcast_to([128, NBL, 8]), ALU.add), r=[R_smm, R_dstf], w=[R_dstf])
                    for (sel_, df_, di_) in ((sel1, d1f, d1i), (sel2, d2f, d2i)):
                        S.op("dve", TT(selS[:], sel_[:], dstf[:], ALU.mult), r=[R_sel, R_dstf, R_selS], w=[R_selS])
                        S.op("dve", lambda e, df_=df_: e.tensor_reduce(out=df_[:], in_=selS[:], axis=mybir.AxisListType.X, op=ALU.add), r=[R_selS], w=[R_df])
                        S.op("dve", CP(di_[:], df_[:]), r=[R_df], w=[R_di])
                    S.op("dve", TT(cmp[:], pend.unsqueeze(1).broadcast_to([128, NSL, 8]),
                                   cst2[:, 0:NSL].unsqueeze(2).broadcast_to([128, NSL, 8]), ALU.is_le), r=[R_smm, R_cst2], w=[R_es])
                    S.op("dve", lambda e: e.tensor_reduce(out=esl[:], in_=cmp[:], axis=mybir.AxisListType.X, op=ALU.add), r=[R_es], w=[R_es])
                    S.op("dve", TS(esl[:], esl[:], 7.0, 128.0, ALU.min, ALU.mult), r=[R_es], w=[R_es])
                    S.op("dve", TS(esl[:], esl[:], cst2[:, NSL:NSL + 1], None, ALU.add), r=[R_es, R_cst2], w=[R_es])
                    S.op("dve", TT(idxf[:], esl[:].unsqueeze(2).broadcast_to([128, NSL, NF]),
                                   cst2[:, NSL + 1:NSL + 1 + NF].unsqueeze(1).broadcast_to([128, NSL, NF]), ALU.add), r=[R_es, R_cst2], w=[R_idxf])
                    S.op("dve", CP(idxw[:], idxf[:]), r=[R_idxf], w=[R_idxw])
                    if DBG_M1:
                        dbg = sbt(me, "dbg", [128, D]); R_dbg = S.res()
                        S.op("pool", MEMSET(dbg[:], 0.0), w=[R_dbg])
                        S.op("dve", CP(dbg[:, 0:NBL], d1f[:]), r=[R_df, R_dbg], w=[R_dbg])
                        S.op("dve", CP(dbg[:, 64:64 + NBL], d2f[:]), r=[R_df, R_dbg], w=[R_dbg])
                        S.op("dve", CP(dbg[:, 128:128 + NSL], esl[:]), r=[R_es, R_dbg], w=[R_dbg])
                        S.op("dve", CP(dbg[:, 192:256], sm[:]), r=[R_smm, R_dbg], w=[R_dbg])
                        S.op("dve", CP(dbg[:, 256:256 + NBL * 8], dstf[:].rearrange("p b e -> p (b e)")), r=[R_dstf, R_dbg], w=[R_dbg])
                        S.op("dve", CP(dbg[:, 512:512 + NBL * 8], sel1[:].rearrange("p b e -> p (b e)")), r=[R_sel, R_dbg], w=[R_dbg])
                        S.op("dve", CP(dbg[:, 768:768 + NBL * 8], sel2[:].rearrange("p b e -> p (b e)")), r=[R_sel, R_dbg], w=[R_dbg])
                        S.dma("sp", "dbg", xdst[0:128, :], dbg[:], r=[R_dbg], w=[R_dst[0]])
                        S.dma("sp", "dbg", xdst[128:256, 0:NSL * NF], idxf[:].rearrange("p s f -> p (s f)"), r=[R_idxf], w=[R_dst[0]])
                        return
                    for b in range(NBL):
                        S.idma("sc1", hs_d, IOA(ap=d1i[:, b:b + 1], axis=0), H[:, b, :], None, r=[R_H, R_di], w=[R_hs])
                        S.idma("sc1", hs_d, IOA(ap=d2i[:, b:b + 1], axis=0), H[:, b, :], None, r=[R_H, R_di], w=[R_hs])
                S.barrier()

                with ExitStack() as me:
                    hTt2 = [sbt(me, "shTt%d" % i, [128, 8, SLOTR], BF16) for i in range(2)]; R_hTt2 = [S.res() for _ in range(2)]
                    hrow = [sbt(me, "hrow%d" % i, [128, D], BF16) for i in range(2)]; R_hrow = [S.res() for _ in range(2)]
                    acc = sbt(me, "sacc", [128, SLOTR // 128, D]); R_acc = S.res()
                    actT = [sbt(me, "sactT%d" % i, [128, GF, SLOTR], BF16) for i in range(2)]; R_actT = [S.res() for _ in range(2)]
                    sg = [sbt(me, "ssg%d" % i, [128, 512], BF16) for i in range(2)]; R_sg = [S.res() for _ in range(2)]
                    NSLOT = 2 * GF
                    gstg = [sbt(me, "sgstg%d" % i, [128, 8, 128]) for i in range(3)]; R_gstg = [S.res() for _ in range(3)]
                    ustg = [sbt(me, "sustg%d" % i, [128, 8, 128]) for i in range(3)]; R_ustg = [S.res() for _ in range(3)]
                    dstg = [sbt(me, "sdstg%d" % i, [128, D]) for i in range(3)]; R_dstg = [S.res() for _ in range(3)]
                    wgb = [sbt(me, "swgb%d" % i, [128, 8, 128], BF16) for i in range(NSLOT)]; R_wgb = [S.res() for _ in range(NSLOT)]
                    wub = [sbt(me, "swub%d" % i, [128, 8, 128], BF16) for i in range(NSLOT)]; R_wub = [S.res() for _ in range(NSLOT)]
                    wdb = [sbt(me, "swdb%d" % i, [128, D], BF16) for i in range(NSLOT)]; R_wdb = [S.res() for _ in range(NSLOT)]
                    slot = [0]; stgc = [0]; castc = [0]
                    NRB = SLOTR // 128
                    def prep(sj):
                        hTt_, R_hTt_ = hTt2[sj % 2], R_hTt2[sj % 2]
                        for rb in range(NRB):
                            k = rb % 2
                            r0 = sj * SLOTR + rb * 128
                            S.dma("sp", "hrow%d" % k, hrow[k][:], hs_d[r0:r0 + 128, :], r=[R_hs], w=[R_hrow[k]])
                            pt, pr = PS()
                            ptb = pt[:, :].bitcast(BF16)
                            S.op("pe", TRS([(ptb[:, j * 128:(j + 1) * 128], hrow[k][:, j * 128:(j + 1) * 128], ident_b) for j in range(8)]),
                                 r=[R_hrow[k], R_c], w=[pr])
                            S.op("act", ACTF(hTt_[:, :, rb * 128:(rb + 1) * 128], ptb[:, :].rearrange("p (c t) -> p c t", c=8), AF.Copy), w=[pr, R_hTt_])
                    prep(0)
                    for s_i in range(NSL):
                        hTt, R_hTt = hTt2[s_i % 2], R_hTt2[s_i % 2]
                        for fg in range(NF // GF):
                            if fg == max(0, NF // GF - 2) and s_i + 1 < NSL:
                                prep(s_i + 1)
                            ab = fg % 2
                            slots = []
                            for fi in range(GF):
                                fc = fg * GF + fi
                                sl_ = slot[0] % NSLOT
                                slot[0] += 1
                                slots.append(sl_)
                                k = stgc[0] % 3
                                stgc[0] += 1
                                io = IOA(ap=idxw[:, s_i, fc:fc + 1], axis=0)
                                S.idma("sgs%d" % k, gstg[k][:].rearrange("p c n -> p (c n)"), None, mgl_in, io, r=[R_idxw], w=[R_gstg[k]])
                                S.idma("sus%d" % k, ustg[k][:].rearrange("p c n -> p (c n)"), None, mul_in, io, r=[R_idxw], w=[R_ustg[k]])
                                S.idma("sds%d" % k, dstg[k][:], None, mdl_in, io, r=[R_idxw], w=[R_dstg[k]])
                                for (src_, R_s, dst_, R_d) in ((gstg[k], R_gstg[k], wgb[sl_], R_wgb[sl_]), (ustg[k], R_ustg[k], wub[sl_], R_wub[sl_])):
                                    ce = ("act", "dve")[castc[0] % 2]
                                    castc[0] += 1
                                    if ce == "act":
                                        S.op("act", ACTF(dst_[:], src_[:], AF.Copy), r=[R_s], w=[R_d])
                                    else:
                                        S.op("dve", CP(dst_[:], src_[:]), r=[R_s], w=[R_d])
                                S.op("dve", TT(wdb[sl_][:], dstg[k][:], gfrow[l][:], ALU.mult), r=[R_dstg[k], R_mod], w=[R_wdb[sl_]])
                                for sub in range(SLOTR // 512):
                                    tcs = slice(sub * 512, (sub + 1) * 512)
                                    pg, rg_ = PS()
                                    S.op("pe", MMS([(pg[:, :], wgb[sl_][:, dch, :], hTt[:, dch, tcs], dch == 0, dch == 7) for dch in range(8)]),
                                         r=[R_wgb[sl_], R_hTt], w=[rg_])
                                    pu, ru = PS()
                                    S.op("pe", MMS([(pu[:, :], wub[sl_][:, dch, :], hTt[:, dch, tcs], dch == 0, dch == 7) for dch in range(8)]),
                                         r=[R_wub[sl_], R_hTt], w=[ru])
                                    kk = sub % 2
                                    S.op("act", ACTF(sg[kk][:], pg[:, :], AF.Silu), w=[rg_, R_sg[kk]])
                                    S.op("dve", TT(actT[ab][:, fi, tcs], sg[kk][:], pu[:, :], ALU.mult), r=[R_sg[kk]], w=[ru, R_actT[ab]])
                            for rb in range(NRB):
                                for hf in range(2):
                                    pd, rd = PS()
                                    S.op("pe", MMS([(pd[:, :], actT[ab][:, fi, rb * 128:(rb + 1) * 128], wdb[slots[fi]][:, hf * 512:(hf + 1) * 512],
                                                     fi == 0, fi == GF - 1) for fi in range(GF)]),
                                         r=[R_actT[ab]] + [R_wdb[x_] for x_ in slots], w=[rd])
                                    xs_ = acc[:, rb, hf * 512:(hf + 1) * 512]
                                    if fg == 0:
                                        S.op("act", ACTF(xs_, pd[:, :], AF.Copy), w=[rd, R_acc])
                                    else:
                                        S.op("dve", TT(xs_, pd[:, :], xs_, ALU.add), w=[rd, R_acc])
                        for rb in range(NRB):
                            r0 = s_i * SLOTR + rb * 128
                            S.dma("sp", "yst", ys_d[r0:r0 + 128, :], acc[:, rb, :], r=[R_acc], w=[R_ys])
                S.barrier()

                with ExitStack() as me:
                    xt1 = [sbt(me, "m3x%d" % i, [128, D]) for i in range(2)]; R_xt1 = [S.res() for _ in range(2)]
                    xB = [sbt(me, "m3xB%d" % i, [128, D]) for i in range(2)]; R_xB = [S.res() for _ in range(2)]
                    y1t = [sbt(me, "m3y1%d" % i, [128, D]) for i in range(2)]; R_y1t = [S.res() for _ in range(2)]
                    y2t = [sbt(me, "m3y2%d" % i, [128, D]) for i in range(2)]; R_y2t = [S.res() for _ in range(2)]
                    ob = [sbt(me, "m3ob%d" % i, [128, D]) for i in range(2)]; R_ob = [S.res() for _ in range(2)]
                    junk = sbt(me, "m3junk", [128, D], BF16); R_junk = S.res()
                    ss = sbt(me, "m3ss", [128, 1]); lnv = sbt(me, "m3lnv", [128, 1]); rstd = sbt(me, "m3rstd", [128, 1]); R_ss = S.res()
                    fng = sbt(me, "m3fng", [128, D]); R_fng = S.res()
                    S.dma("sp", "fng", fng[:], fng_in, w=[R_fng])
                    for b in range(NBL):
                        k = b % 2
                        load_x(b, xt1, R_xt1, xB, R_xB, k)
                        S.idma("g1_%d" % k, y1t[k][:], None, ys_d, IOA(ap=d1i[:, b:b + 1], axis=0), r=[R_ys, R_di], w=[R_y1t[k]])
                        S.idma("g2_%d" % k, y2t[k][:], None, ys_d, IOA(ap=d2i[:, b:b + 1], axis=0), r=[R_ys, R_di], w=[R_y2t[k]])
                        S.op("dve", STT(xt1[k][:], y1t[k][:], g12[:, b, 0:1], xt1[k][:], ALU.mult, ALU.add), r=[R_y1t[k], R_g12, R_xt1[k]], w=[R_xt1[k]])
                        S.op("dve", STT(xt1[k][:], y2t[k][:], g12[:, b, 1:2], xt1[k][:], ALU.mult, ALU.add), r=[R_y2t[k], R_g12, R_xt1[k]], w=[R_xt1[k]])
                        S.op("act", ACTF(junk[:], xt1[k][:], AF.Square, accum_out=ss[:]), r=[R_xt1[k]], w=[R_junk, R_ss])
                        S.op("act", ACTF(lnv[:], ss[:], AF.Ln, bias=epsD[:], scale=1.0 / D), r=[R_ss, R_c2], w=[R_ss])
                        S.op("act", ACTF(rstd[:], lnv[:], AF.Exp, scale=-0.5), r=[R_ss], w=[R_ss])
                        S.op("dve", STT(ob[k][:], xt1[k][:], rstd[:], fng[:], ALU.mult, ALU.mult), r=[R_xt1[k], R_ss, R_fng], w=[R_ob[k]])
                        S.dma("sp", "m3st%d" % k, xdst[b * 128:(b + 1) * 128, :], ob[k][:], r=[R_ob[k]], w=[R_dst[k]])

        R_x = [S.res("x_in")]; R_xa = [S.res("xa0"), S.res("xa1")]; R_xb = [S.res("xb0"), S.res("xb1")]
        R_out = [S.res("out0"), S.res("out1")]
        stages = [
            ("mix0", lambda dst, Rd: mixer(0, x_in, R_x, dst, Rd)),
            ("ffn0", lambda dst, Rd: chanmix(0, xa_d, R_xa, dst, Rd, False, False, L, False)),
            ("mix1", lambda dst, Rd: mixer(1, xb_d, R_xb, dst, Rd)),
            ("moe1", (lambda dst, Rd: moe_sparse(1, xa_d, R_xa, dst, Rd, LM, split)) if sparse else
                     (lambda dst, Rd: chanmix(1, xa_d, R_xa, dst, Rd, True, True, LM, split))),
        ]
        dsts = [(xa_d, R_xa), (xb_d, R_xb), (xa_d, R_xa), (out_d, R_out)]
        for (name, fn), (dst, Rd) in zip(stages, dsts):
            if stop_after == name:
                fn(out_d, R_out)
                break
            fn(dst, Rd)
            S.barrier()
        S.wait_all("sp", R_out)
        with nc.Block() as block:
            S.emit(block)
    return nc


def _consts():
    cstf = np.zeros((128, 130), np.float32)
    cstf[:, :128] = np.eye(128, dtype=np.float32)
    p = np.arange(128)
    cstf[:, 128] = (10000.0 ** (-((p % 32) * 2).astype(np.float64) / 64.0)).astype(np.float32)
    cstf[:, 129] = np.where((p % 64) < 32, -1.0, 1.0)
    cb = np.zeros((128, B_TOT), np.float32)
    cb[:, B_ID:B_ID + 128] = np.eye(128)
    k = np.arange(128)[:, None]
    q = np.arange(128)[None, :]
    cb[:, B_U:B_U + 128] = (k <= q)
    cb[:, B_ONES:B_ONES + 128] = 1.0
    cb[:, B_MCUR:B_MCUR + 512] = np.tile(np.where(k <= q, 0.0, NEG), (1, 4))
    cb[:, B_MPREV:B_MPREV + 512] = np.tile(np.where(k > q, 0.0, NEG), (1, 4))
    return cstf, cb.astype(ml_dtypes.bfloat16)


def _perm_win(w):
    q = w[:, 0:512].reshape(D, 8, 64)
    k = w[:, 512:640].reshape(D, 2, 64)
    v = w[:, 640:768]
    z = w[:, 768:1280]
    xbc = w[:, 1280:2304]
    dt = w[:, 2304:2312]
    sw = lambda a: np.concatenate([a[..., 32:], a[..., :32]], axis=-1)
    qp = np.stack([np.concatenate([q[:, j], q[:, 4 + j]], axis=-1) for j in range(4)], axis=1).reshape(D, 512)
    qs = sw(q)
    qsp = np.stack([np.concatenate([qs[:, j], qs[:, 4 + j]], axis=-1) for j in range(4)], axis=1).reshape(D, 512)
    kp = k.reshape(D, 128)
    ks = sw(k).reshape(D, 128)
    return np.ascontiguousarray(np.concatenate([qp, qsp, kp, ks, xbc, v, z, dt], axis=1))


def _lp(l, norm_mix_g, norm_ffn_g, conv_w, conv_b, attn_sinks, dt_bias, a_log, d_skip, ssm_norm_g, router_w):
    lp = np.zeros((128, P_TOT), np.float32)
    lp[:, P_GM:P_GM + 8] = norm_mix_g[l].reshape(8, 128).T
    lp[:, P_GF:P_GF + 8] = norm_ffn_g[l].reshape(8, 128).T
    lp[:, P_CW:P_CW + 32] = conv_w[l].reshape(4, 8, 128).transpose(2, 1, 0).reshape(128, 32)
    lp[:, P_CB:P_CB + 8] = conv_b[l].reshape(8, 128).T
    lp[:, P_SK:P_SK + 8] = attn_sinks[l][None, :]
    lp[:, P_DTB:P_DTB + 8] = dt_bias[l][None, :]
    lp[:, P_ALOG:P_ALOG + 8] = a_log[l][None, :]
    lp[:, P_DSK:P_DSK + 8] = d_skip[l][None, :]
    lp[:, P_SSMG:P_SSMG + 512] = ssm_norm_g[l][None, :]
    if l == 1:
        lp[:, P_RW:P_RW + 64] = router_w[0].reshape(8, 128, 8).transpose(1, 0, 2).reshape(128, 64)
    return lp


def make_in_maps(inp, L, ncores, split=True, sparse=True):
    f = lambda a: np.ascontiguousarray(np.asarray(a, dtype=np.float32))
    cstf, cstb = _consts()
    shared = {
        "cstf": cstf, "cstb": cstb,
        "fng": np.ascontiguousarray(np.broadcast_to(f(inp["final_norm_g"])[None, :], (128, D))),
        "ffg": f(inp["ffn_w_gate"][0]), "ffu": f(inp["ffn_w_up"][0]), "ffd": f(inp["ffn_w_down"][0]),
    }
    if not sparse:
        shared.update({"mog": f(inp["moe_w_gate"][0]), "mou": f(inp["moe_w_up"][0]), "mod": f(inp["moe_w_down"][0])})
    for l in range(2):
        shared["lp%d" % l] = _lp(l, f(inp["norm_mix_g"]), f(inp["norm_ffn_g"]), f(inp["conv_w"]), f(inp["conv_b"]),
                                 f(inp["attn_sinks"]), f(inp["dt_bias"]), f(inp["a_log"]), f(inp["d_skip"]),
                                 f(inp["ssm_norm_g"]), f(inp["router_w"]))
        shared["adaw%d" % l] = f(inp["ada_w"][l])
        shared["adab%d" % l] = f(inp["ada_b"][l])[None, :]
        shared["win%d" % l] = _perm_win(f(inp["w_in"][l]))
        shared["wout%d" % l] = f(inp["w_out"][l])
    if sparse:
        NFM = EXD // 128
        LM = L // 2 if split else L
        NSL = (2 * LM) // SLOTR + NEXP
        wg = f(inp["moe_w_gate"][0]); wu = f(inp["moe_w_up"][0]); wd = f(inp["moe_w_down"][0])
        lay = lambda w: np.ascontiguousarray(w.reshape(8, 8, 128, NFM, 128).transpose(3, 0, 2, 1, 4).reshape(NFM * 1024, 1024))
        shared["mogl"] = lay(wg)
        shared["moul"] = lay(wu)
        shared["modl"] = np.ascontiguousarray(wd.reshape(8, NFM, 128, 1024).transpose(1, 0, 2, 3).reshape(NFM * 1024, 1024))
        c2 = np.zeros((128, NSL + 1 + NFM), np.float32)
        c2[:, :NSL] = (np.arange(NSL) * SLOTR)[None, :]
        c2[:, NSL] = np.arange(128)
        c2[:, NSL + 1:] = (np.arange(NFM) * 1024)[None, :]
        shared["cst2"] = c2
    x = np.asarray(inp["x"], dtype=np.float32)
    c = np.asarray(inp["c"], dtype=np.float32)
    pos = np.asarray(inp["positions"], dtype=np.int32)
    maps = []
    for r in range(ncores):
        b, half = (r // 2, r % 2) if split else (r, 0)
        m = dict(shared)
        m["x"] = np.ascontiguousarray(x[b, :L])
        m["pos"] = np.ascontiguousarray(pos[b:b + 1, :L])
        m["cT"] = np.ascontiguousarray(c[b].reshape(8, 128).T)
        sel = np.zeros((128, 2), np.float32)
        sel[:, half] = 1.0
        m["sel"] = sel
        maps.append(m)
    return maps


_NC_CACHE = {}


def kernel(**inputs):
    x = np.asarray(inputs["x"])
    B, L, _ = x.shape
    if L not in _NC_CACHE:
        _NC_CACHE[L] = build(L)
    nc = _NC_CACHE[L]
    ncores = 2 * B
    maps = make_in_maps(inputs, L, ncores)
    res = run_bass_kernel_spmd(nc, maps, core_ids=list(range(ncores)))
    out = np.empty((B, L, D), np.float32)
    h = L // 2
    for r in range(ncores):
        out[r // 2, (r % 2) * h:(r % 2 + 1) * h] = np.asarray(res.results[r]["out"])
    return out
```
